# Optimizing a Trainium2 kernel written in Bass

```python
import math
import jax
import jax.numpy as jnp
from jax import lax
import numpy as np

D_MODEL = 1024
BATCH = 2
SEQ = 16384
DEPTH = 1

N_MEM = 256
SSD_EXPAND = 2
D_INNER = SSD_EXPAND * D_MODEL
SSD_HEAD_DIM = 64
SSD_HEADS = D_INNER // SSD_HEAD_DIM
SSD_GROUPS = 4
SSD_STATE = 128
CONV_WIDTH = 4
SSD_CHUNK = 128
XBC_DIM = D_INNER + 2 * SSD_GROUPS * SSD_STATE
MLA_HEADS = 16
QK_NOPE_DIM = 64
QK_ROPE_DIM = 32
QK_HEAD_DIM = QK_NOPE_DIM + QK_ROPE_DIM
V_HEAD_DIM = 64
Q_LORA_RANK = 384
KV_LORA_RANK = 256
ROPE_THETA = 10000.0
Q_BLOCK = 128
XA_HEADS = 4
XA_HEAD_DIM = D_MODEL // XA_HEADS
N_EXPERT_GROUPS = 4
EXPERTS_PER_GROUP = 8
N_EXPERTS = N_EXPERT_GROUPS * EXPERTS_PER_GROUP
TOP_K = 2
D_EXPERT = 256
RMS_EPS = 1e-6

IN_SPLITS = (D_INNER, XBC_DIM, SSD_HEADS, Q_LORA_RANK, KV_LORA_RANK, QK_ROPE_DIM, D_MODEL, D_MODEL)
IN_PROJ_DIM = D_INNER + XBC_DIM + SSD_HEADS + Q_LORA_RANK + KV_LORA_RANK + QK_ROPE_DIM + 2 * D_MODEL

kernel_name = 'hybrid_ssd_mla_hmoe_block'


def rms_norm(x, gain):
    xf = x.astype(jnp.float32)
    xf = xf * lax.rsqrt(jnp.mean(xf * xf, axis=-1, keepdims=True) + RMS_EPS)
    return xf.astype(x.dtype) * gain


def split_cols(x, sizes):
    offs = np.cumsum(np.array(sizes))[:-1].tolist()
    return jnp.split(x, offs, axis=-1)


def causal_depthwise_conv(x, w, b):
    k, c = w.shape
    y = lax.conv_general_dilated(x, w[:, None, :], window_strides=(1,), padding=[(k - 1, 0)],
                                 dimension_numbers=('NWC', 'WIO', 'NWC'), feature_group_count=c)
    return y + b


def apply_rope(x, positions):
    half = x.shape[-1] // 2
    inv_freq = ROPE_THETA ** (-jnp.arange(half, dtype=jnp.float32) / half)
    ang = positions.astype(jnp.float32)[..., None] * inv_freq
    cos = jnp.cos(ang)[:, :, None, :]
    sin = jnp.sin(ang)[:, :, None, :]
    xf = x.astype(jnp.float32)
    x1, x2 = xf[..., :half], xf[..., half:]
    return jnp.concatenate([x1 * cos - x2 * sin, x2 * cos + x1 * sin], axis=-1).astype(x.dtype)


def ssd_chunked_scan(xh, dt, a, bm, cm):
    b, s, h, p = xh.shape
    g, n = bm.shape[2], bm.shape[3]
    r = h // g
    nc, l = s // SSD_CHUNK, SSD_CHUNK
    x_dt = (xh.astype(jnp.float32) * dt[..., None]).reshape(b, nc, l, g, r, p)
    a_dt = (dt * a).reshape(b, nc, l, g, r).transpose(0, 3, 4, 1, 2)
    bc = bm.astype(jnp.float32).reshape(b, nc, l, g, n)
    cc = cm.astype(jnp.float32).reshape(b, nc, l, g, n)
    a_cum = jnp.cumsum(a_dt, axis=-1)
    causal = jnp.tril(jnp.ones((l, l), dtype=bool))
    seg = a_cum[..., :, None] - a_cum[..., None, :]
    decay_in = jnp.exp(jnp.where(causal, seg, -jnp.inf))
    cb = jnp.einsum('bclgn,bcsgn->bcgls', cc, bc)
    y_diag = jnp.einsum('bcgls,bgrcls,bcsgrp->bclgrp', cb, decay_in, x_dt)
    decay_to_end = jnp.exp(a_cum[..., -1:] - a_cum)
    chunk_states = jnp.einsum('bclgn,bgrcl,bclgrp->bcgrpn', bc, decay_to_end, x_dt)
    chunk_decay = jnp.exp(a_cum[..., -1])

    def step(state, inp):
        st, dec = inp
        return state * dec[..., None, None] + st, state

    init = jnp.zeros((b, g, r, p, n), jnp.float32)
    _, prev_states = lax.scan(step, init, (jnp.moveaxis(chunk_states, 1, 0), jnp.moveaxis(chunk_decay, -1, 0)))
    prev_states = jnp.moveaxis(prev_states, 0, 1)
    y_off = jnp.einsum('bclgn,bcgrpn,bgrcl->bclgrp', cc, prev_states, jnp.exp(a_cum))
    return (y_diag + y_off).reshape(b, s, h, p)


def ssd_branch(z, xbc, dt_raw, conv_w, conv_b, dt_bias, a_log, d_skip, ssd_norm):
    b, s, _ = z.shape
    xbc = jax.nn.silu(causal_depthwise_conv(xbc, conv_w, conv_b))
    xs, bm, cm = split_cols(xbc, (D_INNER, SSD_GROUPS * SSD_STATE, SSD_GROUPS * SSD_STATE))
    xh = xs.reshape(b, s, SSD_HEADS, SSD_HEAD_DIM)
    bm = bm.reshape(b, s, SSD_GROUPS, SSD_STATE)
    cm = cm.reshape(b, s, SSD_GROUPS, SSD_STATE)
    dt = jax.nn.softplus(dt_raw.astype(jnp.float32) + dt_bias.astype(jnp.float32))
    a = -jnp.exp(a_log.astype(jnp.float32))
    y = ssd_chunked_scan(xh, dt, a, bm, cm)
    y = y + d_skip.astype(jnp.float32)[:, None] * xh.astype(jnp.float32)
    y = y.reshape(b, s, D_INNER) * jax.nn.silu(z.astype(jnp.float32))
    y = y.reshape(b, s, SSD_GROUPS, D_INNER // SSD_GROUPS)
    y = y * lax.rsqrt(jnp.mean(y * y, axis=-1, keepdims=True) + RMS_EPS)
    return y.reshape(b, s, D_INNER).astype(z.dtype) * ssd_norm


def causal_block_attention(q, k, v, scale):
    b, s, h, dq = q.shape
    nb = s // Q_BLOCK
    qb = jnp.moveaxis(q.reshape(b, nb, Q_BLOCK, h, dq), 1, 0)
    kpos = jnp.arange(s)

    def one_block(args):
        i, qi = args
        sc = jnp.einsum('bqhd,bkhd->bhqk', qi, k).astype(jnp.float32) * scale
        qpos = i * Q_BLOCK + jnp.arange(Q_BLOCK)
        sc = jnp.where(kpos[None, :] <= qpos[:, None], sc, -jnp.inf)
        pr = jax.nn.softmax(sc, axis=-1).astype(v.dtype)
        return jnp.einsum('bhqk,bkhd->bqhd', pr, v)

    out = lax.map(one_block, (jnp.arange(nb), qb))
    return jnp.moveaxis(out, 0, 1).reshape(b, s, h, v.shape[-1])


def mla_branch(q_a, c_kv, k_rope, positions, q_a_norm, w_q_b, kv_a_norm, w_kv_b):
    b, s, _ = q_a.shape
    q = (rms_norm(q_a, q_a_norm) @ w_q_b).reshape(b, s, MLA_HEADS, QK_HEAD_DIM)
    kv = (rms_norm(c_kv, kv_a_norm) @ w_kv_b).reshape(b, s, MLA_HEADS, QK_NOPE_DIM + V_HEAD_DIM)
    q_nope, q_pe = q[..., :QK_NOPE_DIM], q[..., QK_NOPE_DIM:]
    k_nope, v = kv[..., :QK_NOPE_DIM], kv[..., QK_NOPE_DIM:]
    q_pe = apply_rope(q_pe, positions)
    k_pe = apply_rope(k_rope[:, :, None, :], positions)
    q = jnp.concatenate([q_nope, q_pe], axis=-1)
    k = jnp.concatenate([k_nope, jnp.broadcast_to(k_pe, (b, s, MLA_HEADS, QK_ROPE_DIM))], axis=-1)
    o = causal_block_attention(q, k, v, QK_HEAD_DIM ** -0.5)
    return o.reshape(b, s, MLA_HEADS * V_HEAD_DIM)


def memory_cross_attention(h, mem, w_xq, w_xkv, w_xo):
    b, s, _ = h.shape
    m = mem.shape[1]
    q = (h @ w_xq).reshape(b, s, XA_HEADS, XA_HEAD_DIM)
    k, v = jnp.split(mem @ w_xkv, 2, axis=-1)
    k = k.reshape(b, m, XA_HEADS, XA_HEAD_DIM)
    v = v.reshape(b, m, XA_HEADS, XA_HEAD_DIM)
    sc = jnp.einsum('bshd,bmhd->bhsm', q, k).astype(jnp.float32) * XA_HEAD_DIM ** -0.5
    pr = jax.nn.softmax(sc, axis=-1).astype(v.dtype)
    o = jnp.einsum('bhsm,bmhd->bshd', pr, v).reshape(b, s, XA_HEADS * XA_HEAD_DIM)
    return o @ w_xo


def hierarchical_moe(h, w_rg, b_rg, w_re, b_re, w_gate, w_up, w_down):
    b, s, d = h.shape
    t = h.reshape(b * s, d)
    g_logits = (t @ w_rg).astype(jnp.float32) + b_rg
    g_prob = jax.nn.softmax(g_logits, axis=-1)
    g_sel = jnp.argmax(g_logits, axis=-1)
    g_w = jnp.take_along_axis(g_prob, g_sel[:, None], axis=1)
    e_logits = ((t @ w_re).astype(jnp.float32) + b_re).reshape(-1, N_EXPERT_GROUPS, EXPERTS_PER_GROUP)
    e_in = jnp.take_along_axis(e_logits, g_sel[:, None, None], axis=1)[:, 0]
    top_v, top_i = lax.top_k(e_in, TOP_K)
    w_top = jax.nn.softmax(top_v, axis=-1) * g_w
    expert_id = g_sel[:, None] * EXPERTS_PER_GROUP + top_i
    combine = jnp.sum(jax.nn.one_hot(expert_id, N_EXPERTS, dtype=jnp.float32) * w_top[..., None], axis=1).astype(t.dtype)
    hg = jnp.einsum('td,edf->tef', t, w_gate)
    hu = jnp.einsum('td,edf->tef', t, w_up)
    act = jax.nn.silu(hg) * hu * combine[:, :, None]
    y = jnp.einsum('tef,efd->td', act, w_down)
    return y.reshape(b, s, d)


def setup_inputs(seed: int = 0) -> dict:
    key = jax.random.key(seed)
    ks = iter(jax.random.split(key, 48))
    f32 = jnp.float32
    L = DEPTH

    def nrm(shape, fan_in):
        return jax.random.normal(next(ks), shape, f32) * fan_in ** -0.5

    def gain(shape):
        return 1.0 + 0.02 * jax.random.normal(next(ks), shape, f32)

    x = jax.random.normal(next(ks), (BATCH, SEQ, D_MODEL), f32)
    mem = jax.random.normal(next(ks), (BATCH, N_MEM, D_MODEL), f32)
    offset = jax.random.randint(next(ks), (BATCH, 1), 0, 1024, dtype=jnp.int32)
    positions = (offset + jnp.arange(SEQ, dtype=jnp.int32)[None, :]).astype(jnp.int32)
    dt0 = jnp.exp(jax.random.uniform(next(ks), (L, SSD_HEADS), f32, math.log(1e-3), math.log(1e-1)))
    dt_bias = dt0 + jnp.log(-jnp.expm1(-dt0))
    a_log = jnp.log(jax.random.uniform(next(ks), (L, SSD_HEADS), f32, 1.0, 16.0))
    return {
        'x': x,
        'mem': mem,
        'positions': positions,
        'norm_mix': gain((L, D_MODEL)),
        'w_in': nrm((L, D_MODEL, IN_PROJ_DIM), D_MODEL),
        'conv_w': nrm((L, CONV_WIDTH, XBC_DIM), CONV_WIDTH),
        'conv_b': 0.02 * jax.random.normal(next(ks), (L, XBC_DIM), f32),
        'dt_bias': dt_bias,
        'a_log': a_log,
        'd_skip': 1.0 + 0.1 * jax.random.normal(next(ks), (L, SSD_HEADS), f32),
        'ssd_norm': gain((L, D_INNER)),
        'w_ssd_out': nrm((L, D_INNER, D_MODEL), D_INNER),
        'q_a_norm': gain((L, Q_LORA_RANK)),
        'w_q_b': nrm((L, Q_LORA_RANK, MLA_HEADS * QK_HEAD_DIM), Q_LORA_RANK),
        'kv_a_norm': gain((L, KV_LORA_RANK)),
        'w_kv_b': nrm((L, KV_LORA_RANK, MLA_HEADS * (QK_NOPE_DIM + V_HEAD_DIM)), KV_LORA_RANK),
        'w_mla_out': nrm((L, MLA_HEADS * V_HEAD_DIM, D_MODEL), MLA_HEADS * V_HEAD_DIM),
        'w_o': nrm((L, D_MODEL, D_MODEL), D_MODEL),
        'norm_xattn': gain((L, D_MODEL)),
        'norm_mem': gain((L, D_MODEL)),
        'w_xq': nrm((L, D_MODEL, XA_HEADS * XA_HEAD_DIM), D_MODEL),
        'w_xkv': nrm((L, D_MODEL, 2 * XA_HEADS * XA_HEAD_DIM), D_MODEL),
        'w_xo': nrm((L, XA_HEADS * XA_HEAD_DIM, D_MODEL), XA_HEADS * XA_HEAD_DIM),
        'norm_moe': gain((L, D_MODEL)),
        'w_router_group': nrm((L, D_MODEL, N_EXPERT_GROUPS), D_MODEL),
        'b_router_group': 0.01 * jax.random.normal(next(ks), (L, N_EXPERT_GROUPS), f32),
        'w_router_expert': nrm((L, D_MODEL, N_EXPERTS), D_MODEL),
        'b_router_expert': 0.01 * jax.random.normal(next(ks), (L, N_EXPERTS), f32),
        'w_exp_gate': nrm((L, N_EXPERTS, D_MODEL, D_EXPERT), D_MODEL),
        'w_exp_up': nrm((L, N_EXPERTS, D_MODEL, D_EXPERT), D_MODEL),
        'w_exp_down': nrm((L, N_EXPERTS, D_EXPERT, D_MODEL), D_EXPERT),
        'norm_final': gain((D_MODEL,)),
    }


def reference(x, mem, positions, norm_mix, w_in, conv_w, conv_b, dt_bias, a_log, d_skip, ssd_norm, w_ssd_out,
              q_a_norm, w_q_b, kv_a_norm, w_kv_b, w_mla_out, w_o, norm_xattn, norm_mem, w_xq, w_xkv, w_xo,
              norm_moe, w_router_group, b_router_group, w_router_expert, b_router_expert,
              w_exp_gate, w_exp_up, w_exp_down, norm_final):
    h = x
    for i in range(DEPTH):
        u = rms_norm(h, norm_mix[i])
        z, xbc, dt_raw, q_a, c_kv, k_rope, gate_ssd, gate_mla = split_cols(u @ w_in[i], IN_SPLITS)
        y_ssd = ssd_branch(z, xbc, dt_raw, conv_w[i], conv_b[i], dt_bias[i], a_log[i], d_skip[i], ssd_norm[i]) @ w_ssd_out[i]
        y_mla = mla_branch(q_a, c_kv, k_rope, positions, q_a_norm[i], w_q_b[i], kv_a_norm[i], w_kv_b[i]) @ w_mla_out[i]
        merged = jax.nn.sigmoid(gate_ssd) * y_ssd + jax.nn.sigmoid(gate_mla) * y_mla
        h = h + merged @ w_o[i]
        h = h + memory_cross_attention(rms_norm(h, norm_xattn[i]), rms_norm(mem, norm_mem[i]), w_xq[i], w_xkv[i], w_xo[i])
        h = h + hierarchical_moe(rms_norm(h, norm_moe[i]), w_router_group[i], b_router_group[i],
                                 w_router_expert[i], b_router_expert[i], w_exp_gate[i], w_exp_up[i], w_exp_down[i])
    return rms_norm(h, norm_final)
```

```python
import contextlib
import numpy as np
import ml_dtypes
import concourse.bass as bass
import concourse.mybir as mybir
from concourse.bass_utils import run_bass_kernel_spmd

F32 = mybir.dt.float32
BF16 = mybir.dt.bfloat16
I32 = mybir.dt.int32
U32 = mybir.dt.uint32
AF = mybir.ActivationFunctionType
ALU = mybir.AluOpType

ENGS = ("pe", "act", "dve", "pool", "sp")


class Prog:
    dead = False
    stop_at = None
    WINDOW = 40

    def __init__(self, nc):
        self.nc = nc
        self.ops = []
        self.state = {}
        self.seg = 0
        self.last_bar = {}
        self.n_bar = 0

    def checkpoint(self, name):
        if self.stop_at is not None and name == self.stop_at:
            self.barrier()
            self.dead = True

    @staticmethod
    def _norm(acc):
        return [a if isinstance(a, tuple) else (a, None) for a in acc]

    def _deps_for(self, reads, writes):
        deps = set()
        for (name, sub) in reads:
            st = self.state.get(name)
            if not st:
                continue
            subs = list(st.keys()) if sub is None else [s for s in st.keys() if s is None or s == sub]
            for s in subs:
                w = st[s][0]
                if w is not None:
                    deps.add(w)
        for (name, sub) in writes:
            st = self.state.get(name)
            if not st:
                continue
            subs = list(st.keys()) if sub is None else [s for s in st.keys() if s is None or s == sub]
            for s in subs:
                w, rs = st[s]
                if w is not None:
                    deps.add(w)
                deps.update(rs)
        return deps

    def _update(self, idx, reads, writes):
        for (name, sub) in writes:
            st = self.state.setdefault(name, {})
            if sub is None:
                st.clear()
                st[None] = [idx, []]
            else:
                st[sub] = [idx, []]
        for (name, sub) in reads:
            st = self.state.setdefault(name, {})
            if sub is None:
                for s in st.values():
                    s[1].append(idx)
                if not st:
                    st[None] = [None, [idx]]
            else:
                if sub in st:
                    st[sub][1].append(idx)
                elif None in st:
                    st[None][1].append(idx)
                else:
                    st[sub] = [None, [idx]]

    def op(self, eng, fn, reads=(), writes=(), cost=0.2):
        if self.dead:
            return -1
        reads = self._norm(reads)
        writes = self._norm(writes)
        if eng != "pe":
            pr = [a for a in reads if a[0].startswith("PP")]
            if pr:
                writes = writes + pr
        idx = len(self.ops)
        deps = self._deps_for(reads, writes)
        if eng in self.last_bar:
            deps.add(self.last_bar[eng])
        self.ops.append(dict(eng=eng, fn=fn, deps=deps, dma=None, idx=idx, seg=self.seg, cost=cost, lat=cost))
        self._update(idx, reads, writes)
        return idx

    def dma(self, queue, fn, reads=(), writes=(), key=None, nbytes=65536):
        if self.dead:
            return -1
        reads = self._norm(reads)
        writes = self._norm(writes)
        idx = len(self.ops)
        deps = self._deps_for(reads, writes)
        if queue in self.last_bar:
            deps.add(self.last_bar[queue])
        assert key is not None
        self.ops.append(dict(eng=queue, fn=fn, deps=deps, dma=key, idx=idx, seg=self.seg, cost=0.15,
                             lat=2.5 + nbytes / 60000.0))
        self._update(idx, reads, writes)
        return idx

    def barrier(self):
        if self.dead:
            return
        self.n_bar += 1
        for e in ENGS:
            idx = len(self.ops)
            self.ops.append(dict(eng=e, fn=None, deps=set(), dma=None, idx=idx, bar=self.n_bar, seg=self.seg, cost=0.0, lat=0.0))
            self.last_bar[e] = idx
        self.seg += 1
        self.state = {}

    def schedule(self):
        ops = self.ops
        n = len(ops)
        import os as _os
        WD = int(_os.environ.get("K_WDEF", str(self.WINDOW)))
        segw = {}
        for kv in _os.environ.get("K_WIN", "").split(","):
            if ":" in kv:
                a_, b_ = kv.split(":")
                segw[int(a_)] = int(b_)
        per_eng = {e: [] for e in ENGS}
        for o in ops:
            per_eng[o["eng"]].append(o["idx"])
        pos = {e: 0 for e in ENGS}
        free = {e: 0.0 for e in ENGS}
        sched = [False] * n
        finish = [0.0] * n
        dep_left = [0] * n
        users = [[] for _ in range(n)]
        ready_t = [0.0] * n
        for o in ops:
            i = o["idx"]
            dep_left[i] = len(o["deps"])
            for d in o["deps"]:
                users[d].append(i)
        nseg = self.seg + 1
        seg_left = [0] * (nseg + 1)
        seg_fin = [0.0] * (nseg + 1)
        for o in ops:
            if not o.get("bar"):
                seg_left[o["seg"]] += 1
        order = []
        remaining = n
        while remaining > 0:
            best = None
            for e in ENGS:
                lst = per_eng[e]
                p = pos[e]
                while p < len(lst) and sched[lst[p]]:
                    p += 1
                pos[e] = p
                cnt = 0
                q = p
                fe = free[e]
                W = segw.get(ops[lst[p]]["seg"], WD) if p < len(lst) else WD
                while q < len(lst) and cnt < W:
                    i = lst[q]
                    q += 1
                    if sched[i]:
                        continue
                    cnt += 1
                    o = ops[i]
                    if o.get("bar"):
                        if seg_left[o["seg"]] == 0:
                            st = max(fe, seg_fin[o["seg"]])
                            if best is None or (st, i) < (best[0], best[1]):
                                best = (st, i, e)
                        break
                    if dep_left[i] != 0:
                        continue
                    st = max(fe, ready_t[i])
                    if best is None or (st, i) < (best[0], best[1]):
                        best = (st, i, e)
                    if st <= fe:
                        break
            assert best is not None, "scheduler deadlock"
            st, i, e = best
            o = ops[i]
            sched[i] = True
            remaining -= 1
            free[e] = st + o["cost"]
            finish[i] = st + o["lat"]
            o["t"] = st
            order.append(i)
            if not o.get("bar"):
                sg = o["seg"]
                seg_left[sg] -= 1
                if finish[i] > seg_fin[sg]:
                    seg_fin[sg] = finish[i]
            for u in users[i]:
                dep_left[u] -= 1
                lat = 0.12 if ops[u]["eng"] != e else 0.05
                if ops[u]["eng"] == "pe" and e == "pe" and o["dma"] is None:
                    lat = 0.0
                t = finish[i] + lat
                if t > ready_t[u]:
                    ready_t[u] = t
        self.sim_time = max(finish) if finish else 0.0
        return order

    def emit(self, reorder=True):
        nc = self.nc
        ops = self.ops
        order = self.schedule() if reorder else list(range(len(ops)))
        needs_sig = [False] * len(ops)
        last_real = {e: None for e in ENGS}
        bar_snap = {}
        for i in order:
            o = ops[i]
            if o.get("bar"):
                if o["bar"] not in bar_snap:
                    bar_snap[o["bar"]] = dict(last_real)
                    for e2, li in last_real.items():
                        if li is not None:
                            needs_sig[li] = True
                continue
            for d in o["deps"]:
                needs_sig[d] = True
            if o["dma"] is None:
                last_real[o["eng"]] = i
        eng_sig = {e: 0 for e in ENGS}
        phys_count = []
        key_phys = {}
        key_sealed = {}
        free_phys = []
        waited = {e: {} for e in ENGS}
        sig_of = [None] * len(ops)
        plan = {}
        bar_dma_snap = {}
        for i in order:
            o = ops[i]
            e = o["eng"]
            waits = []
            w = waited[e]
            if o.get("bar"):
                bid = o["bar"]
                if bid not in bar_dma_snap:
                    bar_dma_snap[bid] = list(phys_count)
                    for k, p in key_phys.items():
                        free_phys.append(p)
                    key_phys = {}
                    key_sealed = {}
                for e2, li in bar_snap[bid].items():
                    if li is None:
                        continue
                    s_ = sig_of[li]
                    if s_ is None:
                        continue
                    kind, name, val = s_
                    if w.get((kind, name), 0) < val:
                        w[(kind, name)] = val
                        waits.append((kind, name, val))
                for p, cnt in enumerate(bar_dma_snap[bid]):
                    if cnt > 0 and w.get(("dma", p), 0) < cnt:
                        w[("dma", p)] = cnt
                        waits.append(("dma", p, cnt))
                plan[i] = (waits, None)
                continue
            for d in sorted(o["deps"]):
                od = ops[d]
                if od["dma"] is None and od["eng"] == "pe" and e == "pe" and o["dma"] is None:
                    continue
                s_ = sig_of[d]
                if s_ is None:
                    continue
                kind, name, val = s_
                if kind == "dma":
                    k = od["dma"]
                    if key_phys.get(k) == name:
                        val = max(val, phys_count[name])
                        key_sealed[k] = True
                if w.get((kind, name), 0) >= val:
                    continue
                w[(kind, name)] = val
                waits.append((kind, name, val))
            sig = None
            if o["dma"] is not None:
                k = o["dma"]
                if k not in key_phys:
                    if free_phys:
                        p = free_phys.pop(0)
                    else:
                        p = len(phys_count)
                        phys_count.append(0)
                    key_phys[k] = p
                    key_sealed[k] = False
                p = key_phys[k]
                if key_sealed[k] and phys_count[p] > 0:
                    if w.get(("dma", p), 0) < phys_count[p]:
                        w[("dma", p)] = phys_count[p]
                        waits.append(("dma", p, phys_count[p]))
                    key_sealed[k] = False
                phys_count[p] += 16
                sig = ("dma", p, phys_count[p])
            elif needs_sig[i]:
                eng_sig[e] += 1
                sig = ("eng", e, eng_sig[e])
            sig_of[i] = sig
            plan[i] = (waits, sig)
        self.max_sig = dict(eng_sig)
        self.n_dma_sems = len(phys_count)
        with contextlib.ExitStack() as es:
            sems = {}
            for e in ENGS:
                sems[("eng", e)] = es.enter_context(nc.semaphore("s_" + e))
            for p in range(len(phys_count)):
                sems[("dma", p)] = es.enter_context(nc.semaphore("d_%d" % p))
            block = es.enter_context(nc.Block())
            per_eng = {e: [] for e in ENGS}
            for i in order:
                per_eng[ops[i]["eng"]].append(i)
            final_waits = [(("dma", p), phys_count[p]) for p in range(len(phys_count))]
            final_eng = [(("eng", e), eng_sig[e]) for e in ENGS if eng_sig[e] > 0]

            def make(e):
                def body(engobj):
                    for i in per_eng[e]:
                        o = ops[i]
                        waits, sig = plan[i]
                        for (kind, name, val) in waits:
                            engobj.wait_ge(sems[(kind, name)], val)
                        if o["fn"] is None:
                            continue
                        ins = o["fn"](engobj)
                        if sig is not None:
                            kind, name, val = sig
                            ins.then_inc(sems[(kind, name)], 16 if kind == "dma" else 1)
                    if e == "sp":
                        for (sk, v) in final_waits:
                            engobj.wait_ge(sems[sk], v)
                        for (sk, v) in final_eng:
                            engobj.wait_ge(sems[sk], v)
                return body

            block.tensor(make("pe"))
            block.scalar(make("act"))
            block.vector(make("dve"))
            block.gpsimd(make("pool"))
            block.sync(make("sp"))


D = 1024
DI = 2048
NH = 32
NMEM = 256
RMS_EPS = 1e-6
Z0, X0, BC0, C0, DT0, QA0, CKV0, KR0, G10, G20 = 0, 2048, 4096, 4608, 5120, 5152, 5536, 5792, 5824, 6848
NIN = 7872
TWO_PI = 6.283185307179586
CW1 = 6.28125
CW2 = TWO_PI - 6.28125
NEG = -30000.0

WEIGHTS = [
    ("w_in", (1024, 7872)), ("w_ssd_out", (2048, 1024)), ("w_q_b", (384, 1536)), ("w_kv_b", (256, 2048)),
    ("w_mla_out", (1024, 1024)), ("w_o", (1024, 1024)), ("w_xq", (1024, 1024)), ("w_xkv", (1024, 2048)),
    ("w_xo", (1024, 1024)), ("w_router_group", (1024, 4)), ("w_router_expert", (1024, 32)),
    ("w_exp_gate", (32 * 1024, 256)), ("w_exp_up", (32 * 1024, 256)), ("w_exp_down", (32 * 256, 1024)),
]
VECS = [
    ("norm_mix", 1024), ("conv_b", 3072), ("dt_bias", 32), ("a_log", 32), ("d_skip", 32), ("ssd_norm", 2048),
    ("q_a_norm", 384), ("kv_a_norm", 256), ("norm_xattn", 1024), ("norm_mem", 1024), ("norm_moe", 1024),
    ("b_router_group", 4), ("b_router_expert", 32), ("norm_final", 1024),
]


class K:
    pass


def build(S, debug=False, phases=(1, 2), stop_at=None):
    NT = S // 512
    NJ = NT // 4
    nc = bass.Bass("TRN2", target_bir_lowering=False)
    P = Prog(nc)
    P.stop_at = stop_at

    def din(name, shape, dt=F32):
        return nc.dram_tensor(name, list(shape), dt, kind="ExternalInput").ap()

    def dscr(name, shape, dt):
        return nc.dram_tensor(name, list(shape), dt, kind=("ExternalOutput" if debug else "Internal")).ap()

    xb = din("xb", (S, D))
    xo = din("xo", (NJ, 515, D))
    posb = din("posb", (1, S), I32)
    poso = din("poso", (1, NJ * 512), I32)
    memb = din("memb", (NMEM, D))
    maskb_d = din("maskb", (128, 16, 512), BF16)
    ohq_d = din("ohq", (128, 4))
    c_identb = din("c_identb", (128, 128), BF16)
    c_identf = din("c_identf", (128, 128))
    c_tri = din("c_tri", (128, 128))
    c_onesf = din("c_onesf", (128, 128))
    c_maskneg = din("c_maskneg", (128, 128), BF16)
    c_selh = din("c_selh", (32, 32 * 128))
    c_invf = din("c_invf", (32, 2))
    wd = {n: din(n, shp) for n, shp in WEIGHTS}
    vd = {n: din(n, (1, ln)) for n, ln in VECS}
    conv_w_d = din("conv_w", (4, 3072))
    out_d = nc.dram_tensor("out", [NJ * 512, D], F32, kind="ExternalOutput").ap()

    wb = {n: nc.dram_tensor(n + "_b", list(shp), BF16, kind="Internal").ap() for n, shp in WEIGHTS}
    KT = dscr("KT", (16, 96, S), BF16)
    VS = dscr("VS", (4, S, 260), BF16)
    OSD = dscr("OSD", (NJ * 4, 128, 2048), BF16)

    es = contextlib.ExitStack()

    sb_cnt = [0]

    def sb(name, shape, dt, st=None):
        sb_cnt[0] += 1
        return (st or es).enter_context(nc.sbuf_tensor("s%d_%s" % (sb_cnt[0], name), list(shape), dt))

    def fsz(ap):
        n = 1
        for d_ in ap.shape[1:]:
            n *= int(d_)
        return n

    def nbytes_of(ap):
        n = 1
        for d_ in ap.shape:
            n *= int(d_)
        return n * (4 if ap.dtype in (F32, I32, U32) else 2)

    def DMA(q, out, in_, r, w, key, **kw):
        P.dma(q, lambda e: e.dma_start(out=out, in_=in_, **kw), reads=r, writes=w, key=key, nbytes=nbytes_of(out))

    def MM(out, lhsT, rhs, start, stop, r, w):
        n_ = fsz(rhs)
        c_ = max(n_, 64) / 2400.0 * (4.0 if lhsT.dtype == F32 else 1.0) + 0.03
        P.op("pe", lambda e: e.matmul(out, lhsT=lhsT, rhs=rhs, start=start, stop=stop), reads=r, writes=w, cost=c_)

    def TR(out, in_, ident, r, w):
        P.op("pe", lambda e: e.transpose(out=out, in_=in_, identity=ident), reads=r, writes=w, cost=0.09)

    def ACT(out, in_, func, r, w, bias=None, scale=None, accum=None):
        kw = {}
        if bias is not None:
            kw["bias"] = bias
        if scale is not None:
            kw["scale"] = scale
        if accum is not None:
            kw["accum_out"] = accum
        c_ = fsz(in_) / 1100.0 + 0.22 + (0.1 if accum is not None else 0.0)
        P.op("act", lambda e: e.activation(out=out, in_=in_, func=func, **kw), reads=r, writes=w, cost=c_)

    def vcost(eng, ap):
        return fsz(ap) / (900.0 if eng == "dve" else 500.0) + (0.12 if eng == "dve" else 0.25)

    def TT(eng, out, in0, in1, op, r, w):
        P.op(eng, lambda e: e.tensor_tensor(out=out, in0=in0, in1=in1, op=op), reads=r, writes=w, cost=vcost(eng, out))

    def TS(eng, out, in0, s1, s2, op0, op1, r, w):
        if op1 is None:
            P.op(eng, lambda e: e.tensor_scalar(out=out, in0=in0, scalar1=s1, scalar2=None, op0=op0), reads=r, writes=w, cost=vcost(eng, out))
        else:
            P.op(eng, lambda e: e.tensor_scalar(out=out, in0=in0, scalar1=s1, scalar2=s2, op0=op0, op1=op1), reads=r, writes=w, cost=vcost(eng, out))

    def STT(eng, out, in0, scalar, in1, op0, op1, r, w):
        P.op(eng, lambda e: e.scalar_tensor_tensor(out=out, in0=in0, scalar=scalar, in1=in1, op0=op0, op1=op1), reads=r, writes=w, cost=vcost(eng, out))

    def CP(eng, out, in_, r, w):
        P.op(eng, lambda e: e.tensor_copy(out=out, in_=in_), reads=r, writes=w, cost=vcost(eng, out))

    def MEMSET(eng, ap, val, w):
        P.op(eng, lambda e: e.memset(ap, val), writes=w, cost=vcost(eng, ap))

    def RECIP(out, in_, r, w):
        P.op("dve", lambda e: e.reciprocal(out=out, in_=in_), reads=r, writes=w, cost=vcost("dve", out))

    PP = [es.enter_context(nc.psum_tensor("PP%d" % i, [128, 1024], F32)) for i in range(4)]

    def bank(i):
        return PP[i // 2][:, (i % 2) * 512:(i % 2 + 1) * 512]

    def bankb(i):
        return PP[i // 2][:].bitcast(BF16)[:, (i % 2) * 1024:(i % 2 + 1) * 1024]

    def bn(i):
        return ("PP%d" % (i // 2), i % 2)

    identb = sb("identb", (128, 128), BF16)
    identf = sb("identf", (128, 128), F32)
    tri = sb("tri", (128, 128), F32)
    onesf = sb("onesf", (128, 128), F32)
    onesb = sb("onesb", (128, 128), BF16)
    maskneg = sb("maskneg", (128, 128), BF16)
    invf = sb("invf", (32, 2), F32)
    ohq = sb("ohq", (128, 4), F32)
    for t, d_, nm in ((identb, c_identb, "identb"), (identf, c_identf, "identf"), (tri, c_tri, "tri"),
                      (onesf, c_onesf, "onesf"), (maskneg, c_maskneg, "maskneg"),
                      (invf, c_invf, "invf"), (ohq, ohq_d, "ohq")):
        DMA("sp", t[:], d_[:, :], [], [nm], "c_" + nm)
    CP("dve", onesb[:], onesf[:], ["onesf"], ["onesb"])

    def pvec(name, n):
        t = sb("pv_" + name, (128, n // 128), F32)
        DMA("sp", t[:], vd[name].rearrange("o (k p) -> p (o k)", p=128), [], ["pv_" + name], "pv_" + name,
            allow_slow_non_contiguous=True)
        return t

    g_mix = pvec("norm_mix", 1024)
    g_ssd = pvec("ssd_norm", 2048)
    g_qa = pvec("q_a_norm", 384)
    g_kv = pvec("kv_a_norm", 256)
    g_xa = pvec("norm_xattn", 1024)
    g_mem = pvec("norm_mem", 1024)
    g_moe = pvec("norm_moe", 1024)
    cb_p = pvec("conv_b", 3072)
    cw_p = sb("cw_p", (128, 24, 4), F32)
    for k in range(4):
        DMA("sp", cw_p[:, :, k], conv_w_d[k:k + 1, :].rearrange("o (k p) -> p (o k)", p=128), [], [("cw_p", k)],
            "cw_p%d" % k, allow_slow_non_contiguous=True)

    def bvec(name, n):
        t = sb("bv_" + name, (128, n), F32)
        DMA("sp", t[:], vd[name].partition_broadcast(128), [], ["bv_" + name], "bv_" + name)
        return t

    dtb_bc = bvec("dt_bias", 32)
    alog_bc = bvec("a_log", 32)
    dsk_bc = bvec("d_skip", 32)
    a_bc = sb("a_bc", (128, 32), F32)
    ACT(a_bc[:], alog_bc[:], AF.Exp, ["bv_a_log"], ["a_bc"])
    TS("dve", a_bc[:], a_bc[:], -1.0, None, ALU.mult, None, ["a_bc"], ["a_bc"])

    def cast_weight(name, rows_first=None):
        src, dst = wd[name], wb[name]
        R, C = src.shape
        i = 0
        for r0 in range(0, R, 512):
            r1 = min(R, r0 + 512)
            for c0 in range(0, C, 2048):
                c1 = min(C, c0 + 2048)
                DMA("pool", dst[r0:r1, c0:c1], src[r0:r1, c0:c1], [], [("wb_" + name, i)], "cast_" + name)
                i += 1

    for n, _ in WEIGHTS:
        cast_weight(n)

    def rope_tables(pos_ap, cs1, cs2, tmp_i, tmp_f, tmp_a, nm):
        DMA("sp", tmp_i[:], pos_ap.partition_broadcast(32), [], [nm + "i"], nm + "pos")
        CP("dve", tmp_f[:], tmp_i[:], [nm + "i"], [nm + "f"])
        TS("dve", tmp_f[:], tmp_f[:], invf[:, 0:1], None, ALU.mult, None, [nm + "f", "invf"], [nm + "f"])
        for which, dst in ((0, cs2), (1, cs1)):
            src = tmp_f
            if which == 1:
                TS("dve", tmp_a[:], tmp_f[:], float(np.pi / 2), None, ALU.add, None, [nm + "f"], [nm + "a"])
                src = tmp_a
            srcn = nm + ("a" if which == 1 else "f")
            TS("dve", tmp_i[:], src[:], float(1.0 / TWO_PI), None, ALU.mult, None, [srcn], [nm + "i"])
            CP("dve", dst[:], tmp_i[:], [nm + "i"], [nm + "c%d" % which])
            STT("dve", tmp_a[:], dst[:], -CW1, src[:], ALU.mult, ALU.add, [nm + "c%d" % which, srcn], [nm + "a"])
            STT("dve", tmp_a[:], dst[:], -CW2, tmp_a[:], ALU.mult, ALU.add, [nm + "c%d" % which, nm + "a"], [nm + "a"])
            TS("dve", tmp_a[:], tmp_a[:], float(np.pi), float(-np.pi), ALU.min, ALU.max, [nm + "a"], [nm + "a"])
            ACT(dst[:], tmp_a[:], AF.Sin, [nm + "a"], [nm + "c%d" % which])
        TS("dve", cs2[:], cs2[:], invf[:, 1:2], None, ALU.mult, None, [nm + "c0", "invf"], [nm + "c0"])

    def norm_transpose(src_f32, ncols, gain_p, dstT, dst_cols, names_r, name_dst, scr, pbank, tag, gname):
        junk, ssq, rstd, xs = scr
        nk = ncols // 128
        ACT(junk[:, 0:ncols], src_f32, AF.Square, names_r, [tag + "junk", tag + "ssq"], accum=ssq[:])
        ACT(rstd[:], ssq[:], AF.Sqrt, [tag + "ssq", "eps_t"], [tag + "rstd"], bias=eps_t[:], scale=1.0 / ncols)
        RECIP(rstd[:], rstd[:], [tag + "rstd"], [tag + "rstd"])
        TS("dve", xs[:, 0:ncols], src_f32, rstd[:, 0:1], None, ALU.mult, None, names_r + [tag + "rstd"], [tag + "xs"])
        pv = bankb(pbank)
        for k in range(nk):
            TR(pv[:, k * 128:(k + 1) * 128], xs[:, k * 128:(k + 1) * 128], identb[:], [tag + "xs", "identb"], [bn(pbank)])
        TT("dve", dstT[:, :, dst_cols], pv[:, 0:ncols].rearrange("p (k t) -> p k t", k=nk),
           gain_p[:, 0:nk].unsqueeze(2).to_broadcast([128, nk, 128]), ALU.mult,
           [bn(pbank), gname], [name_dst])

    eps_t = sb("eps_t", (128, 1), F32)
    MEMSET("dve", eps_t[:], RMS_EPS, ["eps_t"])

    if 1 in phases:
        with contextlib.ExitStack() as s1:
            WxB = sb("WxB", (128, 8, 2560), BF16, s1)
            Wdt = sb("Wdt", (128, 8, 32), BF16, s1)
            Wckv = sb("Wckv", (128, 8, 288), BF16, s1)
            Wkr = sb("Wkr", (128, 8, 64), BF16, s1)
            wkn = sb("wkn", (128, 2, 1024), BF16, s1)
            wv = sb("wv", (128, 2, 1024), BF16, s1)
            win_v = wb["w_in"].rearrange("(k p) c -> p k c", p=128)
            DMA("sp", WxB[:], win_v[:, :, X0:X0 + 2560], ["wb_w_in"], ["WxB"], "WxB")
            DMA("sp", Wdt[:], win_v[:, :, DT0:DT0 + 32], ["wb_w_in"], ["Wdt"], "Wdt")
            DMA("sp", Wckv[:], win_v[:, :, CKV0:CKV0 + 288], ["wb_w_in"], ["Wckv"], "Wckv")
            DMA("sp", Wkr[:, :, 0:32], win_v[:, :, KR0:KR0 + 32], ["wb_w_in"], [("Wkr", 0)], "Wkr")
            DMA("sp", Wkr[:, :, 32:48], win_v[:, :, KR0 + 16:KR0 + 32], ["wb_w_in"], [("Wkr", 1)], "Wkr")
            DMA("sp", Wkr[:, :, 48:64], win_v[:, :, KR0:KR0 + 16], ["wb_w_in"], [("Wkr", 2)], "Wkr")
            wkv_v = wb["w_kv_b"].rearrange("(k p) (h e) -> p k h e", p=128, e=128)
            for kb in range(2):
                DMA("sp", wkn[:, kb, :].rearrange("p (h e) -> p h e", e=64), wkv_v[:, kb, :, 0:64], ["wb_w_kv_b"], [("wkn", kb)], "wkn")
                DMA("sp", wv[:, kb, :].rearrange("p (h e) -> p h e", e=64), wkv_v[:, kb, :, 64:128], ["wb_w_kv_b"], [("wv", kb)], "wv")

            xin = [sb("xin%d" % i, (128, 1024), F32, s1) for i in range(3)]
            junk = sb("junk1", (128, 1024), BF16, s1)
            ssq = sb("ssq1", (128, 1), F32, s1)
            rstd = sb("rstd1", (128, 1), F32, s1)
            xs = sb("xs1", (128, 1024), BF16, s1)
            uT = sb("uT1", (128, 8, 512), BF16, s1)
            rb = [sb("rb%d" % i, (128, 515), BF16, s1) for i in range(2)]
            hal = sb("hal", (128, 20, 3), BF16, s1)
            cacc = [sb("cacc%d" % i, (128, 512), F32, s1) for i in range(2)]
            xsT = sb("xsT1", (128, 20, 512), BF16, s1)
            dtr = sb("dtr1", (128, 32), F32, s1)
            dt_ = sb("dt1", (128, 32), F32, s1)
            adt = sb("adt1", (128, 32), F32, s1)
            acum = sb("acum1", (128, 32), F32, s1)
            tmp32 = sb("tmp32", (128, 32), F32, s1)
            dte = sb("dte1", (128, 32), F32, s1)
            cd = sb("cd1", (128, 32), F32, s1)
            wdt = sb("wdt1", (128, 32), F32, s1)
            xw = sb("xw1", (128, 2048), BF16, s1)
            Btm = sb("Btm1", (128, 512), BF16, s1)
            Sst = sb("Sst", (128, 2048), F32, s1)
            Sb = sb("Sb", (128, 2048), BF16, s1)
            OS = [sb("OS%d" % i, (128, 2048), BF16, s1) for i in range(4)]
            tmpb = sb("tmpb", (128, 512), F32, s1)
            junk2 = sb("junk2", (128, 256), F32, s1)
            ssq2 = sb("ssq2", (128, 1), F32, s1)
            rstd2 = sb("rstd2", (128, 1), F32, s1)
            cn = sb("cn1", (128, 256), BF16, s1)
            cnT = sb("cnT1", (128, 2, 512), BF16, s1)
            cs1 = sb("cs1_1", (32, 512), F32, s1)
            cs2 = sb("cs2_1", (32, 512), F32, s1)
            rt_i = sb("rt_i1", (32, 512), I32, s1)
            rt_f = sb("rt_f1", (32, 512), F32, s1)
            rt_a = sb("rt_a1", (32, 512), F32, s1)
            kpe = sb("kpe1", (32, 512), BF16, s1)
            kst = [sb("kst%d" % i, (128, 512), BF16, s1) for i in range(2)]
            vst = [sb("vst%d" % i, (128, 16, 65), BF16, s1) for i in range(2)]

            MEMSET("dve", hal[:], 0.0, ["hal"])
            MEMSET("dve", Sst[:], 0.0, ["Sst"])
            MEMSET("dve", Sb[:], 0.0, ["Sb"])
            for i in range(4):
                MEMSET("pool", OS[i][:], 0.0, ["OS%d" % i])
            for i in range(2):
                MEMSET("pool", vst[i][:], 1.0, ["vst%d" % i])

            for g in range(NT):
                rope_tables(posb[0:1, g * 512:(g + 1) * 512], cs1, cs2, rt_i, rt_f, rt_a, "rt1")
                for c in range(4):
                    xi = (g * 4 + c) % 3
                    xt = xin[xi]
                    xtn = "xin%d" % xi
                    DMA("sp", xt[:], xb[g * 512 + c * 128:g * 512 + (c + 1) * 128, :], [], [xtn], xtn)
                    norm_transpose(xt[:], 1024, g_mix, uT, slice(c * 128, (c + 1) * 128), [xtn], ("uT1", c),
                                   (junk, ssq, rstd, xs), 0, "n1", "pv_norm_mix")
                for blk in range(20):
                    pb_ = 2 + (blk % 2)
                    for k in range(8):
                        MM(bank(pb_), WxB[:, k, blk * 128:(blk + 1) * 128], uT[:, k, :], k == 0, k == 7,
                           ["WxB", "uT1"], [bn(pb_)])
                    r_ = rb[blk % 2]
                    rn = "rb%d" % (blk % 2)
                    CP("dve", r_[:, 0:3], hal[:, blk, :], [("hal", blk)], [(rn, 0)])
                    ACT(r_[:, 3:515], bank(pb_), AF.Copy, [bn(pb_)], [(rn, 1)])
                    CP("dve", hal[:, blk, :], r_[:, 512:515], [(rn, 1)], [("hal", blk)])
                    ca_ = cacc[blk % 2]
                    can = "cacc%d" % (blk % 2)
                    TS("dve", ca_[:], r_[:, 0:512], cw_p[:, blk, 0:1], cb_p[:, blk:blk + 1], ALU.mult, ALU.add, [rn, "cw_p", "pv_conv_b"], [can])
                    for k in range(1, 4):
                        STT("dve", ca_[:], r_[:, k:k + 512], cw_p[:, blk, k:k + 1], ca_[:], ALU.mult, ALU.add, [rn, "cw_p", can], [can])
                    ACT(xsT[:, blk, :], ca_[:], AF.Silu, [can], [("xsT1", blk)])
                for v_ in range(2):
                    for k in range(8):
                        MM(bank(2 + v_)[0:32, :], Wkr[:, k, v_ * 32:(v_ + 1) * 32], uT[:, k, :], k == 0, k == 7,
                           ["Wkr", "uT1"], [bn(2 + v_)])
                TT("dve", rt_f[:], bank(2)[0:32, :], cs1[:], ALU.mult, [bn(2), "rt1c1"], ["rt1f"])
                TT("dve", rt_a[:], bank(3)[0:32, :], cs2[:], ALU.mult, [bn(3), "rt1c0"], ["rt1a"])
                TT("dve", kpe[:], rt_f[:], rt_a[:], ALU.add, ["rt1f", "rt1a"], ["kpe1"])
                for h in range(16):
                    DMA("sp", KT[h, 0:32, g * 512:(g + 1) * 512], kpe[:], ["kpe1"], [("KT", g)], "KTpe")
                for c in range(4):
                    cc = slice(c * 128, (c + 1) * 128)
                    b7 = bank(7)
                    for k in range(8):
                        MM(b7[:, 0:32], uT[:, k, cc], Wdt[:, k, :], k == 0, k == 7, ["uT1", "Wdt"], [bn(7)])
                    TT("dve", dtr[:], b7[:, 0:32], dtb_bc[:], ALU.add, [bn(7), "bv_dt_bias"], ["dtr1"])
                    ACT(dtr[:], dtr[:], AF.Exp, ["dtr1"], ["dtr1"])
                    ACT(dt_[:], dtr[:], AF.Ln, ["dtr1"], ["dt1"], bias=1.0)
                    TT("dve", adt[:], dt_[:], a_bc[:], ALU.mult, ["dt1", "a_bc"], ["adt1"])
                    MM(b7[:, 32:64], tri[:], adt[:], True, True, ["tri", "adt1"], [bn(7)])
                    MM(b7[:, 64:96], onesf[:], adt[:], True, True, ["onesf", "adt1"], [bn(7)])
                    ACT(acum[:], b7[:, 32:64], AF.Copy, [bn(7)], ["acum1"])
                    TT("dve", tmp32[:], b7[:, 64:96], acum[:], ALU.subtract, [bn(7), "acum1"], ["tmp32"])
                    ACT(dte[:], tmp32[:], AF.Exp, ["tmp32"], ["dte1"])
                    ACT(cd[:], b7[:, 64:96], AF.Exp, [bn(7)], ["cd1"])
                    TT("dve", wdt[:], dt_[:], dte[:], ALU.mult, ["dt1", "dte1"], ["wdt1"])
                    pxv = PP[2][:].bitcast(BF16)
                    for blk in range(16):
                        TR(pxv[:, blk * 128:(blk + 1) * 128], xsT[:, blk, cc], identb[:], [("xsT1", blk), "identb"],
                           [("PP2", blk // 8)])
                    TT("dve", xw[:].rearrange("p (h e) -> p h e", e=64), pxv.rearrange("p (h e) -> p h e", e=64),
                       wdt[:, :].unsqueeze(2).to_broadcast([128, 32, 64]), ALU.mult, ["PP2", "wdt1"], ["xw1"])
                    pbv = bankb(0)
                    for blk in range(4):
                        TR(pbv[:, blk * 128:(blk + 1) * 128], xsT[:, 16 + blk, cc], identb[:], [("xsT1", 16 + blk), "identb"],
                           [bn(0)])
                    ACT(Btm[:], pbv[:, 0:512], AF.Copy, [bn(0)], ["Btm1"])
                    ci = c
                    if g % 4 == 0:
                        MEMSET("pool", OS[ci][:], 0.0, ["OS%d" % ci])
                    STT("dve", OS[ci][:], Sb[:], ohq[:, (g % 4):(g % 4) + 1], OS[ci][:], ALU.mult, ALU.add,
                        ["Sb", "ohq", "OS%d" % ci], ["OS%d" % ci])
                    for g4 in range(4):
                        gs = slice(g4 * 512, (g4 + 1) * 512)
                        MM(bank(6), Btm[:, g4 * 128:(g4 + 1) * 128], xw[:, gs], True, True, ["Btm1", "xw1"], [bn(6)])
                        TT("pool", Sst[:, gs].rearrange("p (h e) -> p h e", e=64), Sst[:, gs].rearrange("p (h e) -> p h e", e=64),
                           cd[:, g4 * 8:(g4 + 1) * 8].unsqueeze(2).to_broadcast([128, 8, 64]), ALU.mult,
                           [("Sst", g4), "cd1"], [("Sst", g4)])
                        TT("dve", Sst[:, gs], Sst[:, gs], bank(6), ALU.add, [("Sst", g4), bn(6)], [("Sst", g4)])
                        ACT(Sb[:, gs], Sst[:, gs], AF.Copy, [("Sst", g4)], [("Sb", g4)])
                    for k in range(8):
                        MM(b7[:, 96:384], uT[:, k, cc], Wckv[:, k, :], k == 0, k == 7, ["uT1", "Wckv"], [bn(7)])
                    ACT(junk2[:], b7[:, 96:352], AF.Square, [bn(7)], ["junk2", "ssq2"], accum=ssq2[:])
                    ACT(rstd2[:], ssq2[:], AF.Sqrt, ["ssq2"], ["rstd2"], bias=eps_t[:], scale=1.0 / 256)
                    RECIP(rstd2[:], rstd2[:], ["rstd2"], ["rstd2"])
                    TS("dve", cn[:], b7[:, 96:352], rstd2[:, 0:1], None, ALU.mult, None, [bn(7), "rstd2"], ["cn1"])
                    pcv = bankb(1)
                    for k in range(2):
                        TR(pcv[:, k * 128:(k + 1) * 128], cn[:, k * 128:(k + 1) * 128], identb[:], ["cn1", "identb"],
                           [bn(1)])
                    TT("dve", cnT[:, :, cc], pcv[:, 0:256].rearrange("p (k t) -> p k t", k=2),
                       g_kv[:, 0:2].unsqueeze(2).to_broadcast([128, 2, 128]), ALU.mult, [bn(1), "pv_kv_a_norm"], [("cnT1", c)])
                    for half in range(2):
                        for kb in range(2):
                            MM(bank(2 + half), cnT[:, kb, cc], wv[:, kb, half * 512:(half + 1) * 512], kb == 0, kb == 1,
                               [("cnT1", c), "wv"], [bn(2 + half)])
                    vs_ = vst[c % 2]
                    vn = "vst%d" % (c % 2)
                    CP("dve", vs_[:, :, 0:64], PP[1][:].rearrange("p (h e) -> p h e", e=64), ["PP1"], [vn])
                    t0 = g * 512 + c * 128
                    DMA("sp", VS[:, t0:t0 + 128, :].rearrange("g t e -> t g e"), vs_[:].rearrange("p (g h) e -> p g (h e)", g=4),
                        [vn], [("VS", g)], "VSw")
                for hp in range(8):
                    for kb in range(2):
                        MM(bank(6), wkn[:, kb, hp * 128:(hp + 1) * 128], cnT[:, kb, :], kb == 0, kb == 1, ["wkn", "cnT1"], [bn(6)])
                    ks_ = kst[hp % 2]
                    kn = "kst%d" % (hp % 2)
                    ACT(ks_[:], bank(6), AF.Copy, [bn(6)], [kn])
                    for hh in range(2):
                        DMA("sp", KT[hp * 2 + hh, 32:96, g * 512:(g + 1) * 512], ks_[hh * 64:(hh + 1) * 64, :], [kn], [("KT", g)], "KTn")
                if g % 4 == 3:
                    j = g // 4
                    for ci in range(4):
                        DMA("sp", OSD[j * 4 + ci, :, :], OS[ci][:], ["OS%d" % ci], [("OSD", j)], "OSDw")
        P.barrier()

    if 2 in phases:
        dbg = {}
        if debug:
            for nm in ("dbg_h1", "dbg_h2", "dbg_h3"):
                dbg[nm] = nc.dram_tensor(nm, [NJ * 512, D], F32, kind="ExternalOutput").ap()
            dbg["dbg_ynT"] = nc.dram_tensor("dbg_ynT", [NJ, 128, 16 * 512], BF16, kind="ExternalOutput").ap()
            dbg["dbg_oT"] = nc.dram_tensor("dbg_oT", [NJ, 64, 16 * 512], BF16, kind="ExternalOutput").ap()
            dbg["dbg_QT"] = nc.dram_tensor("dbg_QT", [NJ, 96, 16 * 512], BF16, kind="ExternalOutput").ap()
            dbg["dbg_comb"] = nc.dram_tensor("dbg_comb", [NJ * 512, 32], F32, kind="ExternalOutput").ap()
        KXD = nc.dram_tensor("KXD", [128, 8 * 256], BF16, kind="Internal").ap()
        VXD = nc.dram_tensor("VXD", [128, 2 * 1024], BF16, kind="Internal").ap()
        DSKD = nc.dram_tensor("DSKD", [128, 32 * 128], BF16, kind="Internal").ap()
        win_v = wb["w_in"].rearrange("(k p) c -> p k c", p=128)
        SCALE = float(96 ** -0.5)
        BIG = 10000.0
        with contextlib.ExitStack() as s2:
            with contextlib.ExitStack() as s0:
                mem_t = sb("mem_t", (128, 1024), F32, s0)
                junk = sb("junk0", (128, 1024), BF16, s0)
                ssq = sb("ssq0", (128, 1), F32, s0)
                rstd = sb("rstd0", (128, 1), F32, s0)
                xs = sb("xs0", (128, 1024), BF16, s0)
                memnT = sb("memnT", (128, 8, 256), BF16, s0)
                Wxkv = sb("Wxkv", (128, 8, 2048), BF16, s0)
                kx_st = sb("kx_st", (128, 8, 256), BF16, s0)
                vx_st = sb("vx_st", (128, 2, 1024), BF16, s0)
                dsk_st = sb("dsk_st", (128, 32, 128), BF16, s0)
                DMA("sp", Wxkv[:], wb["w_xkv"].rearrange("(k p) c -> p k c", p=128), ["wb_w_xkv"], ["Wxkv"], "Wxkv")
                for mb in range(2):
                    DMA("sp", mem_t[:], memb[mb * 128:(mb + 1) * 128, :], [], ["mem_t"], "mem_t")
                    norm_transpose(mem_t[:], 1024, g_mem, memnT, slice(mb * 128, (mb + 1) * 128), ["mem_t"], ("memnT", mb),
                                   (junk, ssq, rstd, xs), 0, "n0", "pv_norm_mem")
                for blk in range(8):
                    pb_ = 2 + blk % 2
                    for k in range(8):
                        MM(bank(pb_)[:, 0:256], Wxkv[:, k, blk * 128:(blk + 1) * 128], memnT[:, k, :], k == 0, k == 7, ["Wxkv", "memnT"], [bn(pb_)])
                    ACT(kx_st[:, blk, :], bank(pb_)[:, 0:256], AF.Copy, [bn(pb_)], [("kx_st", blk)])
                for mb in range(2):
                    for half in range(2):
                        pb_ = 4 + half
                        for k in range(8):
                            MM(bank(pb_), memnT[:, k, mb * 128:(mb + 1) * 128], Wxkv[:, k, 1024 + half * 512:1024 + (half + 1) * 512],
                               k == 0, k == 7, ["Wxkv", "memnT"], [bn(pb_)])
                        ACT(vx_st[:, mb, half * 512:(half + 1) * 512], bank(pb_), AF.Copy, [bn(pb_)], [("vx_st", mb * 2 + half)])
                for h in range(32):
                    TS("dve" if h % 2 == 0 else "pool", dsk_st[:, h, :], identf[:], dsk_bc[:, h:h + 1], None, ALU.mult, None,
                       ["identf", "bv_d_skip"], [("dsk_st", h)])
                DMA("sp", KXD[:, :], kx_st[:].rearrange("p a b -> p (a b)"), ["kx_st"], ["KXD"], "KXD")
                DMA("sp", VXD[:, :], vx_st[:].rearrange("p a b -> p (a b)"), ["vx_st"], ["VXD"], "VXD")
                DMA("sp", DSKD[:, :], dsk_st[:].rearrange("p a b -> p (a b)"), ["dsk_st"], ["DSKD"], "DSKD")
            P.barrier()
            P.checkpoint("S0")

            Wqs = sb("Wqs", (128, 3, 16, 32), BF16, s2)
            wq_v = wb["w_q_b"].rearrange("(k p) (h e) -> p k h e", p=128, e=96)
            for kb in range(3):
                DMA("sp", Wqs[:, kb, :, 0:16], wq_v[:, kb, :, 80:96], ["wb_w_q_b"], [("Wqs", kb)], "Wqs")
                DMA("sp", Wqs[:, kb, :, 16:32], wq_v[:, kb, :, 64:80], ["wb_w_q_b"], [("Wqs", kb)], "Wqs")
            rb_bc = sb("rb_bc", (128, 36), F32, s2)
            DMA("sp", rb_bc[:, 0:4], vd["b_router_group"].partition_broadcast(128), [], [("rb_bc", 0)], "rb_bc")
            DMA("sp", rb_bc[:, 4:36], vd["b_router_expert"].partition_broadcast(128), [], [("rb_bc", 1)], "rb_bc")
            Wr = sb("Wr", (128, 8, 36), BF16, s2)
            DMA("sp", Wr[:, :, 0:4], wb["w_router_group"].rearrange("(k p) c -> p k c", p=128), ["wb_w_router_group"], [("Wr", 0)], "Wr")
            DMA("sp", Wr[:, :, 4:36], wb["w_router_expert"].rearrange("(k p) c -> p k c", p=128), ["wb_w_router_expert"], [("Wr", 1)], "Wr")
            hT = sb("hT", (128, 4, 1024), F32, s2)
            uT = sb("uT2", (128, 8, 512), BF16, s2)
            t1T = sb("t1T", (128, 8, 512), BF16, s2)
            Wst = [None, None]
            wst_gen = [0]

            def alloc_wst(scope):
                wst_gen[0] += 1
                for i in range(2):
                    Wst[i] = sb("Wst%d_%d" % (i, wst_gen[0]), (128, 8, 512), BF16, scope)
            junk = sb("junk2p", (128, 1024), BF16, s2)
            ssq = sb("ssq2p", (128, 1), F32, s2)
            rstd = sb("rstd2p", (128, 1), F32, s2)
            xs = sb("xs2p", (128, 1024), BF16, s2)
            scr = (junk, ssq, rstd, xs)
            wst_i = [0]

            def load_wst(c0, ncols):
                i = wst_i[0] % 2
                wst_i[0] += 1
                DMA("sp", Wst[i][:, :, 0:ncols], win_v[:, :, c0:c0 + ncols], ["wb_w_in"], ["Wst%d" % i], "Wst%d" % i)
                return Wst[i], "Wst%d" % i

            P.checkpoint("P0")
            for j in range(NJ):
                KTN = 4 * j + 4
                DMA("sp", hT[:], xo[j, 3:515, :].rearrange("(c p) d -> p c d", p=128), [], ["hT"], "hT")
                for c in range(4):
                    norm_transpose(hT[:, c, :], 1024, g_mix, uT, slice(c * 128, (c + 1) * 128), [("hT", c)], ("uT2", c), scr, 0, "n2", "pv_norm_mix")
                with contextlib.ExitStack() as sA:
                    xsT2 = sb("xsT2", (128, 24, 512), BF16, sA)
                    sz = sb("sz", (128, 4, 2048), BF16, sA)
                    ynT = sb("ynT", (128, 16, 512), BF16, sA)
                    dt_all = sb("dt_all", (128, 4, 32), F32, sA)
                    ac_all = sb("ac_all", (128, 4, 32), F32, sA)
                    ea_all = sb("ea_all", (128, 4, 32), F32, sA)
                    acT = sb("acT", (32, 4, 128), F32, sA)
                    nacT = sb("nacT", (32, 4, 128), F32, sA)
                    with contextlib.ExitStack() as sA1:
                        alloc_wst(sA1)
                        xh = sb("xh", (3, 1024), F32, sA1)
                        xhs = sb("xhs", (3, 1024), BF16, sA1)
                        hj = sb("hj", (3, 1024), BF16, sA1)
                        hss = sb("hss", (3, 1), F32, sA1)
                        hrs = sb("hrs", (3, 1), F32, sA1)
                        uTh = sb("uTh", (128, 8, 4), BF16, sA1)
                        rb = [sb("rb2_%d" % i, (128, 515), BF16, sA1) for i in range(2)]
                        cacc2 = [sb("cacc2_%d" % i, (128, 512), F32, sA1) for i in range(2)]
                        Wdt2 = sb("Wdt2", (128, 8, 32), BF16, sA1)
                        dtr = sb("dtr2", (128, 32), F32, sA1)
                        adt = sb("adt2", (128, 32), F32, sA1)
                        DMA("sp", Wdt2[:], win_v[:, :, DT0:DT0 + 32], ["wb_w_in"], ["Wdt2"], "Wdt2")
                        DMA("sp", xh[:], xo[j, 0:3, :], [], ["xh"], "xh")
                        ACT(hj[:], xh[:], AF.Square, ["xh"], ["hj", "hss"], accum=hss[:])
                        ACT(hrs[:], hss[:], AF.Sqrt, ["hss"], ["hrs"], bias=eps_t[0:3, :], scale=1.0 / 1024)
                        RECIP(hrs[:], hrs[:], ["hrs"], ["hrs"])
                        TS("dve", xhs[:], xh[:], hrs[:, 0:1], None, ALU.mult, None, ["xh", "hrs"], ["xhs"])
                        pv = bankb(1)
                        for k in range(8):
                            TR(pv[:, k * 4:k * 4 + 3], xhs[:, k * 128:(k + 1) * 128], identb[0:3, 0:3], ["xhs", "identb"], [bn(1)])
                        TT("dve", uTh[:, :, 0:3], pv[:, 0:32].rearrange("p (k t) -> p k t", k=8)[:, :, 0:3],
                           g_mix[:, 0:8].unsqueeze(2).to_broadcast([128, 8, 3]), ALU.mult, [bn(1), "pv_norm_mix"], ["uTh"])
                        P.checkpoint("A1h")
                        for unit in range(6):
                            W_, wn = load_wst(X0 + unit * 512, 512)
                            for bi in range(4):
                                blk = unit * 4 + bi
                                pb_ = 2 + (blk % 2)
                                for k in range(8):
                                    MM(bank(pb_), W_[:, k, bi * 128:(bi + 1) * 128], uT[:, k, :], k == 0, k == 7, [wn, "uT2"], [bn(pb_)])
                                for k in range(8):
                                    MM(bank(1)[:, 0:3], W_[:, k, bi * 128:(bi + 1) * 128], uTh[:, k, 0:3], k == 0, k == 7, [wn, "uTh"], [bn(1)])
                                r_ = rb[blk % 2]
                                rn = "rb2_%d" % (blk % 2)
                                CP("dve", r_[:, 0:3], bank(1)[:, 0:3], [bn(1)], [(rn, 0)])
                                ACT(r_[:, 3:515], bank(pb_), AF.Copy, [bn(pb_)], [(rn, 1)])
                                ca_ = cacc2[blk % 2]
                                can = "cacc2_%d" % (blk % 2)
                                TS("dve", ca_[:], r_[:, 0:512], cw_p[:, blk, 0:1], cb_p[:, blk:blk + 1], ALU.mult, ALU.add, [rn, "cw_p", "pv_conv_b"], [can])
                                for k in range(1, 4):
                                    STT("dve", ca_[:], r_[:, k:k + 512], cw_p[:, blk, k:k + 1], ca_[:], ALU.mult, ALU.add, [rn, "cw_p", can], [can])
                                ACT(xsT2[:, blk, :], ca_[:], AF.Silu, [can], [("xsT2", blk)])
                        P.checkpoint("A1x")
                        for zb in range(4):
                            W_, wn = load_wst(Z0 + zb * 512, 512)
                            for c in range(4):
                                pb_ = 5 + (c % 2)
                                for k in range(8):
                                    MM(bank(pb_), uT[:, k, c * 128:(c + 1) * 128], W_[:, k, :], k == 0, k == 7, [wn, "uT2"], [bn(pb_)])
                                ACT(sz[:, c, zb * 512:(zb + 1) * 512], bank(pb_), AF.Silu, [bn(pb_)], [("sz", c)])
                        P.checkpoint("A1z")
                        b7 = bank(7)
                        for c in range(4):
                            cc = slice(c * 128, (c + 1) * 128)
                            for k in range(8):
                                MM(b7[:, 0:32], uT[:, k, cc], Wdt2[:, k, :], k == 0, k == 7, ["uT2", "Wdt2"], [bn(7)])
                            TT("dve", dtr[:], b7[:, 0:32], dtb_bc[:], ALU.add, [bn(7), "bv_dt_bias"], ["dtr2"])
                            ACT(dtr[:], dtr[:], AF.Exp, ["dtr2"], ["dtr2"])
                            ACT(dt_all[:, c, :], dtr[:], AF.Ln, ["dtr2"], [("dt_all", c)], bias=1.0)
                            TT("dve", adt[:], dt_all[:, c, :], a_bc[:], ALU.mult, [("dt_all", c), "a_bc"], ["adt2"])
                            MM(b7[:, 32:64], tri[:], adt[:], True, True, ["tri", "adt2"], [bn(7)])
                            ACT(ac_all[:, c, :], b7[:, 32:64], AF.Copy, [bn(7)], [("ac_all", c)])
                            ACT(ea_all[:, c, :], b7[:, 32:64], AF.Exp, [bn(7)], [("ea_all", c)])
                            TR(b7[0:32, 128:256], ac_all[:, c, :], identf[:], [("ac_all", c), "identf"], [bn(7)])
                            ACT(acT[:, c, :], b7[0:32, 128:256], AF.Copy, [bn(7)], [("acT", c)])
                            TS("dve", nacT[:, c, :], acT[:, c, :], -1.0, None, ALU.mult, None, [("acT", c)], [("nacT", c)])
                    P.barrier()
                    P.checkpoint("A1")
                    with contextlib.ExitStack() as sA2:
                        selh = sb("selh", (32, 32 * 128), F32, sA2)
                        dskI = sb("dskI", (128, 32 * 128), BF16, sA2)
                        DMA("sp", selh[:], c_selh[:, :], [], ["selh"], "selh")
                        DMA("sp", dskI[:], DSKD[:, :], ["DSKD"], ["dskI"], "dskI")
                        x_tm = sb("x_tm", (128, 2048), BF16, sA2)
                        xdt = sb("xdt", (128, 2048), BF16, sA2)
                        OSs = [sb("OSs%d" % i, (128, 2048), BF16, sA2) for i in range(2)]
                        CBT = sb("CBT", (128, 4, 128), BF16, sA2)
                        E4 = [sb("E4_%d" % i, (128, 4, 128), BF16, sA2) for i in range(2)]
                        M4 = [sb("M4_%d" % i, (128, 4, 128), BF16, sA2) for i in range(2)]
                        tsb = [sb("tsb%d" % i, (128, 512), F32, sA2) for i in range(2)]
                        ssqg = sb("ssqg", (128, 4), F32, sA2)
                        rsg = sb("rsg", (128, 4), F32, sA2)
                        jg = sb("jg", (128, 512), BF16, sA2)
                        yn = sb("yn", (128, 2048), BF16, sA2)
                        for c in range(4):
                            cc = slice(c * 128, (c + 1) * 128)
                            os_ = OSs[c % 2]
                            osn = "OSs%d" % (c % 2)
                            DMA("sp", os_[:], OSD[j * 4 + c, :, :], ["OSD"], [osn], osn)
                            pxv = PP[2][:].bitcast(BF16)
                            for blk in range(16):
                                TR(pxv[:, blk * 128:(blk + 1) * 128], xsT2[:, blk, cc], identb[:], [("xsT2", blk), "identb"], [("PP2", blk // 8)])
                            ACT(x_tm[:], pxv, AF.Copy, ["PP2"], ["x_tm"])
                            TT("dve", xdt[:].rearrange("p (h e) -> p h e", e=64), pxv.rearrange("p (h e) -> p h e", e=64),
                               dt_all[:, c, :].unsqueeze(2).to_broadcast([128, 32, 64]), ALU.mult, ["PP2", ("dt_all", c)], ["xdt"])
                            for g4 in range(4):
                                MM(bank(6)[:, g4 * 128:(g4 + 1) * 128], xsT2[:, 16 + g4, cc], xsT2[:, 20 + g4, cc], True, True,
                                   [("xsT2", 16 + g4), ("xsT2", 20 + g4)], [bn(6)])
                            ACT(CBT[:].rearrange("p a b -> p (a b)"), bank(6), AF.Copy, [bn(6)], ["CBT"])
                            for g4 in range(4):
                                gs = slice(g4 * 512, (g4 + 1) * 512)
                                yb = 2 + (g4 % 2)
                                for hq2 in range(2):
                                    hq = g4 * 2 + hq2
                                    sbk = hq % 2
                                    for hh in range(4):
                                        h = hq * 4 + hh
                                        reg = bank(sbk)[:, hh * 128:(hh + 1) * 128]
                                        MM(reg, selh[:, h * 128:(h + 1) * 128], acT[:, c, :], True, False, ["selh", ("acT", c)], [bn(sbk)])
                                        MM(reg, nacT[:, c, :], selh[:, h * 128:(h + 1) * 128], False, False, ["selh", ("nacT", c)], [bn(sbk)])
                                        MM(reg, identb[:], maskneg[:], False, True, ["identb", "maskneg"], [bn(sbk)])
                                    e4 = E4[hq % 2]
                                    en = "E4_%d" % (hq % 2)
                                    m4 = M4[hq % 2]
                                    mn = "M4_%d" % (hq % 2)
                                    ACT(e4[:].rearrange("p a b -> p (a b)"), bank(sbk), AF.Exp, [bn(sbk)], [en])
                                    TT("dve", m4[:], e4[:], CBT[:, g4:g4 + 1, :].to_broadcast([128, 4, 128]), ALU.mult, [en, "CBT"], [mn])
                                    for hh in range(4):
                                        h = hq * 4 + hh
                                        yreg = bank(yb)[:, (h % 8) * 64:(h % 8 + 1) * 64]
                                        MM(yreg, m4[:, hh, :], xdt[:, h * 64:(h + 1) * 64], True, False, [mn, "xdt"], [bn(yb)])
                                        MM(yreg, dskI[:, h * 128:(h + 1) * 128], x_tm[:, h * 64:(h + 1) * 64], False, True, ["dskI", "x_tm"], [bn(yb)])
                                MM(bank(7), xsT2[:, 20 + g4, cc], os_[:, gs], True, True, [("xsT2", 20 + g4), osn], [bn(7)])
                                t_ = tsb[g4 % 2]
                                tn = "tsb%d" % (g4 % 2)
                                TT("dve", t_[:].rearrange("p (h e) -> p h e", e=64), bank(7).rearrange("p (h e) -> p h e", e=64),
                                   ea_all[:, c, g4 * 8:(g4 + 1) * 8].unsqueeze(2).to_broadcast([128, 8, 64]), ALU.mult,
                                   [bn(7), ("ea_all", c)], [tn])
                                TT("dve", t_[:], t_[:], bank(yb), ALU.add, [tn, bn(yb)], [tn])
                                TT("dve", t_[:], t_[:], sz[:, c, gs], ALU.mult, [tn, ("sz", c)], [tn])
                                ACT(jg[:], t_[:], AF.Square, [tn], ["jg", ("ssqg", g4)], accum=ssqg[:, g4:g4 + 1])
                                ACT(rsg[:, g4:g4 + 1], ssqg[:, g4:g4 + 1], AF.Sqrt, [("ssqg", g4)], [("rsg", g4)], bias=eps_t[:], scale=1.0 / 512)
                                RECIP(rsg[:, g4:g4 + 1], rsg[:, g4:g4 + 1], [("rsg", g4)], [("rsg", g4)])
                                ACT(yn[:, gs], t_[:], AF.Copy, [tn, ("rsg", g4)], [("yn", g4)], scale=rsg[:, g4:g4 + 1])
                            for blk in range(16):
                                TR(pxv[:, blk * 128:(blk + 1) * 128], yn[:, blk * 128:(blk + 1) * 128], identb[:], ["yn", "identb"], [("PP2", blk // 8)])
                            TT("dve", ynT[:, :, cc], pxv.rearrange("p (k t) -> p k t", k=16),
                               g_ssd[:, 0:16].unsqueeze(2).to_broadcast([128, 16, 128]), ALU.mult, ["PP2", "pv_ssd_norm"], [("ynT", c)])
                    P.barrier()
                    P.checkpoint("A2")
                    if debug:
                        DMA("sp", dbg["dbg_ynT"][j, :, :], ynT[:].rearrange("p a b -> p (a b)"), ["ynT"], [], "dbg_ynT")
                    with contextlib.ExitStack() as sA3:
                        alloc_wst(sA3)
                        Wso = sb("Wso", (128, 16, 1024), BF16, sA3)
                        sg = [sb("sg%d" % i, (128, 512), F32, sA3) for i in range(2)]
                        DMA("sp", Wso[:], wb["w_ssd_out"].rearrange("(k p) c -> p k c", p=128), ["wb_w_ssd_out"], ["Wso"], "Wso")
                        for unit in range(2):
                            W_, wn = load_wst(G10 + unit * 512, 512)
                            for bi in range(4):
                                cb_ = unit * 4 + bi
                                yb = 2 + cb_ % 2
                                gb = 5 + cb_ % 2
                                for kb in range(16):
                                    MM(bank(yb), Wso[:, kb, cb_ * 128:(cb_ + 1) * 128], ynT[:, kb, :], kb == 0, kb == 15, ["Wso", "ynT"], [bn(yb)])
                                for k in range(8):
                                    MM(bank(gb), W_[:, k, bi * 128:(bi + 1) * 128], uT[:, k, :], k == 0, k == 7, [wn, "uT2"], [bn(gb)])
                                s_ = sg[cb_ % 2]
                                sn = "sg%d" % (cb_ % 2)
                                ACT(s_[:], bank(gb), AF.Sigmoid, [bn(gb)], [sn])
                                TT("dve", t1T[:, cb_, :], s_[:], bank(yb), ALU.mult, [sn, bn(yb)], [("t1T", cb_)])
                P.barrier()
                P.checkpoint("A3")
                with contextlib.ExitStack() as sBC:
                    oT = sb("oT", (64, 16, 512), BF16, sBC)
                    with contextlib.ExitStack() as sB:
                        QT = sb("QT", (96, 16, 512), BF16, sB)
                        maskb = sb("maskb", (128, 16, 512), BF16, sB)
                        DMA("sp", maskb[:], maskb_d[:, :, :], [], ["maskb"], "maskb")
                        with contextlib.ExitStack() as sB1:
                            alloc_wst(sB1)
                            qnT = sb("qnT", (128, 3, 512), BF16, sB1)
                            Wqb = sb("Wqb", (128, 3, 1536), BF16, sB1)
                            cs1 = sb("cs1o", (32, 512), F32, sB1)
                            cs2 = sb("cs2o", (32, 512), F32, sB1)
                            rt_i = sb("rt_i2", (32, 512), I32, sB1)
                            rt_f = sb("rt_f2", (32, 512), F32, sB1)
                            rt_a = sb("rt_a2", (32, 512), F32, sB1)
                            qpe = [sb("qpe%d" % i, (32, 512), BF16, sB1) for i in range(2)]
                            qno = [sb("qno%d" % i, (64, 512), BF16, sB1) for i in range(2)]
                            DMA("sp", Wqb[:], wb["w_q_b"].rearrange("(k p) c -> p k c", p=128), ["wb_w_q_b"], ["Wqb"], "Wqb")
                            rope_tables(poso[0:1, j * 512:(j + 1) * 512], cs1, cs2, rt_i, rt_f, rt_a, "rt2")
                            W_, wn = load_wst(QA0, 384)
                            for c in range(4):
                                pb_ = 2 + c % 2
                                for k in range(8):
                                    MM(bank(pb_)[:, 0:384], uT[:, k, c * 128:(c + 1) * 128], W_[:, k, 0:384], k == 0, k == 7, [wn, "uT2"], [bn(pb_)])
                                norm_transpose(bank(pb_)[:, 0:384], 384, g_qa, qnT, slice(c * 128, (c + 1) * 128), [bn(pb_)], ("qnT", c), scr, 1, "n2", "pv_q_a_norm")
                            for h in range(16):
                                pn = 5 + h % 2
                                for kb in range(3):
                                    MM(bank(pn)[0:64, :], Wqb[:, kb, h * 96:h * 96 + 64], qnT[:, kb, :], kb == 0, kb == 2, ["Wqb", "qnT"], [bn(pn)])
                                for kb in range(3):
                                    MM(bank(7)[0:32, :], Wqb[:, kb, h * 96 + 64:h * 96 + 96], qnT[:, kb, :], kb == 0, kb == 2, ["Wqb", "qnT"], [bn(7)])
                                for kb in range(3):
                                    MM(bank(0)[0:32, :], Wqs[:, kb, h, :], qnT[:, kb, :], kb == 0, kb == 2, ["Wqs", "qnT"], [bn(0)])
                                TT("dve", rt_f[:], bank(7)[0:32, :], cs1[:], ALU.mult, [bn(7), "rt2c1"], ["rt2f"])
                                TT("dve", rt_a[:], bank(0)[0:32, :], cs2[:], ALU.mult, [bn(0), "rt2c0"], ["rt2a"])
                                qp_ = qpe[h % 2]
                                qpn = "qpe%d" % (h % 2)
                                qn_ = qno[h % 2]
                                qnn = "qno%d" % (h % 2)
                                TT("dve", qp_[:], rt_f[:], rt_a[:], ALU.add, ["rt2f", "rt2a"], [qpn])
                                ACT(qn_[:], bank(pn)[0:64, :], AF.Copy, [bn(pn)], [qnn])
                                DMA("sp", QT[0:32, h, :], qp_[:], [qpn], [("QT", h)], "QTp%d" % (h % 2))
                                DMA("sp", QT[32:96, h, :], qn_[:], [qnn], [("QT", h)], "QTn%d" % (h % 2))
                        P.barrier()
                        P.checkpoint("B1")
                        if debug:
                            DMA("sp", dbg["dbg_QT"][j, :, :], QT[:].rearrange("p a b -> p (a b)"), ["QT"], [], "dbg_QT")
                        with contextlib.ExitStack() as sB3:
                            KTt = [sb("KTt%d" % i, (96, 4, 512), BF16, sB3) for i in range(2)]
                            Vt = [sb("Vt%d" % i, (128, 4, 260), BF16, sB3) for i in range(2)]
                            PT = [sb("PT%d" % i, (128, 1024), BF16, sB3) for i in range(3)]
                            ost = [sb("ost%d" % i, (65, 512), F32, sB3) for i in range(2)]
                            rr = [sb("rr%d" % i, (65, 512), F32, sB3) for i in range(2)]
                            sci = 0
                            pti = 0
                            kvi = 0
                            for hg in range(4):
                                for kt in range(KTN):
                                    kb_ = KTt[kvi % 2]
                                    kn_ = "KTt%d" % (kvi % 2)
                                    vb_ = Vt[kvi % 2]
                                    vn_ = "Vt%d" % (kvi % 2)
                                    kvi += 1
                                    DMA("sp", kb_[:], KT[hg * 4:(hg + 1) * 4, :, kt * 512:(kt + 1) * 512].rearrange("h e s -> e h s"),
                                        ["KT"], [kn_], kn_)
                                    DMA("sp", vb_[:], VS[hg, kt * 512:(kt + 1) * 512, :].rearrange("(kb p) e -> p kb e", p=128),
                                        ["VS"], [vn_], vn_)
                                    masked = kt >= 4 * j
                                    for hh in range(4):
                                        h = hg * 4 + hh
                                        for kbp in range(2):
                                            pi = sci % 2
                                            sci += 1
                                            for kk_ in range(2):
                                                kb = kbp * 2 + kk_
                                                sc = pi * 2 + kk_
                                                MM(bank(sc), kb_[0:96, hh, kb * 128:(kb + 1) * 128], QT[0:96, h, :], True, True,
                                                   [kn_, ("QT", h)], [bn(sc)])
                                            p_ = PT[pti % 3]
                                            pn_ = "PT%d" % (pti % 3)
                                            pti += 1
                                            ACT(p_[:], PP[pi][:, :], AF.Exp, ["PP%d" % pi], [pn_], scale=SCALE)
                                            if masked:
                                                mi = (kt - 4 * j) * 4 + kbp * 2
                                                TT("dve", p_[:].rearrange("p (a b) -> p a b", a=2), p_[:].rearrange("p (a b) -> p a b", a=2),
                                                   maskb[:, mi:mi + 2, :], ALU.mult, [pn_, "maskb"], [pn_])
                                            for kk_ in range(2):
                                                kb = kbp * 2 + kk_
                                                MM(bank(4 + hh)[0:65, :], vb_[:, kb, hh * 65:(hh + 1) * 65], p_[:, kk_ * 512:(kk_ + 1) * 512],
                                                   kt == 0 and kb == 0, kt == KTN - 1 and kb == 3, [vn_, pn_], [bn(4 + hh)])
                                for hh in range(4):
                                    h = hg * 4 + hh
                                    o_ = ost[hh % 2]
                                    on_ = "ost%d" % (hh % 2)
                                    r_ = rr[hh % 2]
                                    rn_ = "rr%d" % (hh % 2)
                                    ACT(o_[:], bank(4 + hh)[0:65, :], AF.Copy, [bn(4 + hh)], [on_])
                                    RECIP(r_[64:65, :], o_[64:65, :], [on_], [rn_])
                                    sc = hh % 4
                                    MM(bank(sc), onesf[64:65, :], r_[64:65, :], True, True, ["onesf", rn_], [bn(sc)])
                                    TT("dve", oT[:, h, :], o_[0:64, :], bank(sc)[0:64, :], ALU.mult, [on_, bn(sc)], [("oT", h)])
                    P.barrier()
                    P.checkpoint("B3")
                    if debug:
                        DMA("sp", dbg["dbg_oT"][j, :, :], oT[:].rearrange("p a b -> p (a b)"), ["oT"], [], "dbg_oT")
                    with contextlib.ExitStack() as sC:
                        alloc_wst(sC)
                        Wmo = sb("Wmo", (64, 16, 1024), BF16, sC)
                        Wo = sb("Wo", (128, 8, 1024), BF16, sC)
                        sg2 = [sb("sg2_%d" % i, (128, 512), F32, sC) for i in range(2)]
                        mm_ = [sb("mm_%d" % i, (128, 512), F32, sC) for i in range(2)]
                        mT = sb("mT", (128, 8, 512), BF16, sC)
                        DMA("sp", Wmo[:], wb["w_mla_out"].rearrange("(h e) c -> e h c", e=64), ["wb_w_mla_out"], ["Wmo"], "Wmo")
                        DMA("sp", Wo[:], wb["w_o"].rearrange("(k p) c -> p k c", p=128), ["wb_w_o"], ["Wo"], "Wo")
                        for unit in range(2):
                            W_, wn = load_wst(G20 + unit * 512, 512)
                            for bi in range(4):
                                cb_ = unit * 4 + bi
                                yb = cb_ % 2
                                gb = 2 + cb_ % 2
                                for h in range(16):
                                    MM(bank(yb), Wmo[0:64, h, cb_ * 128:(cb_ + 1) * 128], oT[0:64, h, :], h == 0, h == 15, ["Wmo", "oT"], [bn(yb)])
                                for k in range(8):
                                    MM(bank(gb), W_[:, k, bi * 128:(bi + 1) * 128], uT[:, k, :], k == 0, k == 7, [wn, "uT2"], [bn(gb)])
                                s_ = sg2[cb_ % 2]
                                sn = "sg2_%d" % (cb_ % 2)
                                m_ = mm_[cb_ % 2]
                                mn = "mm_%d" % (cb_ % 2)
                                ACT(s_[:], bank(gb), AF.Sigmoid, [bn(gb)], [sn])
                                TT("dve", m_[:], s_[:], bank(yb), ALU.mult, [sn, bn(yb)], [mn])
                                TT("dve", mT[:, cb_, :], m_[:], t1T[:, cb_, :], ALU.add, [mn, ("t1T", cb_)], [("mT", cb_)])
                        for c in range(4):
                            for half in range(2):
                                pb_ = 4 + half
                                for k in range(8):
                                    MM(bank(pb_), mT[:, k, c * 128:(c + 1) * 128], Wo[:, k, half * 512:(half + 1) * 512], k == 0, k == 7,
                                       ["mT", "Wo"], [bn(pb_)])
                                TT("dve", hT[:, c, half * 512:(half + 1) * 512], hT[:, c, half * 512:(half + 1) * 512], bank(pb_), ALU.add,
                                   [("hT", c), bn(pb_)], [("hT", c)])
                P.barrier()
                P.checkpoint("C")
                if debug:
                    DMA("sp", dbg["dbg_h1"][j * 512:(j + 1) * 512, :].rearrange("(c p) d -> p c d", p=128), hT[:], ["hT"], [], "dbg_h1")
                with contextlib.ExitStack() as sD:
                    hnT = sb("hnT", (128, 8, 512), BF16, sD)
                    Wxq = sb("Wxq", (128, 8, 1024), BF16, sD)
                    Wxo = sb("Wxo", (128, 8, 1024), BF16, sD)
                    qxT = sb("qxT", (128, 8, 512), BF16, sD)
                    kxT = sb("kxT", (128, 8, 256), BF16, sD)
                    vx = sb("vx", (128, 2, 1024), BF16, sD)
                    PTx = [sb("PTx%d" % i, (128, 512), BF16, sD) for i in range(2)]
                    rden = sb("rden", (128, 512), F32, sD)
                    oxT = sb("oxT", (128, 8, 512), BF16, sD)
                    DMA("sp", Wxq[:], wb["w_xq"].rearrange("(k p) c -> p k c", p=128), ["wb_w_xq"], ["Wxq"], "Wxq")
                    DMA("sp", Wxo[:], wb["w_xo"].rearrange("(k p) c -> p k c", p=128), ["wb_w_xo"], ["Wxo"], "Wxo")
                    DMA("sp", kxT[:].rearrange("p a b -> p (a b)"), KXD[:, :], ["KXD"], ["kxT"], "kxT")
                    DMA("sp", vx[:].rearrange("p a b -> p (a b)"), VXD[:, :], ["VXD"], ["vx"], "vx")
                    for c in range(4):
                        norm_transpose(hT[:, c, :], 1024, g_xa, hnT, slice(c * 128, (c + 1) * 128), [("hT", c)], ("hnT", c), scr, 0, "n2", "pv_norm_xattn")
                    for blk in range(8):
                        pb_ = 2 + blk % 2
                        for k in range(8):
                            MM(bank(pb_), Wxq[:, k, blk * 128:(blk + 1) * 128], hnT[:, k, :], k == 0, k == 7, ["Wxq", "hnT"], [bn(pb_)])
                        ACT(qxT[:, blk, :], bank(pb_), AF.Copy, [bn(pb_)], [("qxT", blk)])
                    for a in range(4):
                        for mb in range(2):
                            for dc in range(2):
                                MM(bank(mb), kxT[:, a * 2 + dc, mb * 128:(mb + 1) * 128], qxT[:, a * 2 + dc, :], dc == 0, dc == 1,
                                   ["kxT", ("qxT", a * 2 + dc)], [bn(mb)])
                            ACT(PTx[mb][:], bank(mb), AF.Exp, [bn(mb)], ["PTx%d" % mb], scale=float(256 ** -0.5))
                        for mb in range(2):
                            MM(bank(4), onesb[:], PTx[mb][:], mb == 0, mb == 1, ["onesb", "PTx%d" % mb], [bn(4)])
                        RECIP(rden[:], bank(4), [bn(4)], ["rden"])
                        for db in range(2):
                            pb_ = 5 + db
                            for mb in range(2):
                                MM(bank(pb_), vx[:, mb, a * 256 + db * 128:a * 256 + (db + 1) * 128], PTx[mb][:], mb == 0, mb == 1,
                                   ["vx", "PTx%d" % mb], [bn(pb_)])
                            TT("dve", oxT[:, a * 2 + db, :], rden[:], bank(pb_), ALU.mult, ["rden", bn(pb_)], [("oxT", a * 2 + db)])
                    for c in range(4):
                        for half in range(2):
                            pb_ = 2 + half
                            for k in range(8):
                                MM(bank(pb_), oxT[:, k, c * 128:(c + 1) * 128], Wxo[:, k, half * 512:(half + 1) * 512], k == 0, k == 7,
                                   ["oxT", "Wxo"], [bn(pb_)])
                            TT("dve", hT[:, c, half * 512:(half + 1) * 512], hT[:, c, half * 512:(half + 1) * 512], bank(pb_), ALU.add,
                               [("hT", c), bn(pb_)], [("hT", c)])
                P.barrier()
                P.checkpoint("D")
                if debug:
                    DMA("sp", dbg["dbg_h2"][j * 512:(j + 1) * 512, :].rearrange("(c p) d -> p c d", p=128), hT[:], ["hT"], [], "dbg_h2")
                with contextlib.ExitStack() as sE:
                    hnT = sb("hn2T", (128, 8, 512), BF16, sE)
                    selh = sb("selhE", (32, 32 * 128), F32, sE)
                    DMA("sp", selh[:], c_selh[:, :], [], ["selhE"], "selhE")
                    L = sb("L", (128, 36), F32, sE)
                    gmax = sb("gmax", (128, 1), F32, sE)
                    ngmax = sb("ngmax", (128, 1), F32, sE)
                    goh = sb("goh", (128, 4), F32, sE)
                    gj = sb("gj", (128, 4), F32, sE)
                    gsum = sb("gsum", (128, 1), F32, sE)
                    gw = sb("gw", (128, 1), F32, sE)
                    pen = sb("pen", (128, 4), F32, sE)
                    em = sb("em", (128, 32), F32, sE)
                    em2 = sb("em2", (128, 32), F32, sE)
                    m1 = sb("m1", (128, 1), F32, sE)
                    m2 = sb("m2", (128, 1), F32, sE)
                    oh1 = sb("oh1", (128, 32), F32, sE)
                    oh2 = sb("oh2", (128, 32), F32, sE)
                    dd = sb("dd", (128, 1), F32, sE)
                    ee = sb("ee", (128, 1), F32, sE)
                    w1 = sb("w1", (128, 1), F32, sE)
                    w2 = sb("w2", (128, 1), F32, sE)
                    comb = sb("comb", (128, 4, 32), F32, sE)
                    combT = sb("combT", (32, 512), F32, sE)
                    cbs = [sb("cbs%d" % i, (128, 512), F32, sE) for i in range(2)]
                    Wg = [sb("Wg%d" % i, (128, 8, 256), BF16, sE) for i in range(2)]
                    Wu = [sb("Wu%d" % i, (128, 8, 256), BF16, sE) for i in range(2)]
                    Wd = [sb("Wd%d" % i, (128, 8, 1024), BF16, sE) for i in range(2)]
                    sgm = [sb("sgm%d" % i, (128, 512), F32, sE) for i in range(2)]
                    tg = [sb("tg%d" % i, (128, 512), F32, sE) for i in range(2)]
                    actT = [sb("actT%d" % i, (128, 8, 512), BF16, sE) for i in range(2)]
                    for c in range(4):
                        norm_transpose(hT[:, c, :], 1024, g_moe, hnT, slice(c * 128, (c + 1) * 128), [("hT", c)], ("hn2T", c), scr, 0, "n2", "pv_norm_moe")
                    for c in range(4):
                        cc = slice(c * 128, (c + 1) * 128)
                        b7 = bank(7)
                        for k in range(8):
                            MM(b7[:, 0:36], hnT[:, k, cc], Wr[:, k, :], k == 0, k == 7, [("hn2T", c), "Wr"], [bn(7)])
                        TT("dve", L[:], b7[:, 0:36], rb_bc[:], ALU.add, [bn(7), "rb_bc"], ["L"])
                        P.op("dve", lambda e: e.reduce_max(out=gmax[:], in_=L[:, 0:4], axis=mybir.AxisListType.X), reads=["L"], writes=["gmax"])
                        TS("dve", ngmax[:], gmax[:], -1.0, None, ALU.mult, None, ["gmax"], ["ngmax"])
                        TS("dve", goh[:], L[:, 0:4], gmax[:, 0:1], None, ALU.is_equal, None, ["L", "gmax"], ["goh"])
                        ACT(gj[:], L[:, 0:4], AF.Exp, ["L", "ngmax"], ["gj", "gsum"], bias=ngmax[:, 0:1], accum=gsum[:])
                        RECIP(gw[:], gsum[:], ["gsum"], ["gw"])
                        TS("dve", pen[:], goh[:], BIG, -BIG, ALU.mult, ALU.add, ["goh"], ["pen"])
                        TT("dve", em[:].rearrange("p (g e) -> p g e", g=4), L[:, 4:36].rearrange("p (g e) -> p g e", g=4),
                           pen[:, :].unsqueeze(2).to_broadcast([128, 4, 8]), ALU.add, ["L", "pen"], ["em"])
                        P.op("dve", lambda e: e.reduce_max(out=m1[:], in_=em[:], axis=mybir.AxisListType.X), reads=["em"], writes=["m1"])
                        TS("dve", oh1[:], em[:], m1[:, 0:1], None, ALU.is_equal, None, ["em", "m1"], ["oh1"])
                        STT("dve", em2[:], oh1[:], -BIG, em[:], ALU.mult, ALU.add, ["oh1", "em"], ["em2"])
                        P.op("dve", lambda e: e.reduce_max(out=m2[:], in_=em2[:], axis=mybir.AxisListType.X), reads=["em2"], writes=["m2"])
                        TS("dve", oh2[:], em2[:], m2[:, 0:1], None, ALU.is_equal, None, ["em2", "m2"], ["oh2"])
                        TT("dve", dd[:], m2[:], m1[:], ALU.subtract, ["m1", "m2"], ["dd"])
                        ACT(ee[:], dd[:], AF.Exp, ["dd"], ["ee"])
                        TS("dve", w1[:], ee[:], 1.0, None, ALU.add, None, ["ee"], ["w1"])
                        RECIP(w1[:], w1[:], ["w1"], ["w1"])
                        TT("dve", w2[:], ee[:], w1[:], ALU.mult, ["ee", "w1"], ["w2"])
                        TT("dve", w1[:], w1[:], gw[:], ALU.mult, ["w1", "gw"], ["w1"])
                        TT("dve", w2[:], w2[:], gw[:], ALU.mult, ["w2", "gw"], ["w2"])
                        TS("dve", oh1[:], oh1[:], w1[:, 0:1], None, ALU.mult, None, ["oh1", "w1"], ["oh1"])
                        STT("dve", comb[:, c, :], oh2[:], w2[:, 0:1], oh1[:], ALU.mult, ALU.add, ["oh2", "w2", "oh1"], [("comb", c)])
                        P.op("pe", lambda e, c=c: e.transpose(out=bank(6)[0:32, c * 128:(c + 1) * 128], in_=comb[:, c, :], identity=identf[:]),
                             reads=[("comb", c), "identf"], writes=[bn(6)])
                    ACT(combT[:], bank(6)[0:32, :], AF.Copy, [bn(6)], ["combT"])
                    if debug:
                        DMA("sp", dbg["dbg_comb"][j * 512:(j + 1) * 512, :].rearrange("(c p) e -> p c e", p=128), comb[:], ["comb"], [], "dbg_comb")
                    wg_v = wb["w_exp_gate"].rearrange("(e k p) f -> e p k f", e=32, p=128)
                    wu_v = wb["w_exp_up"].rearrange("(e k p) f -> e p k f", e=32, p=128)
                    wd_v = wb["w_exp_down"].rearrange("(g k p) c -> g p k c", g=8, p=128)
                    for eg in range(8):
                        wd_ = Wd[eg % 2]
                        wdn = "Wd%d" % (eg % 2)
                        DMA("sp", wd_[:], wd_v[eg], ["wb_w_exp_down"], [wdn], wdn)
                        at_ = actT[eg % 2]
                        atn = "actT%d" % (eg % 2)
                        for ei in range(4):
                            e_ = eg * 4 + ei
                            wg_ = Wg[e_ % 2]
                            wgn = "Wg%d" % (e_ % 2)
                            wu_ = Wu[e_ % 2]
                            wun = "Wu%d" % (e_ % 2)
                            DMA("sp", wg_[:], wg_v[e_], ["wb_w_exp_gate"], [wgn], wgn)
                            DMA("sp", wu_[:], wu_v[e_], ["wb_w_exp_up"], [wun], wun)
                            MM(bank(6), selh[:, e_ * 128:(e_ + 1) * 128], combT[:], True, True, ["selhE", "combT"], [bn(6)])
                            cb_ = cbs[e_ % 2]
                            cbn = "cbs%d" % (e_ % 2)
                            ACT(cb_[:], bank(6), AF.Copy, [bn(6)], [cbn])
                            for fb in range(2):
                                gbk = fb
                                ubk = 2 + fb
                                for k in range(8):
                                    MM(bank(gbk), wg_[:, k, fb * 128:(fb + 1) * 128], hnT[:, k, :], k == 0, k == 7, [wgn, "hn2T"], [bn(gbk)])
                                for k in range(8):
                                    MM(bank(ubk), wu_[:, k, fb * 128:(fb + 1) * 128], hnT[:, k, :], k == 0, k == 7, [wun, "hn2T"], [bn(ubk)])
                                s_ = sgm[fb]
                                sn = "sgm%d" % fb
                                t_ = tg[fb]
                                tn = "tg%d" % fb
                                ACT(s_[:], bank(gbk), AF.Silu, [bn(gbk)], [sn])
                                TT("dve", t_[:], s_[:], bank(ubk), ALU.mult, [sn, bn(ubk)], [tn])
                                TT("pool", at_[:, ei * 2 + fb, :], t_[:], cb_[:], ALU.mult, [tn, cbn], [(atn, ei * 2 + fb)])
                        for c in range(4):
                            for half in range(2):
                                pb_ = 4 + half
                                for fbk in range(8):
                                    MM(bank(pb_), at_[:, fbk, c * 128:(c + 1) * 128], wd_[:, fbk, half * 512:(half + 1) * 512], fbk == 0, fbk == 7,
                                       [atn, wdn], [bn(pb_)])
                                TT("dve", hT[:, c, half * 512:(half + 1) * 512], hT[:, c, half * 512:(half + 1) * 512], bank(pb_), ALU.add,
                                   [("hT", c), bn(pb_)], [("hT", c)])
                P.barrier()
                P.checkpoint("E")
                if debug:
                    DMA("sp", dbg["dbg_h3"][j * 512:(j + 1) * 512, :].rearrange("(c p) d -> p c d", p=128), hT[:], ["hT"], [], "dbg_h3")
                with contextlib.ExitStack() as sF:
                    nf_bc = sb("nf_bc", (128, 1024), F32, sF)
                    jf = sb("jf", (128, 1024), BF16, sF)
                    DMA("sp", nf_bc[:], vd["norm_final"].partition_broadcast(128), [], ["nf_bc"], "nf_bc")
                    for c in range(4):
                        ACT(jf[:], hT[:, c, :], AF.Square, [("hT", c)], ["jf", "n2ssq"], accum=ssq[:])
                        ACT(rstd[:], ssq[:], AF.Sqrt, ["n2ssq"], ["n2rstd"], bias=eps_t[:], scale=1.0 / 1024)
                        RECIP(rstd[:], rstd[:], ["n2rstd"], ["n2rstd"])
                        STT("dve", hT[:, c, :], hT[:, c, :], rstd[:, 0:1], nf_bc[:], ALU.mult, ALU.mult, [("hT", c), "n2rstd", "nf_bc"], [("hT", c)])
                    DMA("sp", out_d[j * 512:(j + 1) * 512, :].rearrange("(c p) d -> p c d", p=128), hT[:], ["hT"], [], "out")
                P.barrier()

    P.emit()
    es.close()
    return nc


def _consts():
    bf = ml_dtypes.bfloat16
    c = {}
    c["c_identb"] = np.eye(128, dtype=np.float32).astype(bf)
    c["c_identf"] = np.eye(128, dtype=np.float32)
    s = np.arange(128)
    c["c_tri"] = (s[:, None] <= s[None, :]).astype(np.float32)
    c["c_onesf"] = np.ones((128, 128), np.float32)
    c["c_maskneg"] = np.where(s[:, None] > s[None, :], NEG, 0.0).astype(np.float32).astype(bf)
    sel = np.zeros((32, 32, 128), np.float32)
    for h in range(32):
        sel[h, h, :] = 1.0
    c["c_selh"] = sel.reshape(32, 32 * 128)
    half = 16
    inv = (np.float32(10000.0) ** (-(np.arange(half, dtype=np.float32)) / np.float32(half))).astype(np.float32)
    invf = np.zeros((32, 2), np.float32)
    invf[:, 0] = np.concatenate([inv, inv])
    invf[:, 1] = np.concatenate([-np.ones(16), np.ones(16)])
    c["c_invf"] = invf
    return c


def make_in_maps(inputs, S):
    bf = ml_dtypes.bfloat16
    NT = S // 512
    NJ = NT // 4
    x = np.asarray(inputs["x"], np.float32)
    mem = np.asarray(inputs["mem"], np.float32)
    pos = np.asarray(inputs["positions"], np.int32)
    consts = _consts()
    shared = {}
    for n, shp in WEIGHTS:
        shared[n] = np.ascontiguousarray(np.asarray(inputs[n], np.float32).reshape(shp))
    for n, ln in VECS:
        shared[n] = np.ascontiguousarray(np.asarray(inputs[n], np.float32).reshape(1, ln))
    shared["conv_w"] = np.ascontiguousarray(np.asarray(inputs["conv_w"], np.float32).reshape(4, 3072))
    shared.update(consts)
    maps = []
    kk = np.arange(512)
    for core in range(8):
        b, q = core // 4, core % 4
        m = dict(shared)
        m["xb"] = np.ascontiguousarray(x[b])
        xo = np.zeros((NJ, 515, D), np.float32)
        po = np.zeros((1, NJ * 512), np.int32)
        for j in range(NJ):
            t0 = (4 * j + q) * 512
            xo[j, 3:] = x[b, t0:t0 + 512]
            if t0 >= 3:
                xo[j, 0:3] = x[b, t0 - 3:t0]
            po[0, j * 512:(j + 1) * 512] = pos[b, t0:t0 + 512]
        m["xo"] = xo
        m["posb"] = np.ascontiguousarray(pos[b:b + 1])
        m["poso"] = po
        m["memb"] = np.ascontiguousarray(mem[b])
        mb = np.zeros((128, 4, 4, 512), np.float32)
        for ktl in range(4):
            for kb in range(4):
                key = ktl * 512 + kb * 128 + np.arange(128)[:, None]
                qq = q * 512 + kk[None, :]
                mb[:, ktl, kb, :] = np.where(key > qq, 0.0, 1.0)
        m["maskb"] = mb.reshape(128, 16, 512).astype(bf)
        oh = np.zeros((128, 4), np.float32)
        oh[:, q] = 1.0
        m["ohq"] = oh
        maps.append(m)
    return maps


_NC_CACHE = {}


def kernel(**inputs):
    S = int(np.asarray(inputs["x"]).shape[1])
    B = int(np.asarray(inputs["x"]).shape[0])
    assert B == 2
    if S not in _NC_CACHE:
        _NC_CACHE[S] = build(S)
    nc = _NC_CACHE[S]
    maps = make_in_maps(inputs, S)
    res = run_bass_kernel_spmd(nc, maps, core_ids=list(range(8)))
    NT = S // 512
    NJ = NT // 4
    out = np.zeros((B, S, D), np.float32)
    for core in range(8):
        b, q = core // 4, core % 4
        o = np.asarray(res.results[core]["out"], np.float32)
        for j in range(NJ):
            t0 = (4 * j + q) * 512
            out[b, t0:t0 + 512] = o[j * 512:(j + 1) * 512]
    return out
```

```python
import contextlib
import numpy as np
import ml_dtypes
import concourse.bass as bass
import concourse.mybir as mybir
from concourse.bass_utils import run_bass_kernel_spmd

F32 = mybir.dt.float32
BF16 = mybir.dt.bfloat16
I32 = mybir.dt.int32
U32 = mybir.dt.uint32
AF = mybir.ActivationFunctionType
ALU = mybir.AluOpType

ENGS = ("pe", "act", "dve", "pool", "sp")


class Prog:
    dead = False
    stop_at = None
    WINDOW = 120

    def __init__(self, nc):
        self.nc = nc
        self.ops = []
        self.state = {}
        self.seg = 0
        self.last_bar = {}
        self.n_bar = 0

    def checkpoint(self, name):
        if self.stop_at is not None and name == self.stop_at:
            self.barrier()
            self.dead = True

    @staticmethod
    def _norm(acc):
        return [a if isinstance(a, tuple) else (a, None) for a in acc]

    def _deps_for(self, reads, writes):
        deps = set()
        for (name, sub) in reads:
            st = self.state.get(name)
            if not st:
                continue
            subs = list(st.keys()) if sub is None else [s for s in st.keys() if s is None or s == sub]
            for s in subs:
                w = st[s][0]
                if w is not None:
                    deps.add(w)
        for (name, sub) in writes:
            st = self.state.get(name)
            if not st:
                continue
            subs = list(st.keys()) if sub is None else [s for s in st.keys() if s is None or s == sub]
            for s in subs:
                w, rs = st[s]
                if w is not None:
                    deps.add(w)
                deps.update(rs)
        return deps

    def _update(self, idx, reads, writes):
        for (name, sub) in writes:
            st = self.state.setdefault(name, {})
            if sub is None:
                st.clear()
                st[None] = [idx, []]
            else:
                st[sub] = [idx, []]
        for (name, sub) in reads:
            st = self.state.setdefault(name, {})
            if sub is None:
                for s in st.values():
                    s[1].append(idx)
                if not st:
                    st[None] = [None, [idx]]
            else:
                if sub in st:
                    st[sub][1].append(idx)
                elif None in st:
                    st[None][1].append(idx)
                else:
                    st[sub] = [None, [idx]]

    def op(self, eng, fn, reads=(), writes=(), cost=0.2):
        if self.dead:
            return -1
        reads = self._norm(reads)
        writes = self._norm(writes)
        if eng != "pe":
            pr = [a for a in reads if a[0].startswith("PP")]
            if pr:
                writes = writes + pr
        idx = len(self.ops)
        deps = self._deps_for(reads, writes)
        if eng in self.last_bar:
            deps.add(self.last_bar[eng])
        self.ops.append(dict(eng=eng, fn=fn, deps=deps, dma=None, idx=idx, seg=self.seg, cost=cost, lat=cost))
        self._update(idx, reads, writes)
        return idx

    def dma(self, queue, fn, reads=(), writes=(), key=None, nbytes=65536):
        if self.dead:
            return -1
        reads = self._norm(reads)
        writes = self._norm(writes)
        idx = len(self.ops)
        deps = self._deps_for(reads, writes)
        if queue in self.last_bar:
            deps.add(self.last_bar[queue])
        assert key is not None
        self.ops.append(dict(eng=queue, fn=fn, deps=deps, dma=key, idx=idx, seg=self.seg, cost=0.15,
                             lat=2.5 + nbytes / 60000.0))
        self._update(idx, reads, writes)
        return idx

    def barrier(self):
        if self.dead:
            return
        self.n_bar += 1
        for e in ENGS:
            idx = len(self.ops)
            self.ops.append(dict(eng=e, fn=None, deps=set(), dma=None, idx=idx, bar=self.n_bar, seg=self.seg, cost=0.0, lat=0.0))
            self.last_bar[e] = idx
        self.seg += 1
        self.state = {}

    def schedule(self):
        ops = self.ops
        n = len(ops)
        import os as _os
        WD = int(_os.environ.get("K_WDEF", str(self.WINDOW)))
        segw = {}
        for kv in _os.environ.get("K_WIN", "").split(","):
            if ":" in kv:
                a_, b_ = kv.split(":")
                segw[int(a_)] = int(b_)
        per_eng = {e: [] for e in ENGS}
        for o in ops:
            per_eng[o["eng"]].append(o["idx"])
        pos = {e: 0 for e in ENGS}
        free = {e: 0.0 for e in ENGS}
        sched = [False] * n
        finish = [0.0] * n
        dep_left = [0] * n
        users = [[] for _ in range(n)]
        ready_t = [0.0] * n
        for o in ops:
            i = o["idx"]
            dep_left[i] = len(o["deps"])
            for d in o["deps"]:
                users[d].append(i)
        nseg = self.seg + 1
        seg_left = [0] * (nseg + 1)
        seg_fin = [0.0] * (nseg + 1)
        for o in ops:
            if not o.get("bar"):
                seg_left[o["seg"]] += 1
        order = []
        remaining = n
        while remaining > 0:
            best = None
            for e in ENGS:
                lst = per_eng[e]
                p = pos[e]
                while p < len(lst) and sched[lst[p]]:
                    p += 1
                pos[e] = p
                cnt = 0
                q = p
                fe = free[e]
                W = segw.get(ops[lst[p]]["seg"], WD) if p < len(lst) else WD
                while q < len(lst) and cnt < W:
                    i = lst[q]
                    q += 1
                    if sched[i]:
                        continue
                    cnt += 1
                    o = ops[i]
                    if o.get("bar"):
                        if seg_left[o["seg"]] == 0:
                            st = max(fe, seg_fin[o["seg"]])
                            if best is None or (st, i) < (best[0], best[1]):
                                best = (st, i, e)
                        break
                    if dep_left[i] != 0:
                        continue
                    st = max(fe, ready_t[i])
                    if best is None or (st, i) < (best[0], best[1]):
                        best = (st, i, e)
                    if st <= fe:
                        break
            assert best is not None, "scheduler deadlock"
            st, i, e = best
            o = ops[i]
            sched[i] = True
            remaining -= 1
            free[e] = st + o["cost"]
            finish[i] = st + o["lat"]
            o["t"] = st
            order.append(i)
            if not o.get("bar"):
                sg = o["seg"]
                seg_left[sg] -= 1
                if finish[i] > seg_fin[sg]:
                    seg_fin[sg] = finish[i]
            for u in users[i]:
                dep_left[u] -= 1
                lat = 0.12 if ops[u]["eng"] != e else 0.05
                if ops[u]["eng"] == "pe" and e == "pe" and o["dma"] is None:
                    lat = 0.0
                t = finish[i] + lat
                if t > ready_t[u]:
                    ready_t[u] = t
        self.sim_time = max(finish) if finish else 0.0
        return order

    def emit(self, reorder=True):
        nc = self.nc
        ops = self.ops
        order = self.schedule() if reorder else list(range(len(ops)))
        needs_sig = [False] * len(ops)
        last_real = {e: None for e in ENGS}
        bar_snap = {}
        for i in order:
            o = ops[i]
            if o.get("bar"):
                if o["bar"] not in bar_snap:
                    bar_snap[o["bar"]] = dict(last_real)
                    for e2, li in last_real.items():
                        if li is not None:
                            needs_sig[li] = True
                continue
            for d in o["deps"]:
                needs_sig[d] = True
            if o["dma"] is None:
                last_real[o["eng"]] = i
        eng_sig = {e: 0 for e in ENGS}
        phys_count = []
        key_phys = {}
        key_sealed = {}
        free_phys = []
        waited = {e: {} for e in ENGS}
        sig_of = [None] * len(ops)
        plan = {}
        bar_dma_snap = {}
        for i in order:
            o = ops[i]
            e = o["eng"]
            waits = []
            w = waited[e]
            if o.get("bar"):
                bid = o["bar"]
                if bid not in bar_dma_snap:
                    bar_dma_snap[bid] = list(phys_count)
                    for k, p in key_phys.items():
                        free_phys.append(p)
                    key_phys = {}
                    key_sealed = {}
                for e2, li in bar_snap[bid].items():
                    if li is None:
                        continue
                    s_ = sig_of[li]
                    if s_ is None:
                        continue
                    kind, name, val = s_
                    if w.get((kind, name), 0) < val:
                        w[(kind, name)] = val
                        waits.append((kind, name, val))
                for p, cnt in enumerate(bar_dma_snap[bid]):
                    if cnt > 0 and w.get(("dma", p), 0) < cnt:
                        w[("dma", p)] = cnt
                        waits.append(("dma", p, cnt))
                plan[i] = (waits, None)
                continue
            for d in sorted(o["deps"]):
                od = ops[d]
                if od["dma"] is None and od["eng"] == "pe" and e == "pe" and o["dma"] is None:
                    continue
                s_ = sig_of[d]
                if s_ is None:
                    continue
                kind, name, val = s_
                if kind == "dma":
                    k = od["dma"]
                    if key_phys.get(k) == name:
                        val = max(val, phys_count[name])
                        key_sealed[k] = True
                if w.get((kind, name), 0) >= val:
                    continue
                w[(kind, name)] = val
                waits.append((kind, name, val))
            sig = None
            if o["dma"] is not None:
                k = o["dma"]
                if k not in key_phys:
                    if free_phys:
                        p = free_phys.pop(0)
                    else:
                        p = len(phys_count)
                        phys_count.append(0)
                    key_phys[k] = p
                    key_sealed[k] = False
                p = key_phys[k]
                if key_sealed[k] and phys_count[p] > 0:
                    if w.get(("dma", p), 0) < phys_count[p]:
                        w[("dma", p)] = phys_count[p]
                        waits.append(("dma", p, phys_count[p]))
                    key_sealed[k] = False
                phys_count[p] += 16
                sig = ("dma", p, phys_count[p])
            elif needs_sig[i]:
                eng_sig[e] += 1
                sig = ("eng", e, eng_sig[e])
            sig_of[i] = sig
            plan[i] = (waits, sig)
        self.max_sig = dict(eng_sig)
        self.n_dma_sems = len(phys_count)
        with contextlib.ExitStack() as es:
            sems = {}
            for e in ENGS:
                sems[("eng", e)] = es.enter_context(nc.semaphore("s_" + e))
            for p in range(len(phys_count)):
                sems[("dma", p)] = es.enter_context(nc.semaphore("d_%d" % p))
            block = es.enter_context(nc.Block())
            per_eng = {e: [] for e in ENGS}
            for i in order:
                per_eng[ops[i]["eng"]].append(i)
            final_waits = [(("dma", p), phys_count[p]) for p in range(len(phys_count))]
            final_eng = [(("eng", e), eng_sig[e]) for e in ENGS if eng_sig[e] > 0]

            def make(e):
                def body(engobj):
                    for i in per_eng[e]:
                        o = ops[i]
                        waits, sig = plan[i]
                        for (kind, name, val) in waits:
                            engobj.wait_ge(sems[(kind, name)], val)
                        if o["fn"] is None:
                            continue
                        ins = o["fn"](engobj)
                        if sig is not None:
                            kind, name, val = sig
                            ins.then_inc(sems[(kind, name)], 16 if kind == "dma" else 1)
                    if e == "sp":
                        for (sk, v) in final_waits:
                            engobj.wait_ge(sems[sk], v)
                        for (sk, v) in final_eng:
                            engobj.wait_ge(sems[sk], v)
                return body

            block.tensor(make("pe"))
            block.scalar(make("act"))
            block.vector(make("dve"))
            block.gpsimd(make("pool"))
            block.sync(make("sp"))


D = 1024
DI = 2048
NH = 32
NMEM = 256
RMS_EPS = 1e-6
Z0, X0, BC0, C0, DT0, QA0, CKV0, KR0, G10, G20 = 0, 2048, 4096, 4608, 5120, 5152, 5536, 5792, 5824, 6848
NIN = 7872
TWO_PI = 6.283185307179586
CW1 = 6.28125
CW2 = TWO_PI - 6.28125
NEG = -30000.0

WEIGHTS = [
    ("w_in", (1024, 7872)), ("w_ssd_out", (2048, 1024)), ("w_q_b", (384, 1536)), ("w_kv_b", (256, 2048)),
    ("w_mla_out", (1024, 1024)), ("w_o", (1024, 1024)), ("w_xq", (1024, 1024)), ("w_xkv", (1024, 2048)),
    ("w_xo", (1024, 1024)), ("w_router_group", (1024, 4)), ("w_router_expert", (1024, 32)),
    ("w_exp_gate", (32 * 1024, 256)), ("w_exp_up", (32 * 1024, 256)), ("w_exp_down", (32 * 256, 1024)),
]
VECS = [
    ("norm_mix", 1024), ("conv_b", 3072), ("dt_bias", 32), ("a_log", 32), ("d_skip", 32), ("ssd_norm", 2048),
    ("q_a_norm", 384), ("kv_a_norm", 256), ("norm_xattn", 1024), ("norm_mem", 1024), ("norm_moe", 1024),
    ("b_router_group", 4), ("b_router_expert", 32), ("norm_final", 1024),
]


class K:
    pass


def build(S, debug=False, phases=(1, 2), stop_at=None):
    NT = S // 512
    NJ = NT // 4
    nc = bass.Bass("TRN2", target_bir_lowering=False)
    P = Prog(nc)
    P.stop_at = stop_at

    def din(name, shape, dt=F32):
        return nc.dram_tensor(name, list(shape), dt, kind="ExternalInput").ap()

    def dscr(name, shape, dt):
        return nc.dram_tensor(name, list(shape), dt, kind=("ExternalOutput" if debug else "Internal")).ap()

    xb = din("xb", (S, D))
    xo = din("xo", (NJ, 515, D))
    posb = din("posb", (1, S), I32)
    poso = din("poso", (1, NJ * 512), I32)
    memb = din("memb", (NMEM, D))
    maskb_d = din("maskb", (128, 16, 512), BF16)
    ohq_d = din("ohq", (128, 4))
    c_identb = din("c_identb", (128, 128), BF16)
    c_identf = din("c_identf", (128, 128))
    c_tri = din("c_tri", (128, 128))
    c_onesf = din("c_onesf", (128, 128))
    c_maskneg = din("c_maskneg", (128, 128), BF16)
    c_selh = din("c_selh", (32, 32 * 128))
    c_invf = din("c_invf", (32, 2))
    wd = {n: din(n, shp) for n, shp in WEIGHTS}
    vd = {n: din(n, (1, ln)) for n, ln in VECS}
    conv_w_d = din("conv_w", (4, 3072))
    out_d = nc.dram_tensor("out", [NJ * 512, D], F32, kind="ExternalOutput").ap()

    wb = {n: nc.dram_tensor(n + "_b", list(shp), BF16, kind="Internal").ap() for n, shp in WEIGHTS}
    KT = dscr("KT", (16, 96, S), BF16)
    VS = dscr("VS", (4, S, 260), BF16)
    OSD = dscr("OSD", (NJ * 4, 128, 2048), BF16)

    es = contextlib.ExitStack()

    sb_cnt = [0]

    def sb(name, shape, dt, st=None):
        sb_cnt[0] += 1
        return (st or es).enter_context(nc.sbuf_tensor("s%d_%s" % (sb_cnt[0], name), list(shape), dt))

    def fsz(ap):
        n = 1
        for d_ in ap.shape[1:]:
            n *= int(d_)
        return n

    def nbytes_of(ap):
        n = 1
        for d_ in ap.shape:
            n *= int(d_)
        return n * (4 if ap.dtype in (F32, I32, U32) else 2)

    def DMA(q, out, in_, r, w, key, **kw):
        P.dma(q, lambda e: e.dma_start(out=out, in_=in_, **kw), reads=r, writes=w, key=key, nbytes=nbytes_of(out))

    def MM(out, lhsT, rhs, start, stop, r, w):
        n_ = fsz(rhs)
        c_ = max(n_, 64) / 2400.0 * (4.0 if lhsT.dtype == F32 else 1.0) + 0.03
        P.op("pe", lambda e: e.matmul(out, lhsT=lhsT, rhs=rhs, start=start, stop=stop), reads=r, writes=w, cost=c_)

    def TR(out, in_, ident, r, w):
        P.op("pe", lambda e: e.transpose(out=out, in_=in_, identity=ident), reads=r, writes=w, cost=0.09)

    def ACT(out, in_, func, r, w, bias=None, scale=None, accum=None):
        kw = {}
        if bias is not None:
            kw["bias"] = bias
        if scale is not None:
            kw["scale"] = scale
        if accum is not None:
            kw["accum_out"] = accum
        c_ = fsz(in_) / 1100.0 + 0.22 + (0.1 if accum is not None else 0.0)
        P.op("act", lambda e: e.activation(out=out, in_=in_, func=func, **kw), reads=r, writes=w, cost=c_)

    def vcost(eng, ap):
        return fsz(ap) / (900.0 if eng == "dve" else 500.0) + (0.12 if eng == "dve" else 0.25)

    def TT(eng, out, in0, in1, op, r, w):
        P.op(eng, lambda e: e.tensor_tensor(out=out, in0=in0, in1=in1, op=op), reads=r, writes=w, cost=vcost(eng, out))

    def TS(eng, out, in0, s1, s2, op0, op1, r, w):
        if op1 is None:
            P.op(eng, lambda e: e.tensor_scalar(out=out, in0=in0, scalar1=s1, scalar2=None, op0=op0), reads=r, writes=w, cost=vcost(eng, out))
        else:
            P.op(eng, lambda e: e.tensor_scalar(out=out, in0=in0, scalar1=s1, scalar2=s2, op0=op0, op1=op1), reads=r, writes=w, cost=vcost(eng, out))

    def STT(eng, out, in0, scalar, in1, op0, op1, r, w):
        P.op(eng, lambda e: e.scalar_tensor_tensor(out=out, in0=in0, scalar=scalar, in1=in1, op0=op0, op1=op1), reads=r, writes=w, cost=vcost(eng, out))

    def CP(eng, out, in_, r, w):
        P.op(eng, lambda e: e.tensor_copy(out=out, in_=in_), reads=r, writes=w, cost=vcost(eng, out))

    def MEMSET(eng, ap, val, w):
        P.op(eng, lambda e: e.memset(ap, val), writes=w, cost=vcost(eng, ap))

    def RECIP(out, in_, r, w):
        P.op("dve", lambda e: e.reciprocal(out=out, in_=in_), reads=r, writes=w, cost=vcost("dve", out))

    PP = [es.enter_context(nc.psum_tensor("PP%d" % i, [128, 1024], F32)) for i in range(4)]

    def bank(i):
        return PP[i // 2][:, (i % 2) * 512:(i % 2 + 1) * 512]

    def bankb(i):
        return PP[i // 2][:].bitcast(BF16)[:, (i % 2) * 1024:(i % 2 + 1) * 1024]

    def bn(i):
        return ("PP%d" % (i // 2), i % 2)

    identb = sb("identb", (128, 128), BF16)
    identf = sb("identf", (128, 128), F32)
    tri = sb("tri", (128, 128), F32)
    onesf = sb("onesf", (128, 128), F32)
    onesb = sb("onesb", (128, 128), BF16)
    maskneg = sb("maskneg", (128, 128), BF16)
    invf = sb("invf", (32, 2), F32)
    ohq = sb("ohq", (128, 4), F32)
    for t, d_, nm in ((identb, c_identb, "identb"), (identf, c_identf, "identf"), (tri, c_tri, "tri"),
                      (onesf, c_onesf, "onesf"), (maskneg, c_maskneg, "maskneg"),
                      (invf, c_invf, "invf"), (ohq, ohq_d, "ohq")):
        DMA("sp", t[:], d_[:, :], [], [nm], "c_" + nm)
    CP("dve", onesb[:], onesf[:], ["onesf"], ["onesb"])

    def pvec(name, n):
        t = sb("pv_" + name, (128, n // 128), F32)
        DMA("sp", t[:], vd[name].rearrange("o (k p) -> p (o k)", p=128), [], ["pv_" + name], "pv_" + name,
            allow_slow_non_contiguous=True)
        return t

    g_mix = pvec("norm_mix", 1024)
    g_ssd = pvec("ssd_norm", 2048)
    g_qa = pvec("q_a_norm", 384)
    g_kv = pvec("kv_a_norm", 256)
    g_xa = pvec("norm_xattn", 1024)
    g_mem = pvec("norm_mem", 1024)
    g_moe = pvec("norm_moe", 1024)
    cb_p = pvec("conv_b", 3072)
    cw_p = sb("cw_p", (128, 24, 4), F32)
    for k in range(4):
        DMA("sp", cw_p[:, :, k], conv_w_d[k:k + 1, :].rearrange("o (k p) -> p (o k)", p=128), [], [("cw_p", k)],
            "cw_p%d" % k, allow_slow_non_contiguous=True)

    def bvec(name, n):
        t = sb("bv_" + name, (128, n), F32)
        DMA("sp", t[:], vd[name].partition_broadcast(128), [], ["bv_" + name], "bv_" + name)
        return t

    dtb_bc = bvec("dt_bias", 32)
    alog_bc = bvec("a_log", 32)
    dsk_bc = bvec("d_skip", 32)
    a_bc = sb("a_bc", (128, 32), F32)
    ACT(a_bc[:], alog_bc[:], AF.Exp, ["bv_a_log"], ["a_bc"])
    TS("dve", a_bc[:], a_bc[:], -1.0, None, ALU.mult, None, ["a_bc"], ["a_bc"])

    def cast_weight(name, rows_first=None):
        src, dst = wd[name], wb[name]
        R, C = src.shape
        i = 0
        for r0 in range(0, R, 512):
            r1 = min(R, r0 + 512)
            for c0 in range(0, C, 2048):
                c1 = min(C, c0 + 2048)
                DMA("pool", dst[r0:r1, c0:c1], src[r0:r1, c0:c1], [], [("wb_" + name, i)], "cast_" + name)
                i += 1

    for n, _ in WEIGHTS:
        cast_weight(n)

    def rope_tables(pos_ap, cs1, cs2, tmp_i, tmp_f, tmp_a, nm):
        DMA("sp", tmp_i[:], pos_ap.partition_broadcast(32), [], [nm + "i"], nm + "pos")
        CP("dve", tmp_f[:], tmp_i[:], [nm + "i"], [nm + "f"])
        TS("dve", tmp_f[:], tmp_f[:], invf[:, 0:1], None, ALU.mult, None, [nm + "f", "invf"], [nm + "f"])
        for which, dst in ((0, cs2), (1, cs1)):
            src = tmp_f
            if which == 1:
                TS("dve", tmp_a[:], tmp_f[:], float(np.pi / 2), None, ALU.add, None, [nm + "f"], [nm + "a"])
                src = tmp_a
            srcn = nm + ("a" if which == 1 else "f")
            TS("dve", tmp_i[:], src[:], float(1.0 / TWO_PI), None, ALU.mult, None, [srcn], [nm + "i"])
            CP("dve", dst[:], tmp_i[:], [nm + "i"], [nm + "c%d" % which])
            STT("dve", tmp_a[:], dst[:], -CW1, src[:], ALU.mult, ALU.add, [nm + "c%d" % which, srcn], [nm + "a"])
            STT("dve", tmp_a[:], dst[:], -CW2, tmp_a[:], ALU.mult, ALU.add, [nm + "c%d" % which, nm + "a"], [nm + "a"])
            TS("dve", tmp_a[:], tmp_a[:], float(np.pi), float(-np.pi), ALU.min, ALU.max, [nm + "a"], [nm + "a"])
            ACT(dst[:], tmp_a[:], AF.Sin, [nm + "a"], [nm + "c%d" % which])
        TS("dve", cs2[:], cs2[:], invf[:, 1:2], None, ALU.mult, None, [nm + "c0", "invf"], [nm + "c0"])

    def norm_transpose(src_f32, ncols, gain_p, dstT, dst_cols, names_r, name_dst, scr, pbank, tag, gname):
        junk, ssq, rstd, xs = scr
        nk = ncols // 128
        ACT(junk[:, 0:ncols], src_f32, AF.Square, names_r, [tag + "junk", tag + "ssq"], accum=ssq[:])
        ACT(rstd[:], ssq[:], AF.Sqrt, [tag + "ssq", "eps_t"], [tag + "rstd"], bias=eps_t[:], scale=1.0 / ncols)
        RECIP(rstd[:], rstd[:], [tag + "rstd"], [tag + "rstd"])
        TS("dve", xs[:, 0:ncols], src_f32, rstd[:, 0:1], None, ALU.mult, None, names_r + [tag + "rstd"], [tag + "xs"])
        pv = bankb(pbank)
        for k in range(nk):
            TR(pv[:, k * 128:(k + 1) * 128], xs[:, k * 128:(k + 1) * 128], identb[:], [tag + "xs", "identb"], [bn(pbank)])
        TT("dve", dstT[:, :, dst_cols], pv[:, 0:ncols].rearrange("p (k t) -> p k t", k=nk),
           gain_p[:, 0:nk].unsqueeze(2).to_broadcast([128, nk, 128]), ALU.mult,
           [bn(pbank), gname], [name_dst])

    eps_t = sb("eps_t", (128, 1), F32)
    MEMSET("dve", eps_t[:], RMS_EPS, ["eps_t"])

    if 1 in phases:
        with contextlib.ExitStack() as s1:
            WxB = sb("WxB", (128, 8, 2560), BF16, s1)
            Wdt = sb("Wdt", (128, 8, 32), BF16, s1)
            Wckv = sb("Wckv", (128, 8, 288), BF16, s1)
            Wkr = sb("Wkr", (128, 8, 64), BF16, s1)
            wkn = sb("wkn", (128, 2, 1024), BF16, s1)
            wv = sb("wv", (128, 2, 1024), BF16, s1)
            win_v = wb["w_in"].rearrange("(k p) c -> p k c", p=128)
            DMA("sp", WxB[:], win_v[:, :, X0:X0 + 2560], ["wb_w_in"], ["WxB"], "WxB")
            DMA("sp", Wdt[:], win_v[:, :, DT0:DT0 + 32], ["wb_w_in"], ["Wdt"], "Wdt")
            DMA("sp", Wckv[:], win_v[:, :, CKV0:CKV0 + 288], ["wb_w_in"], ["Wckv"], "Wckv")
            DMA("sp", Wkr[:, :, 0:32], win_v[:, :, KR0:KR0 + 32], ["wb_w_in"], [("Wkr", 0)], "Wkr")
            DMA("sp", Wkr[:, :, 32:48], win_v[:, :, KR0 + 16:KR0 + 32], ["wb_w_in"], [("Wkr", 1)], "Wkr")
            DMA("sp", Wkr[:, :, 48:64], win_v[:, :, KR0:KR0 + 16], ["wb_w_in"], [("Wkr", 2)], "Wkr")
            wkv_v = wb["w_kv_b"].rearrange("(k p) (h e) -> p k h e", p=128, e=128)
            for kb in range(2):
                DMA("sp", wkn[:, kb, :].rearrange("p (h e) -> p h e", e=64), wkv_v[:, kb, :, 0:64], ["wb_w_kv_b"], [("wkn", kb)], "wkn")
                DMA("sp", wv[:, kb, :].rearrange("p (h e) -> p h e", e=64), wkv_v[:, kb, :, 64:128], ["wb_w_kv_b"], [("wv", kb)], "wv")

            xin = [sb("xin%d" % i, (128, 1024), F32, s1) for i in range(3)]
            junk = sb("junk1", (128, 1024), BF16, s1)
            ssq = sb("ssq1", (128, 1), F32, s1)
            rstd = sb("rstd1", (128, 1), F32, s1)
            xs = sb("xs1", (128, 1024), BF16, s1)
            uT = sb("uT1", (128, 8, 512), BF16, s1)
            rb = [sb("rb%d" % i, (128, 515), BF16, s1) for i in range(2)]
            hal = sb("hal", (128, 20, 3), BF16, s1)
            cdiag = sb("cdiag", (128, 20, 4, 128), BF16, s1)
            for blk in range(20):
                for k in range(4):
                    TS("dve" if (blk + k) % 2 == 0 else "pool", cdiag[:, blk, k, :], identf[:], cw_p[:, blk, k:k + 1], None,
                       ALU.mult, None, ["identf", "cw_p"], [("cdiag", blk * 4 + k)])
            xsT = sb("xsT1", (128, 20, 512), BF16, s1)
            dtr = sb("dtr1", (128, 32), F32, s1)
            dt_ = sb("dt1", (128, 32), F32, s1)
            adt = sb("adt1", (128, 32), F32, s1)
            acum = sb("acum1", (128, 32), F32, s1)
            tmp32 = sb("tmp32", (128, 32), F32, s1)
            dte = sb("dte1", (128, 32), F32, s1)
            cd = sb("cd1", (128, 32), F32, s1)
            wdt = sb("wdt1", (128, 32), F32, s1)
            xw = sb("xw1", (128, 2048), BF16, s1)
            Btm = sb("Btm1", (128, 512), BF16, s1)
            Sst = sb("Sst", (128, 2048), F32, s1)
            Sb = sb("Sb", (128, 2048), BF16, s1)
            OS = [sb("OS%d" % i, (128, 2048), BF16, s1) for i in range(4)]
            tmpb = sb("tmpb", (128, 512), F32, s1)
            junk2 = sb("junk2", (128, 256), F32, s1)
            ssq2 = sb("ssq2", (128, 1), F32, s1)
            rstd2 = sb("rstd2", (128, 1), F32, s1)
            cn = sb("cn1", (128, 256), BF16, s1)
            cnT = sb("cnT1", (128, 2, 512), BF16, s1)
            cs1 = sb("cs1_1", (32, 512), F32, s1)
            cs2 = sb("cs2_1", (32, 512), F32, s1)
            rt_i = sb("rt_i1", (32, 512), I32, s1)
            rt_f = sb("rt_f1", (32, 512), F32, s1)
            rt_a = sb("rt_a1", (32, 512), F32, s1)
            kpe = sb("kpe1", (32, 512), BF16, s1)
            kst = [sb("kst%d" % i, (128, 512), BF16, s1) for i in range(2)]
            vst = [sb("vst%d" % i, (128, 16, 65), BF16, s1) for i in range(2)]

            MEMSET("dve", hal[:], 0.0, ["hal"])
            MEMSET("dve", Sst[:], 0.0, ["Sst"])
            MEMSET("dve", Sb[:], 0.0, ["Sb"])
            for i in range(4):
                MEMSET("pool", OS[i][:], 0.0, ["OS%d" % i])
            for i in range(2):
                MEMSET("pool", vst[i][:], 1.0, ["vst%d" % i])

            for g in range(NT):
                rope_tables(posb[0:1, g * 512:(g + 1) * 512], cs1, cs2, rt_i, rt_f, rt_a, "rt1")
                for c in range(4):
                    xi = (g * 4 + c) % 3
                    xt = xin[xi]
                    xtn = "xin%d" % xi
                    DMA("sp", xt[:], xb[g * 512 + c * 128:g * 512 + (c + 1) * 128, :], [], [xtn], xtn)
                    norm_transpose(xt[:], 1024, g_mix, uT, slice(c * 128, (c + 1) * 128), [xtn], ("uT1", c),
                                   (junk, ssq, rstd, xs), 0, "n1", "pv_norm_mix")
                for blk in range(20):
                    pb_ = 2 + (blk % 2)
                    for k in range(8):
                        MM(bank(pb_), WxB[:, k, blk * 128:(blk + 1) * 128], uT[:, k, :], k == 0, k == 7,
                           ["WxB", "uT1"], [bn(pb_)])
                    r_ = rb[blk % 2]
                    rn = "rb%d" % (blk % 2)
                    CP("dve", r_[:, 0:3], hal[:, blk, :], [("hal", blk)], [(rn, 0)])
                    ACT(r_[:, 3:515], bank(pb_), AF.Copy, [bn(pb_)], [(rn, 1)])
                    CP("dve", hal[:, blk, :], r_[:, 512:515], [(rn, 1)], [("hal", blk)])
                    for k in range(4):
                        MM(bank(4), cdiag[:, blk, k, :], r_[:, k:k + 512], k == 0, k == 3, [rn, ("cdiag", blk * 4 + k)], [bn(4)])
                    ACT(xsT[:, blk, :], bank(4), AF.Silu, [bn(4), "pv_conv_b"], [("xsT1", blk)], bias=cb_p[:, blk:blk + 1])
                for v_ in range(2):
                    for k in range(8):
                        MM(bank(2 + v_)[0:32, :], Wkr[:, k, v_ * 32:(v_ + 1) * 32], uT[:, k, :], k == 0, k == 7,
                           ["Wkr", "uT1"], [bn(2 + v_)])
                TT("dve", rt_f[:], bank(2)[0:32, :], cs1[:], ALU.mult, [bn(2), "rt1c1"], ["rt1f"])
                TT("dve", rt_a[:], bank(3)[0:32, :], cs2[:], ALU.mult, [bn(3), "rt1c0"], ["rt1a"])
                TT("dve", kpe[:], rt_f[:], rt_a[:], ALU.add, ["rt1f", "rt1a"], ["kpe1"])
                for h in range(16):
                    DMA("sp", KT[h, 0:32, g * 512:(g + 1) * 512], kpe[:], ["kpe1"], [("KT", g)], "KTpe")
                for c in range(4):
                    cc = slice(c * 128, (c + 1) * 128)
                    b7 = bank(7)
                    for k in range(8):
                        MM(b7[:, 0:32], uT[:, k, cc], Wdt[:, k, :], k == 0, k == 7, ["uT1", "Wdt"], [bn(7)])
                    TT("dve", dtr[:], b7[:, 0:32], dtb_bc[:], ALU.add, [bn(7), "bv_dt_bias"], ["dtr1"])
                    ACT(dtr[:], dtr[:], AF.Exp, ["dtr1"], ["dtr1"])
                    ACT(dt_[:], dtr[:], AF.Ln, ["dtr1"], ["dt1"], bias=1.0)
                    TT("dve", adt[:], dt_[:], a_bc[:], ALU.mult, ["dt1", "a_bc"], ["adt1"])
                    MM(b7[:, 32:64], tri[:], adt[:], True, True, ["tri", "adt1"], [bn(7)])
                    MM(b7[:, 64:96], onesf[:], adt[:], True, True, ["onesf", "adt1"], [bn(7)])
                    ACT(acum[:], b7[:, 32:64], AF.Copy, [bn(7)], ["acum1"])
                    TT("dve", tmp32[:], b7[:, 64:96], acum[:], ALU.subtract, [bn(7), "acum1"], ["tmp32"])
                    ACT(dte[:], tmp32[:], AF.Exp, ["tmp32"], ["dte1"])
                    ACT(cd[:], b7[:, 64:96], AF.Exp, [bn(7)], ["cd1"])
                    TT("dve", wdt[:], dt_[:], dte[:], ALU.mult, ["dt1", "dte1"], ["wdt1"])
                    pxv = PP[2][:].bitcast(BF16)
                    for blk in range(16):
                        TR(pxv[:, blk * 128:(blk + 1) * 128], xsT[:, blk, cc], identb[:], [("xsT1", blk), "identb"],
                           [("PP2", blk // 8)])
                    TT("dve", xw[:].rearrange("p (h e) -> p h e", e=64), pxv.rearrange("p (h e) -> p h e", e=64),
                       wdt[:, :].unsqueeze(2).to_broadcast([128, 32, 64]), ALU.mult, ["PP2", "wdt1"], ["xw1"])
                    pbv = bankb(0)
                    for blk in range(4):
                        TR(pbv[:, blk * 128:(blk + 1) * 128], xsT[:, 16 + blk, cc], identb[:], [("xsT1", 16 + blk), "identb"],
                           [bn(0)])
                    ACT(Btm[:], pbv[:, 0:512], AF.Copy, [bn(0)], ["Btm1"])
                    ci = c
                    if g % 4 == 0:
                        MEMSET("pool", OS[ci][:], 0.0, ["OS%d" % ci])
                    STT("dve", OS[ci][:], Sb[:], ohq[:, (g % 4):(g % 4) + 1], OS[ci][:], ALU.mult, ALU.add,
                        ["Sb", "ohq", "OS%d" % ci], ["OS%d" % ci])
                    for g4 in range(4):
                        gs = slice(g4 * 512, (g4 + 1) * 512)
                        MM(bank(6), Btm[:, g4 * 128:(g4 + 1) * 128], xw[:, gs], True, True, ["Btm1", "xw1"], [bn(6)])
                        TT("pool", Sst[:, gs].rearrange("p (h e) -> p h e", e=64), Sst[:, gs].rearrange("p (h e) -> p h e", e=64),
                           cd[:, g4 * 8:(g4 + 1) * 8].unsqueeze(2).to_broadcast([128, 8, 64]), ALU.mult,
                           [("Sst", g4), "cd1"], [("Sst", g4)])
                        TT("dve", Sst[:, gs], Sst[:, gs], bank(6), ALU.add, [("Sst", g4), bn(6)], [("Sst", g4)])
                        ACT(Sb[:, gs], Sst[:, gs], AF.Copy, [("Sst", g4)], [("Sb", g4)])
                    b1 = bank(1)
                    for k in range(8):
                        MM(b1[:, 0:288], uT[:, k, cc], Wckv[:, k, :], k == 0, k == 7, ["uT1", "Wckv"], [bn(1)])
                    ACT(junk2[:], b1[:, 0:256], AF.Square, [bn(1)], ["junk2", "ssq2"], accum=ssq2[:])
                    ACT(rstd2[:], ssq2[:], AF.Sqrt, ["ssq2"], ["rstd2"], bias=eps_t[:], scale=1.0 / 256)
                    RECIP(rstd2[:], rstd2[:], ["rstd2"], ["rstd2"])
                    TS("dve", cn[:], b1[:, 0:256], rstd2[:, 0:1], None, ALU.mult, None, [bn(1), "rstd2"], ["cn1"])
                    pcv = bankb(1)
                    for k in range(2):
                        TR(pcv[:, 768 + k * 128:768 + (k + 1) * 128], cn[:, k * 128:(k + 1) * 128], identb[:], ["cn1", "identb"],
                           [bn(1)])
                    TT("dve", cnT[:, :, cc], pcv[:, 768:1024].rearrange("p (k t) -> p k t", k=2),
                       g_kv[:, 0:2].unsqueeze(2).to_broadcast([128, 2, 128]), ALU.mult, [bn(1), "pv_kv_a_norm"], [("cnT1", c)])
                    for half in range(2):
                        for kb in range(2):
                            MM(bank(2 + half), cnT[:, kb, cc], wv[:, kb, half * 512:(half + 1) * 512], kb == 0, kb == 1,
                               [("cnT1", c), "wv"], [bn(2 + half)])
                    vs_ = vst[c % 2]
                    vn = "vst%d" % (c % 2)
                    CP("dve", vs_[:, :, 0:64], PP[1][:].rearrange("p (h e) -> p h e", e=64), ["PP1"], [vn])
                    t0 = g * 512 + c * 128
                    DMA("sp", VS[:, t0:t0 + 128, :].rearrange("g t e -> t g e"), vs_[:].rearrange("p (g h) e -> p g (h e)", g=4),
                        [vn], [("VS", g)], "VSw")
                for hp in range(8):
                    for kb in range(2):
                        MM(bank(6), wkn[:, kb, hp * 128:(hp + 1) * 128], cnT[:, kb, :], kb == 0, kb == 1, ["wkn", "cnT1"], [bn(6)])
                    ks_ = kst[hp % 2]
                    kn = "kst%d" % (hp % 2)
                    ACT(ks_[:], bank(6), AF.Copy, [bn(6)], [kn])
                    for hh in range(2):
                        DMA("sp", KT[hp * 2 + hh, 32:96, g * 512:(g + 1) * 512], ks_[hh * 64:(hh + 1) * 64, :], [kn], [("KT", g)], "KTn")
                if g % 4 == 3:
                    j = g // 4
                    for ci in range(4):
                        DMA("sp", OSD[j * 4 + ci, :, :], OS[ci][:], ["OS%d" % ci], [("OSD", j)], "OSDw")
        P.barrier()

    if 2 in phases:
        dbg = {}
        if debug:
            for nm in ("dbg_h1", "dbg_h2", "dbg_h3"):
                dbg[nm] = nc.dram_tensor(nm, [NJ * 512, D], F32, kind="ExternalOutput").ap()
            dbg["dbg_ynT"] = nc.dram_tensor("dbg_ynT", [NJ, 128, 16 * 512], BF16, kind="ExternalOutput").ap()
            dbg["dbg_oT"] = nc.dram_tensor("dbg_oT", [NJ, 64, 16 * 512], BF16, kind="ExternalOutput").ap()
            dbg["dbg_QT"] = nc.dram_tensor("dbg_QT", [NJ, 96, 16 * 512], BF16, kind="ExternalOutput").ap()
            dbg["dbg_comb"] = nc.dram_tensor("dbg_comb", [NJ * 512, 32], F32, kind="ExternalOutput").ap()
        KXD = nc.dram_tensor("KXD", [128, 8 * 256], BF16, kind="Internal").ap()
        VXD = nc.dram_tensor("VXD", [128, 2 * 1024], BF16, kind="Internal").ap()
        DSKD = nc.dram_tensor("DSKD", [128, 32 * 128], BF16, kind="Internal").ap()
        win_v = wb["w_in"].rearrange("(k p) c -> p k c", p=128)
        SCALE = float(96 ** -0.5)
        BIG = 10000.0
        with contextlib.ExitStack() as s2:
            with contextlib.ExitStack() as s0:
                mem_t = sb("mem_t", (128, 1024), F32, s0)
                junk = sb("junk0", (128, 1024), BF16, s0)
                ssq = sb("ssq0", (128, 1), F32, s0)
                rstd = sb("rstd0", (128, 1), F32, s0)
                xs = sb("xs0", (128, 1024), BF16, s0)
                memnT = sb("memnT", (128, 8, 256), BF16, s0)
                Wxkv = sb("Wxkv", (128, 8, 2048), BF16, s0)
                kx_st = sb("kx_st", (128, 8, 256), BF16, s0)
                vx_st = sb("vx_st", (128, 2, 1024), BF16, s0)
                dsk_st = sb("dsk_st", (128, 32, 128), BF16, s0)
                DMA("sp", Wxkv[:], wb["w_xkv"].rearrange("(k p) c -> p k c", p=128), ["wb_w_xkv"], ["Wxkv"], "Wxkv")
                for mb in range(2):
                    DMA("sp", mem_t[:], memb[mb * 128:(mb + 1) * 128, :], [], ["mem_t"], "mem_t")
                    norm_transpose(mem_t[:], 1024, g_mem, memnT, slice(mb * 128, (mb + 1) * 128), ["mem_t"], ("memnT", mb),
                                   (junk, ssq, rstd, xs), 0, "n0", "pv_norm_mem")
                for blk in range(8):
                    pb_ = 2 + blk % 2
                    for k in range(8):
                        MM(bank(pb_)[:, 0:256], Wxkv[:, k, blk * 128:(blk + 1) * 128], memnT[:, k, :], k == 0, k == 7, ["Wxkv", "memnT"], [bn(pb_)])
                    ACT(kx_st[:, blk, :], bank(pb_)[:, 0:256], AF.Copy, [bn(pb_)], [("kx_st", blk)])
                for mb in range(2):
                    for half in range(2):
                        pb_ = 4 + half
                        for k in range(8):
                            MM(bank(pb_), memnT[:, k, mb * 128:(mb + 1) * 128], Wxkv[:, k, 1024 + half * 512:1024 + (half + 1) * 512],
                               k == 0, k == 7, ["Wxkv", "memnT"], [bn(pb_)])
                        ACT(vx_st[:, mb, half * 512:(half + 1) * 512], bank(pb_), AF.Copy, [bn(pb_)], [("vx_st", mb * 2 + half)])
                for h in range(32):
                    TS("dve" if h % 2 == 0 else "pool", dsk_st[:, h, :], identf[:], dsk_bc[:, h:h + 1], None, ALU.mult, None,
                       ["identf", "bv_d_skip"], [("dsk_st", h)])
                DMA("sp", KXD[:, :], kx_st[:].rearrange("p a b -> p (a b)"), ["kx_st"], ["KXD"], "KXD")
                DMA("sp", VXD[:, :], vx_st[:].rearrange("p a b -> p (a b)"), ["vx_st"], ["VXD"], "VXD")
                DMA("sp", DSKD[:, :], dsk_st[:].rearrange("p a b -> p (a b)"), ["dsk_st"], ["DSKD"], "DSKD")
            P.barrier()
            P.checkpoint("S0")

            Wqs = sb("Wqs", (128, 3, 16, 32), BF16, s2)
            wq_v = wb["w_q_b"].rearrange("(k p) (h e) -> p k h e", p=128, e=96)
            for kb in range(3):
                DMA("sp", Wqs[:, kb, :, 0:16], wq_v[:, kb, :, 80:96], ["wb_w_q_b"], [("Wqs", kb)], "Wqs")
                DMA("sp", Wqs[:, kb, :, 16:32], wq_v[:, kb, :, 64:80], ["wb_w_q_b"], [("Wqs", kb)], "Wqs")
            rb_bc = sb("rb_bc", (128, 36), F32, s2)
            DMA("sp", rb_bc[:, 0:4], vd["b_router_group"].partition_broadcast(128), [], [("rb_bc", 0)], "rb_bc")
            DMA("sp", rb_bc[:, 4:36], vd["b_router_expert"].partition_broadcast(128), [], [("rb_bc", 1)], "rb_bc")
            Wr = sb("Wr", (128, 8, 36), BF16, s2)
            DMA("sp", Wr[:, :, 0:4], wb["w_router_group"].rearrange("(k p) c -> p k c", p=128), ["wb_w_router_group"], [("Wr", 0)], "Wr")
            DMA("sp", Wr[:, :, 4:36], wb["w_router_expert"].rearrange("(k p) c -> p k c", p=128), ["wb_w_router_expert"], [("Wr", 1)], "Wr")
            WPA = sb("WPA", (128, 16, 1024), BF16, s2)
            hT = sb("hT", (128, 4, 1024), F32, s2)
            uT = sb("uT2", (128, 8, 512), BF16, s2)
            t1T = sb("t1T", (128, 8, 512), BF16, s2)
            Wst = [None, None]
            wst_gen = [0]

            def alloc_wst(scope):
                wst_gen[0] += 1
                for i in range(2):
                    Wst[i] = sb("Wst%d_%d" % (i, wst_gen[0]), (128, 8, 512), BF16, scope)
            junk = sb("junk2p", (128, 1024), BF16, s2)
            ssq = sb("ssq2p", (128, 1), F32, s2)
            rstd = sb("rstd2p", (128, 1), F32, s2)
            xs = sb("xs2p", (128, 1024), BF16, s2)
            scr = (junk, ssq, rstd, xs)
            wst_i = [0]

            def load_wst(c0, ncols):
                i = wst_i[0] % 2
                wst_i[0] += 1
                DMA("sp", Wst[i][:, :, 0:ncols], win_v[:, :, c0:c0 + ncols], ["wb_w_in"], ["Wst%d" % i], "Wst%d" % i)
                return Wst[i], "Wst%d" % i

            P.checkpoint("P0")
            for j in range(NJ):
                KTN = 4 * j + 4
                DMA("sp", hT[:], xo[j, 3:515, :].rearrange("(c p) d -> p c d", p=128), [], ["hT"], "hT")
                DMA("sp", WPA[:], wb["w_ssd_out"].rearrange("(k p) c -> p k c", p=128), ["wb_w_ssd_out"], ["WPA"], "WPA")
                for c in range(4):
                    norm_transpose(hT[:, c, :], 1024, g_mix, uT, slice(c * 128, (c + 1) * 128), [("hT", c)], ("uT2", c), scr, 0, "n2", "pv_norm_mix")
                with contextlib.ExitStack() as sA:
                    xsT2 = sb("xsT2", (128, 24, 512), BF16, sA)
                    sz = sb("sz", (128, 4, 2048), BF16, sA)
                    ynT = sb("ynT", (128, 16, 512), BF16, sA)
                    dt_all = sb("dt_all", (128, 4, 32), F32, sA)
                    ac_all = sb("ac_all", (128, 4, 32), F32, sA)
                    ea_all = sb("ea_all", (128, 4, 32), F32, sA)
                    acT = sb("acT", (32, 4, 128), F32, sA)
                    nacT = sb("nacT", (32, 4, 128), F32, sA)
                    with contextlib.ExitStack() as sA1:
                        alloc_wst(sA1)
                        xh = sb("xh", (3, 1024), F32, sA1)
                        xhs = sb("xhs", (3, 1024), BF16, sA1)
                        hj = sb("hj", (3, 1024), BF16, sA1)
                        hss = sb("hss", (3, 1), F32, sA1)
                        hrs = sb("hrs", (3, 1), F32, sA1)
                        uTh = sb("uTh", (128, 8, 4), BF16, sA1)
                        rb = [sb("rb2_%d" % i, (128, 515), BF16, sA1) for i in range(2)]
                        cacc2 = [sb("cacc2_%d" % i, (128, 512), F32, sA1) for i in range(2)]
                        Wdt2 = sb("Wdt2", (128, 8, 32), BF16, sA1)
                        dtr = sb("dtr2", (128, 32), F32, sA1)
                        adt = sb("adt2", (128, 32), F32, sA1)
                        DMA("sp", Wdt2[:], win_v[:, :, DT0:DT0 + 32], ["wb_w_in"], ["Wdt2"], "Wdt2")
                        DMA("sp", xh[:], xo[j, 0:3, :], [], ["xh"], "xh")
                        ACT(hj[:], xh[:], AF.Square, ["xh"], ["hj", "hss"], accum=hss[:])
                        ACT(hrs[:], hss[:], AF.Sqrt, ["hss"], ["hrs"], bias=eps_t[0:3, :], scale=1.0 / 1024)
                        RECIP(hrs[:], hrs[:], ["hrs"], ["hrs"])
                        TS("dve", xhs[:], xh[:], hrs[:, 0:1], None, ALU.mult, None, ["xh", "hrs"], ["xhs"])
                        pv = bankb(1)
                        for k in range(8):
                            TR(pv[:, k * 4:k * 4 + 3], xhs[:, k * 128:(k + 1) * 128], identb[0:3, 0:3], ["xhs", "identb"], [bn(1)])
                        TT("dve", uTh[:, :, 0:3], pv[:, 0:32].rearrange("p (k t) -> p k t", k=8)[:, :, 0:3],
                           g_mix[:, 0:8].unsqueeze(2).to_broadcast([128, 8, 3]), ALU.mult, [bn(1), "pv_norm_mix"], ["uTh"])
                        P.checkpoint("A1h")
                        for unit in range(6):
                            W_, wn = load_wst(X0 + unit * 512, 512)
                            for bi in range(4):
                                blk = unit * 4 + bi
                                pb_ = 2 + (blk % 2)
                                for k in range(8):
                                    MM(bank(pb_), W_[:, k, bi * 128:(bi + 1) * 128], uT[:, k, :], k == 0, k == 7, [wn, "uT2"], [bn(pb_)])
                                for k in range(8):
                                    MM(bank(1)[:, 0:3], W_[:, k, bi * 128:(bi + 1) * 128], uTh[:, k, 0:3], k == 0, k == 7, [wn, "uTh"], [bn(1)])
                                r_ = rb[blk % 2]
                                rn = "rb2_%d" % (blk % 2)
                                CP("dve", r_[:, 0:3], bank(1)[:, 0:3], [bn(1)], [(rn, 0)])
                                ACT(r_[:, 3:515], bank(pb_), AF.Copy, [bn(pb_)], [(rn, 1)])
                                ca_ = cacc2[blk % 2]
                                can = "cacc2_%d" % (blk % 2)
                                TS("dve", ca_[:], r_[:, 0:512], cw_p[:, blk, 0:1], cb_p[:, blk:blk + 1], ALU.mult, ALU.add, [rn, "cw_p", "pv_conv_b"], [can])
                                for k in range(1, 4):
                                    STT("dve", ca_[:], r_[:, k:k + 512], cw_p[:, blk, k:k + 1], ca_[:], ALU.mult, ALU.add, [rn, "cw_p", can], [can])
                                ACT(xsT2[:, blk, :], ca_[:], AF.Silu, [can], [("xsT2", blk)])
                        P.checkpoint("A1x")
                        for zb in range(4):
                            W_, wn = load_wst(Z0 + zb * 512, 512)
                            for c in range(4):
                                pb_ = 5 + (c % 2)
                                for k in range(8):
                                    MM(bank(pb_), uT[:, k, c * 128:(c + 1) * 128], W_[:, k, :], k == 0, k == 7, [wn, "uT2"], [bn(pb_)])
                                ACT(sz[:, c, zb * 512:(zb + 1) * 512], bank(pb_), AF.Silu, [bn(pb_)], [("sz", c)])
                        P.checkpoint("A1z")
                        b7 = bank(7)
                        for c in range(4):
                            cc = slice(c * 128, (c + 1) * 128)
                            for k in range(8):
                                MM(b7[:, 0:32], uT[:, k, cc], Wdt2[:, k, :], k == 0, k == 7, ["uT2", "Wdt2"], [bn(7)])
                            TT("dve", dtr[:], b7[:, 0:32], dtb_bc[:], ALU.add, [bn(7), "bv_dt_bias"], ["dtr2"])
                            ACT(dtr[:], dtr[:], AF.Exp, ["dtr2"], ["dtr2"])
                            ACT(dt_all[:, c, :], dtr[:], AF.Ln, ["dtr2"], [("dt_all", c)], bias=1.0)
                            TT("dve", adt[:], dt_all[:, c, :], a_bc[:], ALU.mult, [("dt_all", c), "a_bc"], ["adt2"])
                            MM(b7[:, 32:64], tri[:], adt[:], True, True, ["tri", "adt2"], [bn(7)])
                            ACT(ac_all[:, c, :], b7[:, 32:64], AF.Copy, [bn(7)], [("ac_all", c)])
                            ACT(ea_all[:, c, :], b7[:, 32:64], AF.Exp, [bn(7)], [("ea_all", c)])
                            TR(b7[0:32, 128:256], ac_all[:, c, :], identf[:], [("ac_all", c), "identf"], [bn(7)])
                            ACT(acT[:, c, :], b7[0:32, 128:256], AF.Copy, [bn(7)], [("acT", c)])
                            TS("dve", nacT[:, c, :], acT[:, c, :], -1.0, None, ALU.mult, None, [("acT", c)], [("nacT", c)])
                    P.barrier()
                    P.checkpoint("A1")
                    with contextlib.ExitStack() as sA2:
                        selh = sb("selh", (32, 32 * 128), F32, sA2)
                        dskI = sb("dskI", (128, 32 * 128), BF16, sA2)
                        DMA("sp", selh[:], c_selh[:, :], [], ["selh"], "selh")
                        DMA("sp", dskI[:], DSKD[:, :], ["DSKD"], ["dskI"], "dskI")
                        x_tm = sb("x_tm", (128, 2048), BF16, sA2)
                        xdt = sb("xdt", (128, 2048), BF16, sA2)
                        OSs = [sb("OSs%d" % i, (128, 2048), BF16, sA2) for i in range(2)]
                        CBT = sb("CBT", (128, 4, 128), BF16, sA2)
                        E4 = [sb("E4_%d" % i, (128, 4, 128), BF16, sA2) for i in range(2)]
                        M4 = [sb("M4_%d" % i, (128, 4, 128), BF16, sA2) for i in range(2)]
                        tsb = [sb("tsb%d" % i, (128, 512), F32, sA2) for i in range(2)]
                        ssqg = sb("ssqg", (128, 4), F32, sA2)
                        rsg = sb("rsg", (128, 4), F32, sA2)
                        jg = sb("jg", (128, 512), BF16, sA2)
                        yn = sb("yn", (128, 2048), BF16, sA2)
                        for c in range(4):
                            cc = slice(c * 128, (c + 1) * 128)
                            os_ = OSs[c % 2]
                            osn = "OSs%d" % (c % 2)
                            DMA("sp", os_[:], OSD[j * 4 + c, :, :], ["OSD"], [osn], osn)
                            pxv = PP[2][:].bitcast(BF16)
                            for blk in range(16):
                                TR(pxv[:, blk * 128:(blk + 1) * 128], xsT2[:, blk, cc], identb[:], [("xsT2", blk), "identb"], [("PP2", blk // 8)])
                            ACT(x_tm[:], pxv, AF.Copy, ["PP2"], ["x_tm"])
                            TT("dve", xdt[:].rearrange("p (h e) -> p h e", e=64), pxv.rearrange("p (h e) -> p h e", e=64),
                               dt_all[:, c, :].unsqueeze(2).to_broadcast([128, 32, 64]), ALU.mult, ["PP2", ("dt_all", c)], ["xdt"])
                            for g4 in range(4):
                                MM(bank(6)[:, g4 * 128:(g4 + 1) * 128], xsT2[:, 16 + g4, cc], xsT2[:, 20 + g4, cc], True, True,
                                   [("xsT2", 16 + g4), ("xsT2", 20 + g4)], [bn(6)])
                            ACT(CBT[:].rearrange("p a b -> p (a b)"), bank(6), AF.Copy, [bn(6)], ["CBT"])
                            for g4 in range(4):
                                gs = slice(g4 * 512, (g4 + 1) * 512)
                                yb = 2 + (g4 % 2)
                                for hq2 in range(2):
                                    hq = g4 * 2 + hq2
                                    sbk = hq % 2
                                    for hh in range(4):
                                        h = hq * 4 + hh
                                        reg = bank(sbk)[:, hh * 128:(hh + 1) * 128]
                                        MM(reg, selh[:, h * 128:(h + 1) * 128], acT[:, c, :], True, False, ["selh", ("acT", c)], [bn(sbk)])
                                        MM(reg, nacT[:, c, :], selh[:, h * 128:(h + 1) * 128], False, False, ["selh", ("nacT", c)], [bn(sbk)])
                                        MM(reg, identb[:], maskneg[:], False, True, ["identb", "maskneg"], [bn(sbk)])
                                    e4 = E4[hq % 2]
                                    en = "E4_%d" % (hq % 2)
                                    m4 = M4[hq % 2]
                                    mn = "M4_%d" % (hq % 2)
                                    ACT(e4[:].rearrange("p a b -> p (a b)"), bank(sbk), AF.Exp, [bn(sbk)], [en])
                                    TT("dve", m4[:], e4[:], CBT[:, g4:g4 + 1, :].to_broadcast([128, 4, 128]), ALU.mult, [en, "CBT"], [mn])
                                    for hh in range(4):
                                        h = hq * 4 + hh
                                        yreg = bank(yb)[:, (h % 8) * 64:(h % 8 + 1) * 64]
                                        MM(yreg, m4[:, hh, :], xdt[:, h * 64:(h + 1) * 64], True, False, [mn, "xdt"], [bn(yb)])
                                        MM(yreg, dskI[:, h * 128:(h + 1) * 128], x_tm[:, h * 64:(h + 1) * 64], False, True, ["dskI", "x_tm"], [bn(yb)])
                                MM(bank(7), xsT2[:, 20 + g4, cc], os_[:, gs], True, True, [("xsT2", 20 + g4), osn], [bn(7)])
                                t_ = tsb[g4 % 2]
                                tn = "tsb%d" % (g4 % 2)
                                TT("dve", t_[:].rearrange("p (h e) -> p h e", e=64), bank(7).rearrange("p (h e) -> p h e", e=64),
                                   ea_all[:, c, g4 * 8:(g4 + 1) * 8].unsqueeze(2).to_broadcast([128, 8, 64]), ALU.mult,
                                   [bn(7), ("ea_all", c)], [tn])
                                TT("dve", t_[:], t_[:], bank(yb), ALU.add, [tn, bn(yb)], [tn])
                                TT("dve", t_[:], t_[:], sz[:, c, gs], ALU.mult, [tn, ("sz", c)], [tn])
                                ACT(jg[:], t_[:], AF.Square, [tn], ["jg", ("ssqg", g4)], accum=ssqg[:, g4:g4 + 1])
                                ACT(rsg[:, g4:g4 + 1], ssqg[:, g4:g4 + 1], AF.Sqrt, [("ssqg", g4)], [("rsg", g4)], bias=eps_t[:], scale=1.0 / 512)
                                RECIP(rsg[:, g4:g4 + 1], rsg[:, g4:g4 + 1], [("rsg", g4)], [("rsg", g4)])
                                ACT(yn[:, gs], t_[:], AF.Copy, [tn, ("rsg", g4)], [("yn", g4)], scale=rsg[:, g4:g4 + 1])
                            for blk in range(16):
                                TR(pxv[:, blk * 128:(blk + 1) * 128], yn[:, blk * 128:(blk + 1) * 128], identb[:], ["yn", "identb"], [("PP2", blk // 8)])
                            TT("dve", ynT[:, :, cc], pxv.rearrange("p (k t) -> p k t", k=16),
                               g_ssd[:, 0:16].unsqueeze(2).to_broadcast([128, 16, 128]), ALU.mult, ["PP2", "pv_ssd_norm"], [("ynT", c)])
                    P.barrier()
                    P.checkpoint("A2")
                    if debug:
                        DMA("sp", dbg["dbg_ynT"][j, :, :], ynT[:].rearrange("p a b -> p (a b)"), ["ynT"], [], "dbg_ynT")
                    with contextlib.ExitStack() as sA3:
                        alloc_wst(sA3)
                        Wso = WPA
                        sg = [sb("sg%d" % i, (128, 512), F32, sA3) for i in range(2)]
                        for unit in range(2):
                            W_, wn = load_wst(G10 + unit * 512, 512)
                            for bi in range(4):
                                cb_ = unit * 4 + bi
                                yb = 2 + cb_ % 2
                                gb = 5 + cb_ % 2
                                for kb in range(16):
                                    MM(bank(yb), Wso[:, kb, cb_ * 128:(cb_ + 1) * 128], ynT[:, kb, :], kb == 0, kb == 15, ["WPA", "ynT"], [bn(yb)])
                                for k in range(8):
                                    MM(bank(gb), W_[:, k, bi * 128:(bi + 1) * 128], uT[:, k, :], k == 0, k == 7, [wn, "uT2"], [bn(gb)])
                                s_ = sg[cb_ % 2]
                                sn = "sg%d" % (cb_ % 2)
                                ACT(s_[:], bank(gb), AF.Sigmoid, [bn(gb)], [sn])
                                TT("dve", t1T[:, cb_, :], s_[:], bank(yb), ALU.mult, [sn, bn(yb)], [("t1T", cb_)])
                P.barrier()
                P.checkpoint("A3")
                with contextlib.ExitStack() as sBC:
                    oT = sb("oT", (64, 16, 512), BF16, sBC)
                    DMA("sp", WPA[0:64, :, :], wb["w_mla_out"].rearrange("(h e) c -> e h c", e=64), ["wb_w_mla_out"], ["WPA"], "WPA")
                    with contextlib.ExitStack() as sB:
                        QT = sb("QT", (96, 16, 512), BF16, sB)
                        maskb = sb("maskb", (128, 16, 512), BF16, sB)
                        DMA("sp", maskb[:], maskb_d[:, :, :], [], ["maskb"], "maskb")
                        with contextlib.ExitStack() as sB1:
                            alloc_wst(sB1)
                            qnT = sb("qnT", (128, 3, 512), BF16, sB1)
                            Wqb = sb("Wqb", (128, 3, 1536), BF16, sB1)
                            cs1 = sb("cs1o", (32, 512), F32, sB1)
                            cs2 = sb("cs2o", (32, 512), F32, sB1)
                            rt_i = sb("rt_i2", (32, 512), I32, sB1)
                            rt_f = sb("rt_f2", (32, 512), F32, sB1)
                            rt_a = sb("rt_a2", (32, 512), F32, sB1)
                            qpe = [sb("qpe%d" % i, (32, 512), BF16, sB1) for i in range(2)]
                            qno = [sb("qno%d" % i, (64, 512), BF16, sB1) for i in range(2)]
                            DMA("sp", Wqb[:], wb["w_q_b"].rearrange("(k p) c -> p k c", p=128), ["wb_w_q_b"], ["Wqb"], "Wqb")
                            rope_tables(poso[0:1, j * 512:(j + 1) * 512], cs1, cs2, rt_i, rt_f, rt_a, "rt2")
                            W_, wn = load_wst(QA0, 384)
                            for c in range(4):
                                pb_ = 2 + c % 2
                                for k in range(8):
                                    MM(bank(pb_)[:, 0:384], uT[:, k, c * 128:(c + 1) * 128], W_[:, k, 0:384], k == 0, k == 7, [wn, "uT2"], [bn(pb_)])
                                norm_transpose(bank(pb_)[:, 0:384], 384, g_qa, qnT, slice(c * 128, (c + 1) * 128), [bn(pb_)], ("qnT", c), scr, 1, "n2", "pv_q_a_norm")
                            for h in range(16):
                                pn = 5 + h % 2
                                for kb in range(3):
                                    MM(bank(pn)[0:64, :], Wqb[:, kb, h * 96:h * 96 + 64], qnT[:, kb, :], kb == 0, kb == 2, ["Wqb", "qnT"], [bn(pn)])
                                for kb in range(3):
                                    MM(bank(7)[0:32, :], Wqb[:, kb, h * 96 + 64:h * 96 + 96], qnT[:, kb, :], kb == 0, kb == 2, ["Wqb", "qnT"], [bn(7)])
                                for kb in range(3):
                                    MM(bank(0)[0:32, :], Wqs[:, kb, h, :], qnT[:, kb, :], kb == 0, kb == 2, ["Wqs", "qnT"], [bn(0)])
                                TT("dve", rt_f[:], bank(7)[0:32, :], cs1[:], ALU.mult, [bn(7), "rt2c1"], ["rt2f"])
                                TT("dve", rt_a[:], bank(0)[0:32, :], cs2[:], ALU.mult, [bn(0), "rt2c0"], ["rt2a"])
                                qp_ = qpe[h % 2]
                                qpn = "qpe%d" % (h % 2)
                                qn_ = qno[h % 2]
                                qnn = "qno%d" % (h % 2)
                                TT("dve", qp_[:], rt_f[:], rt_a[:], ALU.add, ["rt2f", "rt2a"], [qpn])
                                ACT(qn_[:], bank(pn)[0:64, :], AF.Copy, [bn(pn)], [qnn])
                                DMA("sp", QT[0:32, h, :], qp_[:], [qpn], [("QT", h)], "QTp%d" % (h % 2))
                                DMA("sp", QT[32:96, h, :], qn_[:], [qnn], [("QT", h)], "QTn%d" % (h % 2))
                        P.barrier()
                        P.checkpoint("B1")
                        if debug:
                            DMA("sp", dbg["dbg_QT"][j, :, :], QT[:].rearrange("p a b -> p (a b)"), ["QT"], [], "dbg_QT")
                        with contextlib.ExitStack() as sB3:
                            KTt = [sb("KTt%d" % i, (96, 4, 512), BF16, sB3) for i in range(3)]
                            Vt = [sb("Vt%d" % i, (128, 4, 260), BF16, sB3) for i in range(3)]
                            PT = [sb("PT%d" % i, (128, 1024), BF16, sB3) for i in range(6)]
                            ost = [sb("ost%d" % i, (65, 512), F32, sB3) for i in range(2)]
                            rr = [sb("rr%d" % i, (65, 512), F32, sB3) for i in range(2)]
                            sci = 0
                            pti = 0
                            kvi = 0
                            for hg in range(4):
                                for kt in range(KTN):
                                    kb_ = KTt[kvi % 3]
                                    kn_ = "KTt%d" % (kvi % 3)
                                    vb_ = Vt[kvi % 3]
                                    vn_ = "Vt%d" % (kvi % 3)
                                    kvi += 1
                                    DMA("sp", kb_[:], KT[hg * 4:(hg + 1) * 4, :, kt * 512:(kt + 1) * 512].rearrange("h e s -> e h s"),
                                        ["KT"], [kn_], kn_)
                                    DMA("sp", vb_[:], VS[hg, kt * 512:(kt + 1) * 512, :].rearrange("(kb p) e -> p kb e", p=128),
                                        ["VS"], [vn_], vn_)
                                    masked = kt >= 4 * j
                                    for hh in range(4):
                                        h = hg * 4 + hh
                                        for kbp in range(2):
                                            pi = sci % 2
                                            sci += 1
                                            for kk_ in range(2):
                                                kb = kbp * 2 + kk_
                                                sc = pi * 2 + kk_
                                                MM(bank(sc), kb_[0:96, hh, kb * 128:(kb + 1) * 128], QT[0:96, h, :], True, True,
                                                   [kn_, ("QT", h)], [bn(sc)])
                                            p_ = PT[pti % 6]
                                            pn_ = "PT%d" % (pti % 6)
                                            pti += 1
                                            ACT(p_[:], PP[pi][:, :], AF.Exp, ["PP%d" % pi], [pn_], scale=SCALE)
                                            if masked:
                                                mi = (kt - 4 * j) * 4 + kbp * 2
                                                TT("dve", p_[:].rearrange("p (a b) -> p a b", a=2), p_[:].rearrange("p (a b) -> p a b", a=2),
                                                   maskb[:, mi:mi + 2, :], ALU.mult, [pn_, "maskb"], [pn_])
                                            for kk_ in range(2):
                                                kb = kbp * 2 + kk_
                                                MM(bank(4 + hh)[0:65, :], vb_[:, kb, hh * 65:(hh + 1) * 65], p_[:, kk_ * 512:(kk_ + 1) * 512],
                                                   kt == 0 and kb == 0, kt == KTN - 1 and kb == 3, [vn_, pn_], [bn(4 + hh)])
                                for hh in range(4):
                                    h = hg * 4 + hh
                                    o_ = ost[hh % 2]
                                    on_ = "ost%d" % (hh % 2)
                                    r_ = rr[hh % 2]
                                    rn_ = "rr%d" % (hh % 2)
                                    ACT(o_[:], bank(4 + hh)[0:65, :], AF.Copy, [bn(4 + hh)], [on_])
                                    RECIP(r_[64:65, :], o_[64:65, :], [on_], [rn_])
                                    sc = hh % 4
                                    MM(bank(sc), onesf[64:65, :], r_[64:65, :], True, True, ["onesf", rn_], [bn(sc)])
                                    TT("dve", oT[:, h, :], o_[0:64, :], bank(sc)[0:64, :], ALU.mult, [on_, bn(sc)], [("oT", h)])
                    P.barrier()
                    P.checkpoint("B3")
                    if debug:
                        DMA("sp", dbg["dbg_oT"][j, :, :], oT[:].rearrange("p a b -> p (a b)"), ["oT"], [], "dbg_oT")
                    with contextlib.ExitStack() as sC:
                        alloc_wst(sC)
                        Wmo = WPA
                        Wo = sb("Wo", (128, 8, 1024), BF16, sC)
                        sg2 = [sb("sg2_%d" % i, (128, 512), F32, sC) for i in range(2)]
                        mm_ = [sb("mm_%d" % i, (128, 512), F32, sC) for i in range(2)]
                        mT = sb("mT", (128, 8, 512), BF16, sC)
                        DMA("sp", Wo[:], wb["w_o"].rearrange("(k p) c -> p k c", p=128), ["wb_w_o"], ["Wo"], "Wo")
                        for unit in range(2):
                            W_, wn = load_wst(G20 + unit * 512, 512)
                            for bi in range(4):
                                cb_ = unit * 4 + bi
                                yb = cb_ % 2
                                gb = 2 + cb_ % 2
                                for h in range(16):
                                    MM(bank(yb), Wmo[0:64, h, cb_ * 128:(cb_ + 1) * 128], oT[0:64, h, :], h == 0, h == 15, ["WPA", "oT"], [bn(yb)])
                                for k in range(8):
                                    MM(bank(gb), W_[:, k, bi * 128:(bi + 1) * 128], uT[:, k, :], k == 0, k == 7, [wn, "uT2"], [bn(gb)])
                                s_ = sg2[cb_ % 2]
                                sn = "sg2_%d" % (cb_ % 2)
                                m_ = mm_[cb_ % 2]
                                mn = "mm_%d" % (cb_ % 2)
                                ACT(s_[:], bank(gb), AF.Sigmoid, [bn(gb)], [sn])
                                TT("dve", m_[:], s_[:], bank(yb), ALU.mult, [sn, bn(yb)], [mn])
                                TT("dve", mT[:, cb_, :], m_[:], t1T[:, cb_, :], ALU.add, [mn, ("t1T", cb_)], [("mT", cb_)])
                        for c in range(4):
                            for half in range(2):
                                pb_ = 4 + half
                                for k in range(8):
                                    MM(bank(pb_), mT[:, k, c * 128:(c + 1) * 128], Wo[:, k, half * 512:(half + 1) * 512], k == 0, k == 7,
                                       ["mT", "Wo"], [bn(pb_)])
                                TT("dve", hT[:, c, half * 512:(half + 1) * 512], hT[:, c, half * 512:(half + 1) * 512], bank(pb_), ALU.add,
                                   [("hT", c), bn(pb_)], [("hT", c)])
                P.barrier()
                P.checkpoint("C")
                if debug:
                    DMA("sp", dbg["dbg_h1"][j * 512:(j + 1) * 512, :].rearrange("(c p) d -> p c d", p=128), hT[:], ["hT"], [], "dbg_h1")
                with contextlib.ExitStack() as sD:
                    hnT = sb("hnT", (128, 8, 512), BF16, sD)
                    Wxq = sb("Wxq", (128, 8, 1024), BF16, sD)
                    Wxo = sb("Wxo", (128, 8, 1024), BF16, sD)
                    qxT = sb("qxT", (128, 8, 512), BF16, sD)
                    kxT = sb("kxT", (128, 8, 256), BF16, sD)
                    vx = sb("vx", (128, 2, 1024), BF16, sD)
                    PTx = [sb("PTx%d" % i, (128, 512), BF16, sD) for i in range(2)]
                    rden = sb("rden", (128, 512), F32, sD)
                    oxT = sb("oxT", (128, 8, 512), BF16, sD)
                    DMA("sp", Wxq[:], wb["w_xq"].rearrange("(k p) c -> p k c", p=128), ["wb_w_xq"], ["Wxq"], "Wxq")
                    DMA("sp", Wxo[:], wb["w_xo"].rearrange("(k p) c -> p k c", p=128), ["wb_w_xo"], ["Wxo"], "Wxo")
                    DMA("sp", kxT[:].rearrange("p a b -> p (a b)"), KXD[:, :], ["KXD"], ["kxT"], "kxT")
                    DMA("sp", vx[:].rearrange("p a b -> p (a b)"), VXD[:, :], ["VXD"], ["vx"], "vx")
                    for c in range(4):
                        norm_transpose(hT[:, c, :], 1024, g_xa, hnT, slice(c * 128, (c + 1) * 128), [("hT", c)], ("hnT", c), scr, 0, "n2", "pv_norm_xattn")
                    for blk in range(8):
                        pb_ = 2 + blk % 2
                        for k in range(8):
                            MM(bank(pb_), Wxq[:, k, blk * 128:(blk + 1) * 128], hnT[:, k, :], k == 0, k == 7, ["Wxq", "hnT"], [bn(pb_)])
                        ACT(qxT[:, blk, :], bank(pb_), AF.Copy, [bn(pb_)], [("qxT", blk)])
                    for a in range(4):
                        for mb in range(2):
                            for dc in range(2):
                                MM(bank(mb), kxT[:, a * 2 + dc, mb * 128:(mb + 1) * 128], qxT[:, a * 2 + dc, :], dc == 0, dc == 1,
                                   ["kxT", ("qxT", a * 2 + dc)], [bn(mb)])
                            ACT(PTx[mb][:], bank(mb), AF.Exp, [bn(mb)], ["PTx%d" % mb], scale=float(256 ** -0.5))
                        for mb in range(2):
                            MM(bank(4), onesb[:], PTx[mb][:], mb == 0, mb == 1, ["onesb", "PTx%d" % mb], [bn(4)])
                        RECIP(rden[:], bank(4), [bn(4)], ["rden"])
                        for db in range(2):
                            pb_ = 5 + db
                            for mb in range(2):
                                MM(bank(pb_), vx[:, mb, a * 256 + db * 128:a * 256 + (db + 1) * 128], PTx[mb][:], mb == 0, mb == 1,
                                   ["vx", "PTx%d" % mb], [bn(pb_)])
                            TT("dve", oxT[:, a * 2 + db, :], rden[:], bank(pb_), ALU.mult, ["rden", bn(pb_)], [("oxT", a * 2 + db)])
                    for c in range(4):
                        for half in range(2):
                            pb_ = 2 + half
                            for k in range(8):
                                MM(bank(pb_), oxT[:, k, c * 128:(c + 1) * 128], Wxo[:, k, half * 512:(half + 1) * 512], k == 0, k == 7,
                                   ["oxT", "Wxo"], [bn(pb_)])
                            TT("dve", hT[:, c, half * 512:(half + 1) * 512], hT[:, c, half * 512:(half + 1) * 512], bank(pb_), ALU.add,
                               [("hT", c), bn(pb_)], [("hT", c)])
                P.barrier()
                P.checkpoint("D")
                if debug:
                    DMA("sp", dbg["dbg_h2"][j * 512:(j + 1) * 512, :].rearrange("(c p) d -> p c d", p=128), hT[:], ["hT"], [], "dbg_h2")
                with contextlib.ExitStack() as sE:
                    hnT = sb("hn2T", (128, 8, 512), BF16, sE)
                    selh = sb("selhE", (32, 32 * 128), F32, sE)
                    DMA("sp", selh[:], c_selh[:, :], [], ["selhE"], "selhE")
                    L = sb("L", (128, 36), F32, sE)
                    gmax = sb("gmax", (128, 1), F32, sE)
                    ngmax = sb("ngmax", (128, 1), F32, sE)
                    goh = sb("goh", (128, 4), F32, sE)
                    gj = sb("gj", (128, 4), F32, sE)
                    gsum = sb("gsum", (128, 1), F32, sE)
                    gw = sb("gw", (128, 1), F32, sE)
                    pen = sb("pen", (128, 4), F32, sE)
                    em = sb("em", (128, 32), F32, sE)
                    em2 = sb("em2", (128, 32), F32, sE)
                    m1 = sb("m1", (128, 1), F32, sE)
                    m2 = sb("m2", (128, 1), F32, sE)
                    oh1 = sb("oh1", (128, 32), F32, sE)
                    oh2 = sb("oh2", (128, 32), F32, sE)
                    dd = sb("dd", (128, 1), F32, sE)
                    ee = sb("ee", (128, 1), F32, sE)
                    w1 = sb("w1", (128, 1), F32, sE)
                    w2 = sb("w2", (128, 1), F32, sE)
                    comb = sb("comb", (128, 4, 32), F32, sE)
                    combT = sb("combT", (32, 512), F32, sE)
                    cbs = [sb("cbs%d" % i, (128, 512), F32, sE) for i in range(2)]
                    Wg = [sb("Wg%d" % i, (128, 8, 256), BF16, sE) for i in range(2)]
                    Wu = [sb("Wu%d" % i, (128, 8, 256), BF16, sE) for i in range(2)]
                    Wd = [sb("Wd%d" % i, (128, 8, 1024), BF16, sE) for i in range(2)]
                    sgm = [sb("sgm%d" % i, (128, 512), F32, sE) for i in range(2)]
                    tg = [sb("tg%d" % i, (128, 512), F32, sE) for i in range(2)]
                    actT = [sb("actT%d" % i, (128, 8, 512), BF16, sE) for i in range(2)]
                    for c in range(4):
                        norm_transpose(hT[:, c, :], 1024, g_moe, hnT, slice(c * 128, (c + 1) * 128), [("hT", c)], ("hn2T", c), scr, 0, "n2", "pv_norm_moe")
                    for c in range(4):
                        cc = slice(c * 128, (c + 1) * 128)
                        b7 = bank(7)
                        for k in range(8):
                            MM(b7[:, 0:36], hnT[:, k, cc], Wr[:, k, :], k == 0, k == 7, [("hn2T", c), "Wr"], [bn(7)])
                        TT("dve", L[:], b7[:, 0:36], rb_bc[:], ALU.add, [bn(7), "rb_bc"], ["L"])
                        P.op("dve", lambda e: e.reduce_max(out=gmax[:], in_=L[:, 0:4], axis=mybir.AxisListType.X), reads=["L"], writes=["gmax"])
                        TS("dve", ngmax[:], gmax[:], -1.0, None, ALU.mult, None, ["gmax"], ["ngmax"])
                        TS("dve", goh[:], L[:, 0:4], gmax[:, 0:1], None, ALU.is_equal, None, ["L", "gmax"], ["goh"])
                        ACT(gj[:], L[:, 0:4], AF.Exp, ["L", "ngmax"], ["gj", "gsum"], bias=ngmax[:, 0:1], accum=gsum[:])
                        RECIP(gw[:], gsum[:], ["gsum"], ["gw"])
                        TS("dve", pen[:], goh[:], BIG, -BIG, ALU.mult, ALU.add, ["goh"], ["pen"])
                        TT("dve", em[:].rearrange("p (g e) -> p g e", g=4), L[:, 4:36].rearrange("p (g e) -> p g e", g=4),
                           pen[:, :].unsqueeze(2).to_broadcast([128, 4, 8]), ALU.add, ["L", "pen"], ["em"])
                        P.op("dve", lambda e: e.reduce_max(out=m1[:], in_=em[:], axis=mybir.AxisListType.X), reads=["em"], writes=["m1"])
                        TS("dve", oh1[:], em[:], m1[:, 0:1], None, ALU.is_equal, None, ["em", "m1"], ["oh1"])
                        STT("dve", em2[:], oh1[:], -BIG, em[:], ALU.mult, ALU.add, ["oh1", "em"], ["em2"])
                        P.op("dve", lambda e: e.reduce_max(out=m2[:], in_=em2[:], axis=mybir.AxisListType.X), reads=["em2"], writes=["m2"])
                        TS("dve", oh2[:], em2[:], m2[:, 0:1], None, ALU.is_equal, None, ["em2", "m2"], ["oh2"])
                        TT("dve", dd[:], m2[:], m1[:], ALU.subtract, ["m1", "m2"], ["dd"])
                        ACT(ee[:], dd[:], AF.Exp, ["dd"], ["ee"])
                        TS("dve", w1[:], ee[:], 1.0, None, ALU.add, None, ["ee"], ["w1"])
                        RECIP(w1[:], w1[:], ["w1"], ["w1"])
                        TT("dve", w2[:], ee[:], w1[:], ALU.mult, ["ee", "w1"], ["w2"])
                        TT("dve", w1[:], w1[:], gw[:], ALU.mult, ["w1", "gw"], ["w1"])
                        TT("dve", w2[:], w2[:], gw[:], ALU.mult, ["w2", "gw"], ["w2"])
                        TS("dve", oh1[:], oh1[:], w1[:, 0:1], None, ALU.mult, None, ["oh1", "w1"], ["oh1"])
                        STT("dve", comb[:, c, :], oh2[:], w2[:, 0:1], oh1[:], ALU.mult, ALU.add, ["oh2", "w2", "oh1"], [("comb", c)])
                        P.op("pe", lambda e, c=c: e.transpose(out=bank(6)[0:32, c * 128:(c + 1) * 128], in_=comb[:, c, :], identity=identf[:]),
                             reads=[("comb", c), "identf"], writes=[bn(6)])
                    ACT(combT[:], bank(6)[0:32, :], AF.Copy, [bn(6)], ["combT"])
                    if debug:
                        DMA("sp", dbg["dbg_comb"][j * 512:(j + 1) * 512, :].rearrange("(c p) e -> p c e", p=128), comb[:], ["comb"], [], "dbg_comb")
                    wg_v = wb["w_exp_gate"].rearrange("(e k p) f -> e p k f", e=32, p=128)
                    wu_v = wb["w_exp_up"].rearrange("(e k p) f -> e p k f", e=32, p=128)
                    wd_v = wb["w_exp_down"].rearrange("(g k p) c -> g p k c", g=8, p=128)
                    for eg in range(8):
                        wd_ = Wd[eg % 2]
                        wdn = "Wd%d" % (eg % 2)
                        DMA("sp", wd_[:], wd_v[eg], ["wb_w_exp_down"], [wdn], wdn)
                        at_ = actT[eg % 2]
                        atn = "actT%d" % (eg % 2)
                        for ei in range(4):
                            e_ = eg * 4 + ei
                            wg_ = Wg[e_ % 2]
                            wgn = "Wg%d" % (e_ % 2)
                            wu_ = Wu[e_ % 2]
                            wun = "Wu%d" % (e_ % 2)
                            DMA("sp", wg_[:], wg_v[e_], ["wb_w_exp_gate"], [wgn], wgn)
                            DMA("sp", wu_[:], wu_v[e_], ["wb_w_exp_up"], [wun], wun)
                            MM(bank(6), selh[:, e_ * 128:(e_ + 1) * 128], combT[:], True, True, ["selhE", "combT"], [bn(6)])
                            cb_ = cbs[e_ % 2]
                            cbn = "cbs%d" % (e_ % 2)
                            ACT(cb_[:], bank(6), AF.Copy, [bn(6)], [cbn])
                            for fb in range(2):
                                gbk = fb
                                ubk = 2 + fb
                                for k in range(8):
                                    MM(bank(gbk), wg_[:, k, fb * 128:(fb + 1) * 128], hnT[:, k, :], k == 0, k == 7, [wgn, "hn2T"], [bn(gbk)])
                                for k in range(8):
                                    MM(bank(ubk), wu_[:, k, fb * 128:(fb + 1) * 128], hnT[:, k, :], k == 0, k == 7, [wun, "hn2T"], [bn(ubk)])
                                s_ = sgm[fb]
                                sn = "sgm%d" % fb
                                t_ = tg[fb]
                                tn = "tg%d" % fb
                                ACT(s_[:], bank(gbk), AF.Silu, [bn(gbk)], [sn])
                                TT("dve", t_[:], s_[:], bank(ubk), ALU.mult, [sn, bn(ubk)], [tn])
                                TT("pool", at_[:, ei * 2 + fb, :], t_[:], cb_[:], ALU.mult, [tn, cbn], [(atn, ei * 2 + fb)])
                        for c in range(4):
                            for half in range(2):
                                pb_ = 4 + half
                                for fbk in range(8):
                                    MM(bank(pb_), at_[:, fbk, c * 128:(c + 1) * 128], wd_[:, fbk, half * 512:(half + 1) * 512], fbk == 0, fbk == 7,
                                       [atn, wdn], [bn(pb_)])
                                TT("dve", hT[:, c, half * 512:(half + 1) * 512], hT[:, c, half * 512:(half + 1) * 512], bank(pb_), ALU.add,
                                   [("hT", c), bn(pb_)], [("hT", c)])
                P.barrier()
                P.checkpoint("E")
                if debug:
                    DMA("sp", dbg["dbg_h3"][j * 512:(j + 1) * 512, :].rearrange("(c p) d -> p c d", p=128), hT[:], ["hT"], [], "dbg_h3")
                with contextlib.ExitStack() as sF:
                    nf_bc = sb("nf_bc", (128, 1024), F32, sF)
                    jf = sb("jf", (128, 1024), BF16, sF)
                    DMA("sp", nf_bc[:], vd["norm_final"].partition_broadcast(128), [], ["nf_bc"], "nf_bc")
                    for c in range(4):
                        ACT(jf[:], hT[:, c, :], AF.Square, [("hT", c)], ["jf", "n2ssq"], accum=ssq[:])
                        ACT(rstd[:], ssq[:], AF.Sqrt, ["n2ssq"], ["n2rstd"], bias=eps_t[:], scale=1.0 / 1024)
                        RECIP(rstd[:], rstd[:], ["n2rstd"], ["n2rstd"])
                        STT("dve", hT[:, c, :], hT[:, c, :], rstd[:, 0:1], nf_bc[:], ALU.mult, ALU.mult, [("hT", c), "n2rstd", "nf_bc"], [("hT", c)])
                    DMA("sp", out_d[j * 512:(j + 1) * 512, :].rearrange("(c p) d -> p c d", p=128), hT[:], ["hT"], [], "out")
                P.barrier()

    P.emit()
    es.close()
    return nc


def _consts():
    bf = ml_dtypes.bfloat16
    c = {}
    c["c_identb"] = np.eye(128, dtype=np.float32).astype(bf)
    c["c_identf"] = np.eye(128, dtype=np.float32)
    s = np.arange(128)
    c["c_tri"] = (s[:, None] <= s[None, :]).astype(np.float32)
    c["c_onesf"] = np.ones((128, 128), np.float32)
    c["c_maskneg"] = np.where(s[:, None] > s[None, :], NEG, 0.0).astype(np.float32).astype(bf)
    sel = np.zeros((32, 32, 128), np.float32)
    for h in range(32):
        sel[h, h, :] = 1.0
    c["c_selh"] = sel.reshape(32, 32 * 128)
    half = 16
    inv = (np.float32(10000.0) ** (-(np.arange(half, dtype=np.float32)) / np.float32(half))).astype(np.float32)
    invf = np.zeros((32, 2), np.float32)
    invf[:, 0] = np.concatenate([inv, inv])
    invf[:, 1] = np.concatenate([-np.ones(16), np.ones(16)])
    c["c_invf"] = invf
    return c


def make_in_maps(inputs, S):
    bf = ml_dtypes.bfloat16
    NT = S // 512
    NJ = NT // 4
    x = np.asarray(inputs["x"], np.float32)
    mem = np.asarray(inputs["mem"], np.float32)
    pos = np.asarray(inputs["positions"], np.int32)
    consts = _consts()
    shared = {}
    for n, shp in WEIGHTS:
        shared[n] = np.ascontiguousarray(np.asarray(inputs[n], np.float32).reshape(shp))
    for n, ln in VECS:
        shared[n] = np.ascontiguousarray(np.asarray(inputs[n], np.float32).reshape(1, ln))
    shared["conv_w"] = np.ascontiguousarray(np.asarray(inputs["conv_w"], np.float32).reshape(4, 3072))
    shared.update(consts)
    maps = []
    kk = np.arange(512)
    for core in range(8):
        b, q = core // 4, core % 4
        m = dict(shared)
        m["xb"] = np.ascontiguousarray(x[b])
        xo = np.zeros((NJ, 515, D), np.float32)
        po = np.zeros((1, NJ * 512), np.int32)
        for j in range(NJ):
            t0 = (4 * j + q) * 512
            xo[j, 3:] = x[b, t0:t0 + 512]
            if t0 >= 3:
                xo[j, 0:3] = x[b, t0 - 3:t0]
            po[0, j * 512:(j + 1) * 512] = pos[b, t0:t0 + 512]
        m["xo"] = xo
        m["posb"] = np.ascontiguousarray(pos[b:b + 1])
        m["poso"] = po
        m["memb"] = np.ascontiguousarray(mem[b])
        mb = np.zeros((128, 4, 4, 512), np.float32)
        for ktl in range(4):
            for kb in range(4):
                key = ktl * 512 + kb * 128 + np.arange(128)[:, None]
                qq = q * 512 + kk[None, :]
                mb[:, ktl, kb, :] = np.where(key > qq, 0.0, 1.0)
        m["maskb"] = mb.reshape(128, 16, 512).astype(bf)
        oh = np.zeros((128, 4), np.float32)
        oh[:, q] = 1.0
        m["ohq"] = oh
        maps.append(m)
    return maps


_NC_CACHE = {}


def kernel(**inputs):
    S = int(np.asarray(inputs["x"]).shape[1])
    B = int(np.asarray(inputs["x"]).shape[0])
    assert B == 2
    if S not in _NC_CACHE:
        _NC_CACHE[S] = build(S)
    nc = _NC_CACHE[S]
    maps = make_in_maps(inputs, S)
    res = run_bass_kernel_spmd(nc, maps, core_ids=list(range(8)))
    NT = S // 512
    NJ = NT // 4
    out = np.zeros((B, S, D), np.float32)
    for core in range(8):
        b, q = core // 4, core % 4
        o = np.asarray(res.results[core]["out"], np.float32)
        for j in range(NJ):
            t0 = (4 * j + q) * 512
            out[b, t0:t0 + 512] = o[j * 512:(j + 1) * 512]
    return out
```

```python
import contextlib
import numpy as np
import ml_dtypes
import concourse.bass as bass
import concourse.mybir as mybir
from concourse.bass_utils import run_bass_kernel_spmd

F32 = mybir.dt.float32
BF16 = mybir.dt.bfloat16
I32 = mybir.dt.int32
U32 = mybir.dt.uint32
AF = mybir.ActivationFunctionType
ALU = mybir.AluOpType

ENGS = ("pe", "act", "dve", "pool", "sp")


class Prog:
    dead = False
    stop_at = None
    WINDOW = 120

    def __init__(self, nc):
        self.nc = nc
        self.ops = []
        self.state = {}
        self.seg = 0
        self.last_bar = {}
        self.n_bar = 0

    def checkpoint(self, name):
        if self.stop_at is not None and name == self.stop_at:
            self.barrier()
            self.dead = True

    @staticmethod
    def _norm(acc):
        return [a if isinstance(a, tuple) else (a, None) for a in acc]

    def _deps_for(self, reads, writes):
        deps = set()
        for (name, sub) in reads:
            st = self.state.get(name)
            if not st:
                continue
            subs = list(st.keys()) if sub is None else [s for s in st.keys() if s is None or s == sub]
            for s in subs:
                w = st[s][0]
                if w is not None:
                    deps.add(w)
        for (name, sub) in writes:
            st = self.state.get(name)
            if not st:
                continue
            subs = list(st.keys()) if sub is None else [s for s in st.keys() if s is None or s == sub]
            for s in subs:
                w, rs = st[s]
                if w is not None:
                    deps.add(w)
                deps.update(rs)
        return deps

    def _update(self, idx, reads, writes):
        for (name, sub) in writes:
            st = self.state.setdefault(name, {})
            if sub is None:
                st.clear()
                st[None] = [idx, []]
            else:
                st[sub] = [idx, []]
        for (name, sub) in reads:
            st = self.state.setdefault(name, {})
            if sub is None:
                for s in st.values():
                    s[1].append(idx)
                if not st:
                    st[None] = [None, [idx]]
            else:
                if sub in st:
                    st[sub][1].append(idx)
                elif None in st:
                    st[None][1].append(idx)
                else:
                    st[sub] = [None, [idx]]

    def op(self, eng, fn, reads=(), writes=(), cost=0.2):
        if self.dead:
            return -1
        reads = self._norm(reads)
        writes = self._norm(writes)
        if eng != "pe":
            pr = [a for a in reads if a[0].startswith("PP")]
            if pr:
                writes = writes + pr
        idx = len(self.ops)
        deps = self._deps_for(reads, writes)
        if eng in self.last_bar:
            deps.add(self.last_bar[eng])
        self.ops.append(dict(eng=eng, fn=fn, deps=deps, dma=None, idx=idx, seg=self.seg, cost=cost, lat=cost))
        self._update(idx, reads, writes)
        return idx

    def dma(self, queue, fn, reads=(), writes=(), key=None, nbytes=65536):
        if self.dead:
            return -1
        reads = self._norm(reads)
        writes = self._norm(writes)
        idx = len(self.ops)
        deps = self._deps_for(reads, writes)
        if queue in self.last_bar:
            deps.add(self.last_bar[queue])
        assert key is not None
        self.ops.append(dict(eng=queue, fn=fn, deps=deps, dma=key, idx=idx, seg=self.seg, cost=0.15,
                             lat=2.5 + nbytes / 60000.0))
        self._update(idx, reads, writes)
        return idx

    def barrier(self):
        if self.dead:
            return
        self.n_bar += 1
        for e in ENGS:
            idx = len(self.ops)
            self.ops.append(dict(eng=e, fn=None, deps=set(), dma=None, idx=idx, bar=self.n_bar, seg=self.seg, cost=0.0, lat=0.0))
            self.last_bar[e] = idx
        self.seg += 1
        self.state = {}

    def schedule(self):
        ops = self.ops
        n = len(ops)
        import os as _os
        WD = int(_os.environ.get("K_WDEF", str(self.WINDOW)))
        segw = {}
        for kv in _os.environ.get("K_WIN", "").split(","):
            if ":" in kv:
                a_, b_ = kv.split(":")
                segw[int(a_)] = int(b_)
        per_eng = {e: [] for e in ENGS}
        for o in ops:
            per_eng[o["eng"]].append(o["idx"])
        pos = {e: 0 for e in ENGS}
        free = {e: 0.0 for e in ENGS}
        sched = [False] * n
        finish = [0.0] * n
        dep_left = [0] * n
        users = [[] for _ in range(n)]
        ready_t = [0.0] * n
        for o in ops:
            i = o["idx"]
            dep_left[i] = len(o["deps"])
            for d in o["deps"]:
                users[d].append(i)
        nseg = self.seg + 1
        seg_left = [0] * (nseg + 1)
        seg_fin = [0.0] * (nseg + 1)
        for o in ops:
            if not o.get("bar"):
                seg_left[o["seg"]] += 1
        order = []
        remaining = n
        while remaining > 0:
            best = None
            for e in ENGS:
                lst = per_eng[e]
                p = pos[e]
                while p < len(lst) and sched[lst[p]]:
                    p += 1
                pos[e] = p
                cnt = 0
                q = p
                fe = free[e]
                W = segw.get(ops[lst[p]]["seg"], WD) if p < len(lst) else WD
                while q < len(lst) and cnt < W:
                    i = lst[q]
                    q += 1
                    if sched[i]:
                        continue
                    cnt += 1
                    o = ops[i]
                    if o.get("bar"):
                        if seg_left[o["seg"]] == 0:
                            st = max(fe, seg_fin[o["seg"]])
                            if best is None or (st, i) < (best[0], best[1]):
                                best = (st, i, e)
                        break
                    if dep_left[i] != 0:
                        continue
                    st = max(fe, ready_t[i])
                    if best is None or (st, i) < (best[0], best[1]):
                        best = (st, i, e)
                    if st <= fe:
                        break
            assert best is not None, "scheduler deadlock"
            st, i, e = best
            o = ops[i]
            sched[i] = True
            remaining -= 1
            free[e] = st + o["cost"]
            finish[i] = st + o["lat"]
            o["t"] = st
            order.append(i)
            if not o.get("bar"):
                sg = o["seg"]
                seg_left[sg] -= 1
                if finish[i] > seg_fin[sg]:
                    seg_fin[sg] = finish[i]
            for u in users[i]:
                dep_left[u] -= 1
                lat = 0.12 if ops[u]["eng"] != e else 0.05
                if ops[u]["eng"] == "pe" and e == "pe" and o["dma"] is None:
                    lat = 0.0
                t = finish[i] + lat
                if t > ready_t[u]:
                    ready_t[u] = t
        self.sim_time = max(finish) if finish else 0.0
        return order

    def emit(self, reorder=True):
        nc = self.nc
        ops = self.ops
        order = self.schedule() if reorder else list(range(len(ops)))
        needs_sig = [False] * len(ops)
        last_real = {e: None for e in ENGS}
        bar_snap = {}
        for i in order:
            o = ops[i]
            if o.get("bar"):
                if o["bar"] not in bar_snap:
                    bar_snap[o["bar"]] = dict(last_real)
                    for e2, li in last_real.items():
                        if li is not None:
                            needs_sig[li] = True
                continue
            for d in o["deps"]:
                needs_sig[d] = True
            if o["dma"] is None:
                last_real[o["eng"]] = i
        eng_sig = {e: 0 for e in ENGS}
        phys_count = []
        key_phys = {}
        key_sealed = {}
        free_phys = []
        waited = {e: {} for e in ENGS}
        sig_of = [None] * len(ops)
        plan = {}
        bar_dma_snap = {}
        for i in order:
            o = ops[i]
            e = o["eng"]
            waits = []
            w = waited[e]
            if o.get("bar"):
                bid = o["bar"]
                if bid not in bar_dma_snap:
                    bar_dma_snap[bid] = list(phys_count)
                    for k, p in key_phys.items():
                        free_phys.append(p)
                    key_phys = {}
                    key_sealed = {}
                for e2, li in bar_snap[bid].items():
                    if li is None:
                        continue
                    s_ = sig_of[li]
                    if s_ is None:
                        continue
                    kind, name, val = s_
                    if w.get((kind, name), 0) < val:
                        w[(kind, name)] = val
                        waits.append((kind, name, val))
                for p, cnt in enumerate(bar_dma_snap[bid]):
                    if cnt > 0 and w.get(("dma", p), 0) < cnt:
                        w[("dma", p)] = cnt
                        waits.append(("dma", p, cnt))
                plan[i] = (waits, None)
                continue
            for d in sorted(o["deps"]):
                od = ops[d]
                if od["dma"] is None and od["eng"] == "pe" and e == "pe" and o["dma"] is None:
                    continue
                s_ = sig_of[d]
                if s_ is None:
                    continue
                kind, name, val = s_
                if kind == "dma":
                    k = od["dma"]
                    if key_phys.get(k) == name:
                        val = max(val, phys_count[name])
                        key_sealed[k] = True
                if w.get((kind, name), 0) >= val:
                    continue
                w[(kind, name)] = val
                waits.append((kind, name, val))
            sig = None
            if o["dma"] is not None:
                k = o["dma"]
                if k not in key_phys:
                    if free_phys:
                        p = free_phys.pop(0)
                    else:
                        p = len(phys_count)
                        phys_count.append(0)
                    key_phys[k] = p
                    key_sealed[k] = False
                p = key_phys[k]
                if key_sealed[k] and phys_count[p] > 0:
                    if w.get(("dma", p), 0) < phys_count[p]:
                        w[("dma", p)] = phys_count[p]
                        waits.append(("dma", p, phys_count[p]))
                    key_sealed[k] = False
                phys_count[p] += 16
                sig = ("dma", p, phys_count[p])
            elif needs_sig[i]:
                eng_sig[e] += 1
                sig = ("eng", e, eng_sig[e])
            sig_of[i] = sig
            plan[i] = (waits, sig)
        self.max_sig = dict(eng_sig)
        self.n_dma_sems = len(phys_count)
        with contextlib.ExitStack() as es:
            sems = {}
            for e in ENGS:
                sems[("eng", e)] = es.enter_context(nc.semaphore("s_" + e))
            for p in range(len(phys_count)):
                sems[("dma", p)] = es.enter_context(nc.semaphore("d_%d" % p))
            block = es.enter_context(nc.Block())
            per_eng = {e: [] for e in ENGS}
            for i in order:
                per_eng[ops[i]["eng"]].append(i)
            final_waits = [(("dma", p), phys_count[p]) for p in range(len(phys_count))]
            final_eng = [(("eng", e), eng_sig[e]) for e in ENGS if eng_sig[e] > 0]

            def make(e):
                def body(engobj):
                    for i in per_eng[e]:
                        o = ops[i]
                        waits, sig = plan[i]
                        for (kind, name, val) in waits:
                            engobj.wait_ge(sems[(kind, name)], val)
                        if o["fn"] is None:
                            continue
                        ins = o["fn"](engobj)
                        if sig is not None:
                            kind, name, val = sig
                            ins.then_inc(sems[(kind, name)], 16 if kind == "dma" else 1)
                    if e == "sp":
                        for (sk, v) in final_waits:
                            engobj.wait_ge(sems[sk], v)
                        for (sk, v) in final_eng:
                            engobj.wait_ge(sems[sk], v)
                return body

            block.tensor(make("pe"))
            block.scalar(make("act"))
            block.vector(make("dve"))
            block.gpsimd(make("pool"))
            block.sync(make("sp"))


D = 1024
DI = 2048
NH = 32
NMEM = 256
RMS_EPS = 1e-6
Z0, X0, BC0, C0, DT0, QA0, CKV0, KR0, G10, G20 = 0, 2048, 4096, 4608, 5120, 5152, 5536, 5792, 5824, 6848
NIN = 7872
TWO_PI = 6.283185307179586
CW1 = 6.28125
CW2 = TWO_PI - 6.28125
NEG = -30000.0

WEIGHTS = [
    ("w_in", (1024, 7872)), ("w_ssd_out", (2048, 1024)), ("w_q_b", (384, 1536)), ("w_kv_b", (256, 2048)),
    ("w_mla_out", (1024, 1024)), ("w_o", (1024, 1024)), ("w_xq", (1024, 1024)), ("w_xkv", (1024, 2048)),
    ("w_xo", (1024, 1024)), ("w_router_group", (1024, 4)), ("w_router_expert", (1024, 32)),
    ("w_exp_gate", (32 * 1024, 256)), ("w_exp_up", (32 * 1024, 256)), ("w_exp_down", (32 * 256, 1024)),
]
VECS = [
    ("norm_mix", 1024), ("conv_b", 3072), ("dt_bias", 32), ("a_log", 32), ("d_skip", 32), ("ssd_norm", 2048),
    ("q_a_norm", 384), ("kv_a_norm", 256), ("norm_xattn", 1024), ("norm_mem", 1024), ("norm_moe", 1024),
    ("b_router_group", 4), ("b_router_expert", 32), ("norm_final", 1024),
]


class K:
    pass


def build(S, debug=False, phases=(1, 2), stop_at=None):
    NT = S // 512
    NJ = NT // 4
    nc = bass.Bass("TRN2", target_bir_lowering=False)
    P = Prog(nc)
    P.stop_at = stop_at

    def din(name, shape, dt=F32):
        return nc.dram_tensor(name, list(shape), dt, kind="ExternalInput").ap()

    def dscr(name, shape, dt):
        return nc.dram_tensor(name, list(shape), dt, kind=("ExternalOutput" if debug else "Internal")).ap()

    xb = din("xb", (S, D))
    xo = din("xo", (NJ, 515, D))
    posb = din("posb", (1, S), I32)
    poso = din("poso", (1, NJ * 512), I32)
    memb = din("memb", (NMEM, D))
    maskb_d = din("maskb", (128, 16, 512), BF16)
    ohq_d = din("ohq", (128, 4))
    c_identb = din("c_identb", (128, 128), BF16)
    c_identf = din("c_identf", (128, 128))
    c_tri = din("c_tri", (128, 128))
    c_onesf = din("c_onesf", (128, 128))
    c_maskneg = din("c_maskneg", (128, 128), BF16)
    c_selh = din("c_selh", (32, 32 * 128))
    c_selhb = din("c_selhb", (32, 32 * 128), BF16)
    c_invf = din("c_invf", (32, 2))
    wd = {n: din(n, shp) for n, shp in WEIGHTS}
    vd = {n: din(n, (1, ln)) for n, ln in VECS}
    conv_w_d = din("conv_w", (4, 3072))
    out_d = nc.dram_tensor("out", [NJ * 512, D], F32, kind="ExternalOutput").ap()

    wb = {n: nc.dram_tensor(n + "_b", list(shp), BF16, kind="Internal").ap() for n, shp in WEIGHTS}
    KT = dscr("KT", (16, 96, S), BF16)
    VS = dscr("VS", (4, S, 260), BF16)
    OSD = dscr("OSD", (NJ * 4, 128, 2048), BF16)

    es = contextlib.ExitStack()

    sb_cnt = [0]

    def sb(name, shape, dt, st=None):
        sb_cnt[0] += 1
        return (st or es).enter_context(nc.sbuf_tensor("s%d_%s" % (sb_cnt[0], name), list(shape), dt))

    def fsz(ap):
        n = 1
        for d_ in ap.shape[1:]:
            n *= int(d_)
        return n

    def nbytes_of(ap):
        n = 1
        for d_ in ap.shape:
            n *= int(d_)
        return n * (4 if ap.dtype in (F32, I32, U32) else 2)

    def DMA(q, out, in_, r, w, key, **kw):
        P.dma(q, lambda e: e.dma_start(out=out, in_=in_, **kw), reads=r, writes=w, key=key, nbytes=nbytes_of(out))

    def MM(out, lhsT, rhs, start, stop, r, w):
        n_ = fsz(rhs)
        c_ = max(n_, 64) / 2400.0 * (4.0 if lhsT.dtype == F32 else 1.0) + 0.03
        P.op("pe", lambda e: e.matmul(out, lhsT=lhsT, rhs=rhs, start=start, stop=stop), reads=r, writes=w, cost=c_)

    def TR(out, in_, ident, r, w):
        P.op("pe", lambda e: e.transpose(out=out, in_=in_, identity=ident), reads=r, writes=w, cost=0.09)

    def ACT(out, in_, func, r, w, bias=None, scale=None, accum=None):
        kw = {}
        if bias is not None:
            kw["bias"] = bias
        if scale is not None:
            kw["scale"] = scale
        if accum is not None:
            kw["accum_out"] = accum
        c_ = fsz(in_) / 1100.0 + 0.22 + (0.1 if accum is not None else 0.0)
        P.op("act", lambda e: e.activation(out=out, in_=in_, func=func, **kw), reads=r, writes=w, cost=c_)

    def vcost(eng, ap):
        return fsz(ap) / (900.0 if eng == "dve" else 500.0) + (0.12 if eng == "dve" else 0.25)

    def TT(eng, out, in0, in1, op, r, w):
        P.op(eng, lambda e: e.tensor_tensor(out=out, in0=in0, in1=in1, op=op), reads=r, writes=w, cost=vcost(eng, out))

    def TS(eng, out, in0, s1, s2, op0, op1, r, w):
        if op1 is None:
            P.op(eng, lambda e: e.tensor_scalar(out=out, in0=in0, scalar1=s1, scalar2=None, op0=op0), reads=r, writes=w, cost=vcost(eng, out))
        else:
            P.op(eng, lambda e: e.tensor_scalar(out=out, in0=in0, scalar1=s1, scalar2=s2, op0=op0, op1=op1), reads=r, writes=w, cost=vcost(eng, out))

    def STT(eng, out, in0, scalar, in1, op0, op1, r, w):
        P.op(eng, lambda e: e.scalar_tensor_tensor(out=out, in0=in0, scalar=scalar, in1=in1, op0=op0, op1=op1), reads=r, writes=w, cost=vcost(eng, out))

    def CP(eng, out, in_, r, w):
        P.op(eng, lambda e: e.tensor_copy(out=out, in_=in_), reads=r, writes=w, cost=vcost(eng, out))

    def MEMSET(eng, ap, val, w):
        P.op(eng, lambda e: e.memset(ap, val), writes=w, cost=vcost(eng, ap))

    def RECIP(out, in_, r, w):
        P.op("dve", lambda e: e.reciprocal(out=out, in_=in_), reads=r, writes=w, cost=vcost("dve", out))

    PP = [es.enter_context(nc.psum_tensor("PP%d" % i, [128, 1024], F32)) for i in range(4)]

    def bank(i):
        return PP[i // 2][:, (i % 2) * 512:(i % 2 + 1) * 512]

    def bankb(i):
        return PP[i // 2][:].bitcast(BF16)[:, (i % 2) * 1024:(i % 2 + 1) * 1024]

    def bn(i):
        return ("PP%d" % (i // 2), i % 2)

    identb = sb("identb", (128, 128), BF16)
    identf = sb("identf", (128, 128), F32)
    tri = sb("tri", (128, 128), F32)
    onesf = sb("onesf", (128, 128), F32)
    onesb = sb("onesb", (128, 128), BF16)
    maskneg = sb("maskneg", (128, 128), BF16)
    invf = sb("invf", (32, 2), F32)
    ohq = sb("ohq", (128, 4), F32)
    for t, d_, nm in ((identb, c_identb, "identb"), (identf, c_identf, "identf"), (tri, c_tri, "tri"),
                      (onesf, c_onesf, "onesf"), (maskneg, c_maskneg, "maskneg"),
                      (invf, c_invf, "invf"), (ohq, ohq_d, "ohq")):
        DMA("sp", t[:], d_[:, :], [], [nm], "c_" + nm)
    CP("dve", onesb[:], onesf[:], ["onesf"], ["onesb"])

    def pvec(name, n):
        t = sb("pv_" + name, (128, n // 128), F32)
        DMA("sp", t[:], vd[name].rearrange("o (k p) -> p (o k)", p=128), [], ["pv_" + name], "pv_" + name,
            allow_slow_non_contiguous=True)
        return t

    g_mix = pvec("norm_mix", 1024)
    g_ssd = pvec("ssd_norm", 2048)
    g_qa = pvec("q_a_norm", 384)
    g_kv = pvec("kv_a_norm", 256)
    g_xa = pvec("norm_xattn", 1024)
    g_mem = pvec("norm_mem", 1024)
    g_moe = pvec("norm_moe", 1024)
    cb_p = pvec("conv_b", 3072)
    cw_p = sb("cw_p", (128, 24, 4), F32)
    for k in range(4):
        DMA("sp", cw_p[:, :, k], conv_w_d[k:k + 1, :].rearrange("o (k p) -> p (o k)", p=128), [], [("cw_p", k)],
            "cw_p%d" % k, allow_slow_non_contiguous=True)

    def bvec(name, n):
        t = sb("bv_" + name, (128, n), F32)
        DMA("sp", t[:], vd[name].partition_broadcast(128), [], ["bv_" + name], "bv_" + name)
        return t

    dtb_bc = bvec("dt_bias", 32)
    alog_bc = bvec("a_log", 32)
    dsk_bc = bvec("d_skip", 32)
    a_bc = sb("a_bc", (128, 32), F32)
    ACT(a_bc[:], alog_bc[:], AF.Exp, ["bv_a_log"], ["a_bc"])
    TS("dve", a_bc[:], a_bc[:], -1.0, None, ALU.mult, None, ["a_bc"], ["a_bc"])

    def cast_weight(name, rows_first=None):
        src, dst = wd[name], wb[name]
        R, C = src.shape
        i = 0
        for r0 in range(0, R, 512):
            r1 = min(R, r0 + 512)
            for c0 in range(0, C, 2048):
                c1 = min(C, c0 + 2048)
                DMA("pool", dst[r0:r1, c0:c1], src[r0:r1, c0:c1], [], [("wb_" + name, i)], "cast_" + name)
                i += 1

    for n, _ in WEIGHTS:
        cast_weight(n)

    def rope_tables(pos_ap, cs1, cs2, tmp_i, tmp_f, tmp_a, nm):
        DMA("sp", tmp_i[:], pos_ap.partition_broadcast(32), [], [nm + "i"], nm + "pos")
        CP("dve", tmp_f[:], tmp_i[:], [nm + "i"], [nm + "f"])
        TS("dve", tmp_f[:], tmp_f[:], invf[:, 0:1], None, ALU.mult, None, [nm + "f", "invf"], [nm + "f"])
        for which, dst in ((0, cs2), (1, cs1)):
            src = tmp_f
            if which == 1:
                TS("dve", tmp_a[:], tmp_f[:], float(np.pi / 2), None, ALU.add, None, [nm + "f"], [nm + "a"])
                src = tmp_a
            srcn = nm + ("a" if which == 1 else "f")
            TS("dve", tmp_i[:], src[:], float(1.0 / TWO_PI), None, ALU.mult, None, [srcn], [nm + "i"])
            CP("dve", dst[:], tmp_i[:], [nm + "i"], [nm + "c%d" % which])
            STT("dve", tmp_a[:], dst[:], -CW1, src[:], ALU.mult, ALU.add, [nm + "c%d" % which, srcn], [nm + "a"])
            STT("dve", tmp_a[:], dst[:], -CW2, tmp_a[:], ALU.mult, ALU.add, [nm + "c%d" % which, nm + "a"], [nm + "a"])
            TS("dve", tmp_a[:], tmp_a[:], float(np.pi), float(-np.pi), ALU.min, ALU.max, [nm + "a"], [nm + "a"])
            ACT(dst[:], tmp_a[:], AF.Sin, [nm + "a"], [nm + "c%d" % which])
        TS("dve", cs2[:], cs2[:], invf[:, 1:2], None, ALU.mult, None, [nm + "c0", "invf"], [nm + "c0"])

    def norm_transpose(src_f32, ncols, gain_p, dstT, dst_cols, names_r, name_dst, scr, pbank, tag, gname):
        junk, ssq, rstd, xs = scr
        nk = ncols // 128
        ACT(junk[:, 0:ncols], src_f32, AF.Square, names_r, [tag + "junk", tag + "ssq"], accum=ssq[:])
        ACT(rstd[:], ssq[:], AF.Sqrt, [tag + "ssq", "eps_t"], [tag + "rstd"], bias=eps_t[:], scale=1.0 / ncols)
        RECIP(rstd[:], rstd[:], [tag + "rstd"], [tag + "rstd"])
        TS("dve", xs[:, 0:ncols], src_f32, rstd[:, 0:1], None, ALU.mult, None, names_r + [tag + "rstd"], [tag + "xs"])
        pv = bankb(pbank)
        for k in range(nk):
            TR(pv[:, k * 128:(k + 1) * 128], xs[:, k * 128:(k + 1) * 128], identb[:], [tag + "xs", "identb"], [bn(pbank)])
        TT("dve", dstT[:, :, dst_cols], pv[:, 0:ncols].rearrange("p (k t) -> p k t", k=nk),
           gain_p[:, 0:nk].unsqueeze(2).to_broadcast([128, nk, 128]), ALU.mult,
           [bn(pbank), gname], [name_dst])

    eps_t = sb("eps_t", (128, 1), F32)
    MEMSET("dve", eps_t[:], RMS_EPS, ["eps_t"])

    if 1 in phases:
        with contextlib.ExitStack() as s1:
            WxB = sb("WxB", (128, 8, 2560), BF16, s1)
            Wdt = sb("Wdt", (128, 8, 32), BF16, s1)
            Wckv = sb("Wckv", (128, 8, 288), BF16, s1)
            Wkr = sb("Wkr", (128, 8, 64), BF16, s1)
            wkn = sb("wkn", (128, 2, 1024), BF16, s1)
            wv = sb("wv", (128, 2, 1024), BF16, s1)
            win_v = wb["w_in"].rearrange("(k p) c -> p k c", p=128)
            DMA("sp", WxB[:], win_v[:, :, X0:X0 + 2560], ["wb_w_in"], ["WxB"], "WxB")
            DMA("sp", Wdt[:], win_v[:, :, DT0:DT0 + 32], ["wb_w_in"], ["Wdt"], "Wdt")
            DMA("sp", Wckv[:], win_v[:, :, CKV0:CKV0 + 288], ["wb_w_in"], ["Wckv"], "Wckv")
            DMA("sp", Wkr[:, :, 0:32], win_v[:, :, KR0:KR0 + 32], ["wb_w_in"], [("Wkr", 0)], "Wkr")
            DMA("sp", Wkr[:, :, 32:48], win_v[:, :, KR0 + 16:KR0 + 32], ["wb_w_in"], [("Wkr", 1)], "Wkr")
            DMA("sp", Wkr[:, :, 48:64], win_v[:, :, KR0:KR0 + 16], ["wb_w_in"], [("Wkr", 2)], "Wkr")
            wkv_v = wb["w_kv_b"].rearrange("(k p) (h e) -> p k h e", p=128, e=128)
            for kb in range(2):
                DMA("sp", wkn[:, kb, :].rearrange("p (h e) -> p h e", e=64), wkv_v[:, kb, :, 0:64], ["wb_w_kv_b"], [("wkn", kb)], "wkn")
                DMA("sp", wv[:, kb, :].rearrange("p (h e) -> p h e", e=64), wkv_v[:, kb, :, 64:128], ["wb_w_kv_b"], [("wv", kb)], "wv")

            xin = [sb("xin%d" % i, (128, 1024), F32, s1) for i in range(3)]
            junk = sb("junk1", (128, 1024), BF16, s1)
            ssq = sb("ssq1", (128, 1), F32, s1)
            rstd = sb("rstd1", (128, 1), F32, s1)
            xs = sb("xs1", (128, 1024), BF16, s1)
            uT = sb("uT1", (128, 8, 512), BF16, s1)
            rb = [sb("rb%d" % i, (128, 515), BF16, s1) for i in range(2)]
            hal = sb("hal", (128, 20, 3), BF16, s1)
            cdiag = sb("cdiag", (128, 20, 4, 128), BF16, s1)
            for blk in range(20):
                for k in range(4):
                    TS("dve" if (blk + k) % 2 == 0 else "pool", cdiag[:, blk, k, :], identf[:], cw_p[:, blk, k:k + 1], None,
                       ALU.mult, None, ["identf", "cw_p"], [("cdiag", blk * 4 + k)])
            xsT = sb("xsT1", (128, 20, 512), BF16, s1)
            dtr = sb("dtr1", (128, 32), F32, s1)
            dt_ = sb("dt1", (128, 32), F32, s1)
            adt = sb("adt1", (128, 32), F32, s1)
            acum = sb("acum1", (128, 32), F32, s1)
            tmp32 = sb("tmp32", (128, 32), F32, s1)
            dte = sb("dte1", (128, 32), F32, s1)
            cd = sb("cd1", (128, 32), F32, s1)
            wdt = sb("wdt1", (128, 32), F32, s1)
            xw = sb("xw1", (128, 2048), BF16, s1)
            Btm = sb("Btm1", (128, 512), BF16, s1)
            Sst = sb("Sst", (128, 2048), F32, s1)
            Sb = sb("Sb", (128, 2048), BF16, s1)
            OS = [sb("OS%d" % i, (128, 2048), BF16, s1) for i in range(4)]
            tmpb = sb("tmpb", (128, 512), F32, s1)
            junk2 = sb("junk2", (128, 256), F32, s1)
            ssq2 = sb("ssq2", (128, 1), F32, s1)
            rstd2 = sb("rstd2", (128, 1), F32, s1)
            cn = sb("cn1", (128, 256), BF16, s1)
            cnT = sb("cnT1", (128, 2, 512), BF16, s1)
            cs1 = sb("cs1_1", (32, 512), F32, s1)
            cs2 = sb("cs2_1", (32, 512), F32, s1)
            rt_i = sb("rt_i1", (32, 512), I32, s1)
            rt_f = sb("rt_f1", (32, 512), F32, s1)
            rt_a = sb("rt_a1", (32, 512), F32, s1)
            kpe = sb("kpe1", (32, 512), BF16, s1)
            kst = [sb("kst%d" % i, (128, 512), BF16, s1) for i in range(2)]
            vst = [sb("vst%d" % i, (128, 16, 65), BF16, s1) for i in range(2)]

            MEMSET("dve", hal[:], 0.0, ["hal"])
            MEMSET("dve", Sst[:], 0.0, ["Sst"])
            MEMSET("dve", Sb[:], 0.0, ["Sb"])
            for i in range(4):
                MEMSET("pool", OS[i][:], 0.0, ["OS%d" % i])
            for i in range(2):
                MEMSET("pool", vst[i][:], 1.0, ["vst%d" % i])

            for g in range(NT):
                rope_tables(posb[0:1, g * 512:(g + 1) * 512], cs1, cs2, rt_i, rt_f, rt_a, "rt1")
                for c in range(4):
                    xi = (g * 4 + c) % 3
                    xt = xin[xi]
                    xtn = "xin%d" % xi
                    DMA("sp", xt[:], xb[g * 512 + c * 128:g * 512 + (c + 1) * 128, :], [], [xtn], xtn)
                    norm_transpose(xt[:], 1024, g_mix, uT, slice(c * 128, (c + 1) * 128), [xtn], ("uT1", c),
                                   (junk, ssq, rstd, xs), 0, "n1", "pv_norm_mix")
                for blk in range(20):
                    pb_ = 2 + (blk % 2)
                    for k in range(8):
                        MM(bank(pb_), WxB[:, k, blk * 128:(blk + 1) * 128], uT[:, k, :], k == 0, k == 7,
                           ["WxB", "uT1"], [bn(pb_)])
                    r_ = rb[blk % 2]
                    rn = "rb%d" % (blk % 2)
                    CP("dve", r_[:, 0:3], hal[:, blk, :], [("hal", blk)], [(rn, 0)])
                    ACT(r_[:, 3:515], bank(pb_), AF.Copy, [bn(pb_)], [(rn, 1)])
                    CP("dve", hal[:, blk, :], r_[:, 512:515], [(rn, 1)], [("hal", blk)])
                    for k in range(4):
                        MM(bank(4), cdiag[:, blk, k, :], r_[:, k:k + 512], k == 0, k == 3, [rn, ("cdiag", blk * 4 + k)], [bn(4)])
                    ACT(xsT[:, blk, :], bank(4), AF.Silu, [bn(4), "pv_conv_b"], [("xsT1", blk)], bias=cb_p[:, blk:blk + 1])
                for v_ in range(2):
                    for k in range(8):
                        MM(bank(2 + v_)[0:32, :], Wkr[:, k, v_ * 32:(v_ + 1) * 32], uT[:, k, :], k == 0, k == 7,
                           ["Wkr", "uT1"], [bn(2 + v_)])
                TT("dve", rt_f[:], bank(2)[0:32, :], cs1[:], ALU.mult, [bn(2), "rt1c1"], ["rt1f"])
                TT("dve", rt_a[:], bank(3)[0:32, :], cs2[:], ALU.mult, [bn(3), "rt1c0"], ["rt1a"])
                TT("dve", kpe[:], rt_f[:], rt_a[:], ALU.add, ["rt1f", "rt1a"], ["kpe1"])
                for h in range(16):
                    DMA("sp", KT[h, 0:32, g * 512:(g + 1) * 512], kpe[:], ["kpe1"], [("KT", g)], "KTpe")
                for c in range(4):
                    cc = slice(c * 128, (c + 1) * 128)
                    b7 = bank(7)
                    for k in range(8):
                        MM(b7[:, 0:32], uT[:, k, cc], Wdt[:, k, :], k == 0, k == 7, ["uT1", "Wdt"], [bn(7)])
                    TT("dve", dtr[:], b7[:, 0:32], dtb_bc[:], ALU.add, [bn(7), "bv_dt_bias"], ["dtr1"])
                    ACT(dtr[:], dtr[:], AF.Exp, ["dtr1"], ["dtr1"])
                    ACT(dt_[:], dtr[:], AF.Ln, ["dtr1"], ["dt1"], bias=1.0)
                    TT("dve", adt[:], dt_[:], a_bc[:], ALU.mult, ["dt1", "a_bc"], ["adt1"])
                    MM(b7[:, 32:64], tri[:], adt[:], True, True, ["tri", "adt1"], [bn(7)])
                    MM(b7[:, 64:96], onesf[:], adt[:], True, True, ["onesf", "adt1"], [bn(7)])
                    ACT(acum[:], b7[:, 32:64], AF.Copy, [bn(7)], ["acum1"])
                    TT("dve", tmp32[:], b7[:, 64:96], acum[:], ALU.subtract, [bn(7), "acum1"], ["tmp32"])
                    ACT(dte[:], tmp32[:], AF.Exp, ["tmp32"], ["dte1"])
                    ACT(cd[:], b7[:, 64:96], AF.Exp, [bn(7)], ["cd1"])
                    TT("dve", wdt[:], dt_[:], dte[:], ALU.mult, ["dt1", "dte1"], ["wdt1"])
                    pxv = PP[2][:].bitcast(BF16)
                    for blk in range(16):
                        TR(pxv[:, blk * 128:(blk + 1) * 128], xsT[:, blk, cc], identb[:], [("xsT1", blk), "identb"],
                           [("PP2", blk // 8)])
                    TT("dve", xw[:].rearrange("p (h e) -> p h e", e=64), pxv.rearrange("p (h e) -> p h e", e=64),
                       wdt[:, :].unsqueeze(2).to_broadcast([128, 32, 64]), ALU.mult, ["PP2", "wdt1"], ["xw1"])
                    pbv = bankb(0)
                    for blk in range(4):
                        TR(pbv[:, blk * 128:(blk + 1) * 128], xsT[:, 16 + blk, cc], identb[:], [("xsT1", 16 + blk), "identb"],
                           [bn(0)])
                    ACT(Btm[:], pbv[:, 0:512], AF.Copy, [bn(0)], ["Btm1"])
                    ci = c
                    if g % 4 == 0:
                        MEMSET("pool", OS[ci][:], 0.0, ["OS%d" % ci])
                    STT("dve", OS[ci][:], Sb[:], ohq[:, (g % 4):(g % 4) + 1], OS[ci][:], ALU.mult, ALU.add,
                        ["Sb", "ohq", "OS%d" % ci], ["OS%d" % ci])
                    for g4 in range(4):
                        gs = slice(g4 * 512, (g4 + 1) * 512)
                        MM(bank(6), Btm[:, g4 * 128:(g4 + 1) * 128], xw[:, gs], True, True, ["Btm1", "xw1"], [bn(6)])
                        TT("pool", Sst[:, gs].rearrange("p (h e) -> p h e", e=64), Sst[:, gs].rearrange("p (h e) -> p h e", e=64),
                           cd[:, g4 * 8:(g4 + 1) * 8].unsqueeze(2).to_broadcast([128, 8, 64]), ALU.mult,
                           [("Sst", g4), "cd1"], [("Sst", g4)])
                        TT("dve", Sst[:, gs], Sst[:, gs], bank(6), ALU.add, [("Sst", g4), bn(6)], [("Sst", g4)])
                        ACT(Sb[:, gs], Sst[:, gs], AF.Copy, [("Sst", g4)], [("Sb", g4)])
                    for k in range(8):
                        MM(b7[:, 96:384], uT[:, k, cc], Wckv[:, k, :], k == 0, k == 7, ["uT1", "Wckv"], [bn(7)])
                    ACT(junk2[:], b7[:, 96:352], AF.Square, [bn(7)], ["junk2", "ssq2"], accum=ssq2[:])
                    ACT(rstd2[:], ssq2[:], AF.Sqrt, ["ssq2"], ["rstd2"], bias=eps_t[:], scale=1.0 / 256)
                    RECIP(rstd2[:], rstd2[:], ["rstd2"], ["rstd2"])
                    TS("dve", cn[:], b7[:, 96:352], rstd2[:, 0:1], None, ALU.mult, None, [bn(7), "rstd2"], ["cn1"])
                    pcv = bankb(1)
                    for k in range(2):
                        TR(pcv[:, k * 128:(k + 1) * 128], cn[:, k * 128:(k + 1) * 128], identb[:], ["cn1", "identb"],
                           [bn(1)])
                    TT("dve", cnT[:, :, cc], pcv[:, 0:256].rearrange("p (k t) -> p k t", k=2),
                       g_kv[:, 0:2].unsqueeze(2).to_broadcast([128, 2, 128]), ALU.mult, [bn(1), "pv_kv_a_norm"], [("cnT1", c)])
                    for half in range(2):
                        for kb in range(2):
                            MM(bank(2 + half), cnT[:, kb, cc], wv[:, kb, half * 512:(half + 1) * 512], kb == 0, kb == 1,
                               [("cnT1", c), "wv"], [bn(2 + half)])
                    vs_ = vst[c % 2]
                    vn = "vst%d" % (c % 2)
                    CP("dve", vs_[:, :, 0:64], PP[1][:].rearrange("p (h e) -> p h e", e=64), ["PP1"], [vn])
                    t0 = g * 512 + c * 128
                    DMA("sp", VS[:, t0:t0 + 128, :].rearrange("g t e -> t g e"), vs_[:].rearrange("p (g h) e -> p g (h e)", g=4),
                        [vn], [("VS", g)], "VSw")
                for hp in range(8):
                    for kb in range(2):
                        MM(bank(6), wkn[:, kb, hp * 128:(hp + 1) * 128], cnT[:, kb, :], kb == 0, kb == 1, ["wkn", "cnT1"], [bn(6)])
                    ks_ = kst[hp % 2]
                    kn = "kst%d" % (hp % 2)
                    ACT(ks_[:], bank(6), AF.Copy, [bn(6)], [kn])
                    for hh in range(2):
                        DMA("sp", KT[hp * 2 + hh, 32:96, g * 512:(g + 1) * 512], ks_[hh * 64:(hh + 1) * 64, :], [kn], [("KT", g)], "KTn")
                if g % 4 == 3:
                    j = g // 4
                    for ci in range(4):
                        DMA("sp", OSD[j * 4 + ci, :, :], OS[ci][:], ["OS%d" % ci], [("OSD", j)], "OSDw")
        P.barrier()

    if 2 in phases:
        dbg = {}
        if debug:
            for nm in ("dbg_h1", "dbg_h2", "dbg_h3"):
                dbg[nm] = nc.dram_tensor(nm, [NJ * 512, D], F32, kind="ExternalOutput").ap()
            dbg["dbg_ynT"] = nc.dram_tensor("dbg_ynT", [NJ, 128, 16 * 512], BF16, kind="ExternalOutput").ap()
            dbg["dbg_oT"] = nc.dram_tensor("dbg_oT", [NJ, 64, 16 * 512], BF16, kind="ExternalOutput").ap()
            dbg["dbg_QT"] = nc.dram_tensor("dbg_QT", [NJ, 96, 16 * 512], BF16, kind="ExternalOutput").ap()
            dbg["dbg_comb"] = nc.dram_tensor("dbg_comb", [NJ * 512, 32], F32, kind="ExternalOutput").ap()
        KXD = nc.dram_tensor("KXD", [128, 8 * 256], BF16, kind="Internal").ap()
        VXD = nc.dram_tensor("VXD", [128, 2 * 1024], BF16, kind="Internal").ap()
        DSKD = nc.dram_tensor("DSKD", [128, 32 * 128], BF16, kind="Internal").ap()
        win_v = wb["w_in"].rearrange("(k p) c -> p k c", p=128)
        SCALE = float(96 ** -0.5)
        BIG = 10000.0
        with contextlib.ExitStack() as s2:
            with contextlib.ExitStack() as s0:
                mem_t = sb("mem_t", (128, 1024), F32, s0)
                junk = sb("junk0", (128, 1024), BF16, s0)
                ssq = sb("ssq0", (128, 1), F32, s0)
                rstd = sb("rstd0", (128, 1), F32, s0)
                xs = sb("xs0", (128, 1024), BF16, s0)
                memnT = sb("memnT", (128, 8, 256), BF16, s0)
                Wxkv = sb("Wxkv", (128, 8, 2048), BF16, s0)
                kx_st = sb("kx_st", (128, 8, 256), BF16, s0)
                vx_st = sb("vx_st", (128, 2, 1024), BF16, s0)
                dsk_st = sb("dsk_st", (128, 32, 128), BF16, s0)
                DMA("sp", Wxkv[:], wb["w_xkv"].rearrange("(k p) c -> p k c", p=128), ["wb_w_xkv"], ["Wxkv"], "Wxkv")
                for mb in range(2):
                    DMA("sp", mem_t[:], memb[mb * 128:(mb + 1) * 128, :], [], ["mem_t"], "mem_t")
                    norm_transpose(mem_t[:], 1024, g_mem, memnT, slice(mb * 128, (mb + 1) * 128), ["mem_t"], ("memnT", mb),
                                   (junk, ssq, rstd, xs), 0, "n0", "pv_norm_mem")
                for blk in range(8):
                    pb_ = 2 + blk % 2
                    for k in range(8):
                        MM(bank(pb_)[:, 0:256], Wxkv[:, k, blk * 128:(blk + 1) * 128], memnT[:, k, :], k == 0, k == 7, ["Wxkv", "memnT"], [bn(pb_)])
                    ACT(kx_st[:, blk, :], bank(pb_)[:, 0:256], AF.Copy, [bn(pb_)], [("kx_st", blk)])
                for mb in range(2):
                    for half in range(2):
                        pb_ = 4 + half
                        for k in range(8):
                            MM(bank(pb_), memnT[:, k, mb * 128:(mb + 1) * 128], Wxkv[:, k, 1024 + half * 512:1024 + (half + 1) * 512],
                               k == 0, k == 7, ["Wxkv", "memnT"], [bn(pb_)])
                        ACT(vx_st[:, mb, half * 512:(half + 1) * 512], bank(pb_), AF.Copy, [bn(pb_)], [("vx_st", mb * 2 + half)])
                for h in range(32):
                    TS("dve" if h % 2 == 0 else "pool", dsk_st[:, h, :], identf[:], dsk_bc[:, h:h + 1], None, ALU.mult, None,
                       ["identf", "bv_d_skip"], [("dsk_st", h)])
                DMA("sp", KXD[:, :], kx_st[:].rearrange("p a b -> p (a b)"), ["kx_st"], ["KXD"], "KXD")
                DMA("sp", VXD[:, :], vx_st[:].rearrange("p a b -> p (a b)"), ["vx_st"], ["VXD"], "VXD")
                DMA("sp", DSKD[:, :], dsk_st[:].rearrange("p a b -> p (a b)"), ["dsk_st"], ["DSKD"], "DSKD")
            P.barrier()
            P.checkpoint("S0")

            Wqs = sb("Wqs", (128, 3, 16, 32), BF16, s2)
            wq_v = wb["w_q_b"].rearrange("(k p) (h e) -> p k h e", p=128, e=96)
            for kb in range(3):
                DMA("sp", Wqs[:, kb, :, 0:16], wq_v[:, kb, :, 80:96], ["wb_w_q_b"], [("Wqs", kb)], "Wqs")
                DMA("sp", Wqs[:, kb, :, 16:32], wq_v[:, kb, :, 64:80], ["wb_w_q_b"], [("Wqs", kb)], "Wqs")
            rb_bc = sb("rb_bc", (128, 36), F32, s2)
            DMA("sp", rb_bc[:, 0:4], vd["b_router_group"].partition_broadcast(128), [], [("rb_bc", 0)], "rb_bc")
            DMA("sp", rb_bc[:, 4:36], vd["b_router_expert"].partition_broadcast(128), [], [("rb_bc", 1)], "rb_bc")
            Wr = sb("Wr", (128, 8, 36), BF16, s2)
            DMA("sp", Wr[:, :, 0:4], wb["w_router_group"].rearrange("(k p) c -> p k c", p=128), ["wb_w_router_group"], [("Wr", 0)], "Wr")
            DMA("sp", Wr[:, :, 4:36], wb["w_router_expert"].rearrange("(k p) c -> p k c", p=128), ["wb_w_router_expert"], [("Wr", 1)], "Wr")
            WPA = sb("WPA", (128, 16, 1024), BF16, s2)
            hT = sb("hT", (128, 4, 1024), F32, s2)
            uT = sb("uT2", (128, 8, 512), BF16, s2)
            t1T = sb("t1T", (128, 8, 512), BF16, s2)
            Wst = [None, None]
            wst_gen = [0]

            def alloc_wst(scope):
                wst_gen[0] += 1
                for i in range(2):
                    Wst[i] = sb("Wst%d_%d" % (i, wst_gen[0]), (128, 8, 512), BF16, scope)
            junk = sb("junk2p", (128, 1024), BF16, s2)
            ssq = sb("ssq2p", (128, 1), F32, s2)
            rstd = sb("rstd2p", (128, 1), F32, s2)
            xs = sb("xs2p", (128, 1024), BF16, s2)
            scr = (junk, ssq, rstd, xs)
            wst_i = [0]

            def load_wst(c0, ncols):
                i = wst_i[0] % 2
                wst_i[0] += 1
                DMA("sp", Wst[i][:, :, 0:ncols], win_v[:, :, c0:c0 + ncols], ["wb_w_in"], ["Wst%d" % i], "Wst%d" % i)
                return Wst[i], "Wst%d" % i

            P.checkpoint("P0")
            for j in range(NJ):
                KTN = 4 * j + 4
                DMA("sp", hT[:], xo[j, 3:515, :].rearrange("(c p) d -> p c d", p=128), [], ["hT"], "hT")
                DMA("sp", WPA[:], wb["w_ssd_out"].rearrange("(k p) c -> p k c", p=128), ["wb_w_ssd_out"], ["WPA"], "WPA")
                for c in range(4):
                    norm_transpose(hT[:, c, :], 1024, g_mix, uT, slice(c * 128, (c + 1) * 128), [("hT", c)], ("uT2", c), scr, 0, "n2", "pv_norm_mix")
                with contextlib.ExitStack() as sA:
                    xsT2 = sb("xsT2", (128, 24, 512), BF16, sA)
                    sz = sb("sz", (128, 4, 2048), BF16, sA)
                    ynT = sb("ynT", (128, 16, 512), BF16, sA)
                    dt_all = sb("dt_all", (128, 4, 32), F32, sA)
                    ac_all = sb("ac_all", (128, 4, 32), F32, sA)
                    ea_all = sb("ea_all", (128, 4, 32), F32, sA)
                    acT = sb("acT", (32, 4, 128), F32, sA)
                    nacT = sb("nacT", (32, 4, 128), F32, sA)
                    with contextlib.ExitStack() as sA1:
                        alloc_wst(sA1)
                        xh = sb("xh", (3, 1024), F32, sA1)
                        xhs = sb("xhs", (3, 1024), BF16, sA1)
                        hj = sb("hj", (3, 1024), BF16, sA1)
                        hss = sb("hss", (3, 1), F32, sA1)
                        hrs = sb("hrs", (3, 1), F32, sA1)
                        uTh = sb("uTh", (128, 8, 4), BF16, sA1)
                        rb = [sb("rb2_%d" % i, (128, 515), BF16, sA1) for i in range(2)]
                        cacc2 = [sb("cacc2_%d" % i, (128, 512), F32, sA1) for i in range(2)]
                        Wdt2 = sb("Wdt2", (128, 8, 32), BF16, sA1)
                        dtr = sb("dtr2", (128, 32), F32, sA1)
                        adt = sb("adt2", (128, 32), F32, sA1)
                        DMA("sp", Wdt2[:], win_v[:, :, DT0:DT0 + 32], ["wb_w_in"], ["Wdt2"], "Wdt2")
                        DMA("sp", xh[:], xo[j, 0:3, :], [], ["xh"], "xh")
                        ACT(hj[:], xh[:], AF.Square, ["xh"], ["hj", "hss"], accum=hss[:])
                        ACT(hrs[:], hss[:], AF.Sqrt, ["hss"], ["hrs"], bias=eps_t[0:3, :], scale=1.0 / 1024)
                        RECIP(hrs[:], hrs[:], ["hrs"], ["hrs"])
                        TS("dve", xhs[:], xh[:], hrs[:, 0:1], None, ALU.mult, None, ["xh", "hrs"], ["xhs"])
                        pv = bankb(1)
                        for k in range(8):
                            TR(pv[:, k * 4:k * 4 + 3], xhs[:, k * 128:(k + 1) * 128], identb[0:3, 0:3], ["xhs", "identb"], [bn(1)])
                        TT("dve", uTh[:, :, 0:3], pv[:, 0:32].rearrange("p (k t) -> p k t", k=8)[:, :, 0:3],
                           g_mix[:, 0:8].unsqueeze(2).to_broadcast([128, 8, 3]), ALU.mult, [bn(1), "pv_norm_mix"], ["uTh"])
                        P.checkpoint("A1h")
                        for unit in range(6):
                            W_, wn = load_wst(X0 + unit * 512, 512)
                            for bi in range(4):
                                blk = unit * 4 + bi
                                pb_ = 2 + (blk % 2)
                                for k in range(8):
                                    MM(bank(pb_), W_[:, k, bi * 128:(bi + 1) * 128], uT[:, k, :], k == 0, k == 7, [wn, "uT2"], [bn(pb_)])
                                for k in range(8):
                                    MM(bank(1)[:, 0:3], W_[:, k, bi * 128:(bi + 1) * 128], uTh[:, k, 0:3], k == 0, k == 7, [wn, "uTh"], [bn(1)])
                                r_ = rb[blk % 2]
                                rn = "rb2_%d" % (blk % 2)
                                CP("dve", r_[:, 0:3], bank(1)[:, 0:3], [bn(1)], [(rn, 0)])
                                ACT(r_[:, 3:515], bank(pb_), AF.Copy, [bn(pb_)], [(rn, 1)])
                                ca_ = cacc2[blk % 2]
                                can = "cacc2_%d" % (blk % 2)
                                TS("dve", ca_[:], r_[:, 0:512], cw_p[:, blk, 0:1], cb_p[:, blk:blk + 1], ALU.mult, ALU.add, [rn, "cw_p", "pv_conv_b"], [can])
                                for k in range(1, 4):
                                    STT("dve", ca_[:], r_[:, k:k + 512], cw_p[:, blk, k:k + 1], ca_[:], ALU.mult, ALU.add, [rn, "cw_p", can], [can])
                                ACT(xsT2[:, blk, :], ca_[:], AF.Silu, [can], [("xsT2", blk)])
                        P.checkpoint("A1x")
                        for zb in range(4):
                            W_, wn = load_wst(Z0 + zb * 512, 512)
                            for c in range(4):
                                pb_ = 5 + (c % 2)
                                for k in range(8):
                                    MM(bank(pb_), uT[:, k, c * 128:(c + 1) * 128], W_[:, k, :], k == 0, k == 7, [wn, "uT2"], [bn(pb_)])
                                ACT(sz[:, c, zb * 512:(zb + 1) * 512], bank(pb_), AF.Silu, [bn(pb_)], [("sz", c)])
                        P.checkpoint("A1z")
                        b7 = bank(7)
                        for c in range(4):
                            cc = slice(c * 128, (c + 1) * 128)
                            for k in range(8):
                                MM(b7[:, 0:32], uT[:, k, cc], Wdt2[:, k, :], k == 0, k == 7, ["uT2", "Wdt2"], [bn(7)])
                            TT("dve", dtr[:], b7[:, 0:32], dtb_bc[:], ALU.add, [bn(7), "bv_dt_bias"], ["dtr2"])
                            ACT(dtr[:], dtr[:], AF.Exp, ["dtr2"], ["dtr2"])
                            ACT(dt_all[:, c, :], dtr[:], AF.Ln, ["dtr2"], [("dt_all", c)], bias=1.0)
                            TT("dve", adt[:], dt_all[:, c, :], a_bc[:], ALU.mult, [("dt_all", c), "a_bc"], ["adt2"])
                            MM(b7[:, 32:64], tri[:], adt[:], True, True, ["tri", "adt2"], [bn(7)])
                            ACT(ac_all[:, c, :], b7[:, 32:64], AF.Copy, [bn(7)], [("ac_all", c)])
                            ACT(ea_all[:, c, :], b7[:, 32:64], AF.Exp, [bn(7)], [("ea_all", c)])
                            TR(b7[0:32, 128:256], ac_all[:, c, :], identf[:], [("ac_all", c), "identf"], [bn(7)])
                            ACT(acT[:, c, :], b7[0:32, 128:256], AF.Copy, [bn(7)], [("acT", c)])
                            TS("dve", nacT[:, c, :], acT[:, c, :], -1.0, None, ALU.mult, None, [("acT", c)], [("nacT", c)])
                    P.barrier()
                    P.checkpoint("A1")
                    with contextlib.ExitStack() as sA2:
                        selh = sb("selh", (32, 32 * 128), F32, sA2)
                        dskI = sb("dskI", (128, 32 * 128), BF16, sA2)
                        DMA("sp", selh[:], c_selh[:, :], [], ["selh"], "selh")
                        DMA("sp", dskI[:], DSKD[:, :], ["DSKD"], ["dskI"], "dskI")
                        x_tm = sb("x_tm", (128, 2048), BF16, sA2)
                        xdt = sb("xdt", (128, 2048), BF16, sA2)
                        OSs = [sb("OSs%d" % i, (128, 2048), BF16, sA2) for i in range(2)]
                        CBT = sb("CBT", (128, 4, 128), BF16, sA2)
                        E4 = [sb("E4_%d" % i, (128, 4, 128), BF16, sA2) for i in range(2)]
                        M4 = [sb("M4_%d" % i, (128, 4, 128), BF16, sA2) for i in range(2)]
                        tsb = [sb("tsb%d" % i, (128, 512), F32, sA2) for i in range(2)]
                        ssqg = sb("ssqg", (128, 4), F32, sA2)
                        rsg = sb("rsg", (128, 4), F32, sA2)
                        jg = sb("jg", (128, 512), BF16, sA2)
                        yn = sb("yn", (128, 2048), BF16, sA2)
                        for c in range(4):
                            cc = slice(c * 128, (c + 1) * 128)
                            os_ = OSs[c % 2]
                            osn = "OSs%d" % (c % 2)
                            DMA("sp", os_[:], OSD[j * 4 + c, :, :], ["OSD"], [osn], osn)
                            pxv = PP[2][:].bitcast(BF16)
                            for blk in range(16):
                                TR(pxv[:, blk * 128:(blk + 1) * 128], xsT2[:, blk, cc], identb[:], [("xsT2", blk), "identb"], [("PP2", blk // 8)])
                            ACT(x_tm[:], pxv, AF.Copy, ["PP2"], ["x_tm"])
                            TT("dve", xdt[:].rearrange("p (h e) -> p h e", e=64), pxv.rearrange("p (h e) -> p h e", e=64),
                               dt_all[:, c, :].unsqueeze(2).to_broadcast([128, 32, 64]), ALU.mult, ["PP2", ("dt_all", c)], ["xdt"])
                            for g4 in range(4):
                                MM(bank(6)[:, g4 * 128:(g4 + 1) * 128], xsT2[:, 16 + g4, cc], xsT2[:, 20 + g4, cc], True, True,
                                   [("xsT2", 16 + g4), ("xsT2", 20 + g4)], [bn(6)])
                            ACT(CBT[:].rearrange("p a b -> p (a b)"), bank(6), AF.Copy, [bn(6)], ["CBT"])
                            for g4 in range(4):
                                gs = slice(g4 * 512, (g4 + 1) * 512)
                                yb = 2 + (g4 % 2)
                                for hq2 in range(2):
                                    hq = g4 * 2 + hq2
                                    sbk = hq % 2
                                    for hh in range(4):
                                        h = hq * 4 + hh
                                        reg = bank(sbk)[:, hh * 128:(hh + 1) * 128]
                                        MM(reg, selh[:, h * 128:(h + 1) * 128], acT[:, c, :], True, False, ["selh", ("acT", c)], [bn(sbk)])
                                        MM(reg, nacT[:, c, :], selh[:, h * 128:(h + 1) * 128], False, False, ["selh", ("nacT", c)], [bn(sbk)])
                                        MM(reg, identb[:], maskneg[:], False, True, ["identb", "maskneg"], [bn(sbk)])
                                    e4 = E4[hq % 2]
                                    en = "E4_%d" % (hq % 2)
                                    m4 = M4[hq % 2]
                                    mn = "M4_%d" % (hq % 2)
                                    ACT(e4[:].rearrange("p a b -> p (a b)"), bank(sbk), AF.Exp, [bn(sbk)], [en])
                                    TT("dve", m4[:], e4[:], CBT[:, g4:g4 + 1, :].to_broadcast([128, 4, 128]), ALU.mult, [en, "CBT"], [mn])
                                    for hh in range(4):
                                        h = hq * 4 + hh
                                        yreg = bank(yb)[:, (h % 8) * 64:(h % 8 + 1) * 64]
                                        MM(yreg, m4[:, hh, :], xdt[:, h * 64:(h + 1) * 64], True, False, [mn, "xdt"], [bn(yb)])
                                        MM(yreg, dskI[:, h * 128:(h + 1) * 128], x_tm[:, h * 64:(h + 1) * 64], False, True, ["dskI", "x_tm"], [bn(yb)])
                                MM(bank(7), xsT2[:, 20 + g4, cc], os_[:, gs], True, True, [("xsT2", 20 + g4), osn], [bn(7)])
                                t_ = tsb[g4 % 2]
                                tn = "tsb%d" % (g4 % 2)
                                TT("dve", t_[:].rearrange("p (h e) -> p h e", e=64), bank(7).rearrange("p (h e) -> p h e", e=64),
                                   ea_all[:, c, g4 * 8:(g4 + 1) * 8].unsqueeze(2).to_broadcast([128, 8, 64]), ALU.mult,
                                   [bn(7), ("ea_all", c)], [tn])
                                TT("dve", t_[:], t_[:], bank(yb), ALU.add, [tn, bn(yb)], [tn])
                                TT("dve", t_[:], t_[:], sz[:, c, gs], ALU.mult, [tn, ("sz", c)], [tn])
                                ACT(jg[:], t_[:], AF.Square, [tn], ["jg", ("ssqg", g4)], accum=ssqg[:, g4:g4 + 1])
                                ACT(rsg[:, g4:g4 + 1], ssqg[:, g4:g4 + 1], AF.Sqrt, [("ssqg", g4)], [("rsg", g4)], bias=eps_t[:], scale=1.0 / 512)
                                RECIP(rsg[:, g4:g4 + 1], rsg[:, g4:g4 + 1], [("rsg", g4)], [("rsg", g4)])
                                ACT(yn[:, gs], t_[:], AF.Copy, [tn, ("rsg", g4)], [("yn", g4)], scale=rsg[:, g4:g4 + 1])
                            for blk in range(16):
                                TR(pxv[:, blk * 128:(blk + 1) * 128], yn[:, blk * 128:(blk + 1) * 128], identb[:], ["yn", "identb"], [("PP2", blk // 8)])
                            TT("dve", ynT[:, :, cc], pxv.rearrange("p (k t) -> p k t", k=16),
                               g_ssd[:, 0:16].unsqueeze(2).to_broadcast([128, 16, 128]), ALU.mult, ["PP2", "pv_ssd_norm"], [("ynT", c)])
                    P.barrier()
                    P.checkpoint("A2")
                    if debug:
                        DMA("sp", dbg["dbg_ynT"][j, :, :], ynT[:].rearrange("p a b -> p (a b)"), ["ynT"], [], "dbg_ynT")
                    with contextlib.ExitStack() as sA3:
                        alloc_wst(sA3)
                        Wso = WPA
                        sg = [sb("sg%d" % i, (128, 512), F32, sA3) for i in range(2)]
                        for unit in range(2):
                            W_, wn = load_wst(G10 + unit * 512, 512)
                            for bi in range(4):
                                cb_ = unit * 4 + bi
                                yb = 2 + cb_ % 2
                                gb = 5 + cb_ % 2
                                for kb in range(16):
                                    MM(bank(yb), Wso[:, kb, cb_ * 128:(cb_ + 1) * 128], ynT[:, kb, :], kb == 0, kb == 15, ["WPA", "ynT"], [bn(yb)])
                                for k in range(8):
                                    MM(bank(gb), W_[:, k, bi * 128:(bi + 1) * 128], uT[:, k, :], k == 0, k == 7, [wn, "uT2"], [bn(gb)])
                                s_ = sg[cb_ % 2]
                                sn = "sg%d" % (cb_ % 2)
                                ACT(s_[:], bank(gb), AF.Sigmoid, [bn(gb)], [sn])
                                TT("dve", t1T[:, cb_, :], s_[:], bank(yb), ALU.mult, [sn, bn(yb)], [("t1T", cb_)])
                P.barrier()
                P.checkpoint("A3")
                with contextlib.ExitStack() as sBC:
                    oT = sb("oT", (64, 16, 512), BF16, sBC)
                    DMA("sp", WPA[0:64, :, :], wb["w_mla_out"].rearrange("(h e) c -> e h c", e=64), ["wb_w_mla_out"], ["WPA"], "WPA")
                    with contextlib.ExitStack() as sB:
                        QT = sb("QT", (96, 16, 512), BF16, sB)
                        maskb = sb("maskb", (128, 16, 512), BF16, sB)
                        DMA("sp", maskb[:], maskb_d[:, :, :], [], ["maskb"], "maskb")
                        with contextlib.ExitStack() as sB1:
                            alloc_wst(sB1)
                            qnT = sb("qnT", (128, 3, 512), BF16, sB1)
                            Wqb = sb("Wqb", (128, 3, 1536), BF16, sB1)
                            cs1 = sb("cs1o", (32, 512), F32, sB1)
                            cs2 = sb("cs2o", (32, 512), F32, sB1)
                            rt_i = sb("rt_i2", (32, 512), I32, sB1)
                            rt_f = sb("rt_f2", (32, 512), F32, sB1)
                            rt_a = sb("rt_a2", (32, 512), F32, sB1)
                            qpe = [sb("qpe%d" % i, (32, 512), BF16, sB1) for i in range(2)]
                            qno = [sb("qno%d" % i, (64, 512), BF16, sB1) for i in range(2)]
                            DMA("sp", Wqb[:], wb["w_q_b"].rearrange("(k p) c -> p k c", p=128), ["wb_w_q_b"], ["Wqb"], "Wqb")
                            rope_tables(poso[0:1, j * 512:(j + 1) * 512], cs1, cs2, rt_i, rt_f, rt_a, "rt2")
                            W_, wn = load_wst(QA0, 384)
                            for c in range(4):
                                pb_ = 2 + c % 2
                                for k in range(8):
                                    MM(bank(pb_)[:, 0:384], uT[:, k, c * 128:(c + 1) * 128], W_[:, k, 0:384], k == 0, k == 7, [wn, "uT2"], [bn(pb_)])
                                norm_transpose(bank(pb_)[:, 0:384], 384, g_qa, qnT, slice(c * 128, (c + 1) * 128), [bn(pb_)], ("qnT", c), scr, 1, "n2", "pv_q_a_norm")
                            for h in range(16):
                                pn = 5 + h % 2
                                for kb in range(3):
                                    MM(bank(pn)[0:64, :], Wqb[:, kb, h * 96:h * 96 + 64], qnT[:, kb, :], kb == 0, kb == 2, ["Wqb", "qnT"], [bn(pn)])
                                for kb in range(3):
                                    MM(bank(7)[0:32, :], Wqb[:, kb, h * 96 + 64:h * 96 + 96], qnT[:, kb, :], kb == 0, kb == 2, ["Wqb", "qnT"], [bn(7)])
                                for kb in range(3):
                                    MM(bank(0)[0:32, :], Wqs[:, kb, h, :], qnT[:, kb, :], kb == 0, kb == 2, ["Wqs", "qnT"], [bn(0)])
                                TT("dve", rt_f[:], bank(7)[0:32, :], cs1[:], ALU.mult, [bn(7), "rt2c1"], ["rt2f"])
                                TT("dve", rt_a[:], bank(0)[0:32, :], cs2[:], ALU.mult, [bn(0), "rt2c0"], ["rt2a"])
                                qp_ = qpe[h % 2]
                                qpn = "qpe%d" % (h % 2)
                                qn_ = qno[h % 2]
                                qnn = "qno%d" % (h % 2)
                                TT("dve", qp_[:], rt_f[:], rt_a[:], ALU.add, ["rt2f", "rt2a"], [qpn])
                                ACT(qn_[:], bank(pn)[0:64, :], AF.Copy, [bn(pn)], [qnn])
                                DMA("sp", QT[0:32, h, :], qp_[:], [qpn], [("QT", h)], "QTp%d" % (h % 2))
                                DMA("sp", QT[32:96, h, :], qn_[:], [qnn], [("QT", h)], "QTn%d" % (h % 2))
                        P.barrier()
                        P.checkpoint("B1")
                        if debug:
                            DMA("sp", dbg["dbg_QT"][j, :, :], QT[:].rearrange("p a b -> p (a b)"), ["QT"], [], "dbg_QT")
                        with contextlib.ExitStack() as sB3:
                            KTt = [sb("KTt%d" % i, (96, 4, 512), BF16, sB3) for i in range(3)]
                            Vt = [sb("Vt%d" % i, (128, 4, 260), BF16, sB3) for i in range(3)]
                            PT = [sb("PT%d" % i, (128, 1024), BF16, sB3) for i in range(6)]
                            ost = [sb("ost%d" % i, (65, 512), F32, sB3) for i in range(2)]
                            rr = [sb("rr%d" % i, (65, 512), F32, sB3) for i in range(2)]
                            sci = 0
                            pti = 0
                            kvi = 0
                            for hg in range(4):
                                for kt in range(KTN):
                                    kb_ = KTt[kvi % 3]
                                    kn_ = "KTt%d" % (kvi % 3)
                                    vb_ = Vt[kvi % 3]
                                    vn_ = "Vt%d" % (kvi % 3)
                                    kvi += 1
                                    DMA("sp", kb_[:], KT[hg * 4:(hg + 1) * 4, :, kt * 512:(kt + 1) * 512].rearrange("h e s -> e h s"),
                                        ["KT"], [kn_], kn_)
                                    DMA("sp", vb_[:], VS[hg, kt * 512:(kt + 1) * 512, :].rearrange("(kb p) e -> p kb e", p=128),
                                        ["VS"], [vn_], vn_)
                                    masked = kt >= 4 * j
                                    for hh in range(4):
                                        h = hg * 4 + hh
                                        for kbp in range(2):
                                            pi = sci % 2
                                            sci += 1
                                            for kk_ in range(2):
                                                kb = kbp * 2 + kk_
                                                sc = pi * 2 + kk_
                                                MM(bank(sc), kb_[0:96, hh, kb * 128:(kb + 1) * 128], QT[0:96, h, :], True, True,
                                                   [kn_, ("QT", h)], [bn(sc)])
                                            p_ = PT[pti % 6]
                                            pn_ = "PT%d" % (pti % 6)
                                            pti += 1
                                            ACT(p_[:], PP[pi][:, :], AF.Exp, ["PP%d" % pi], [pn_], scale=SCALE)
                                            if masked:
                                                mi = (kt - 4 * j) * 4 + kbp * 2
                                                TT("dve", p_[:].rearrange("p (a b) -> p a b", a=2), p_[:].rearrange("p (a b) -> p a b", a=2),
                                                   maskb[:, mi:mi + 2, :], ALU.mult, [pn_, "maskb"], [pn_])
                                            for kk_ in range(2):
                                                kb = kbp * 2 + kk_
                                                MM(bank(4 + hh)[0:65, :], vb_[:, kb, hh * 65:(hh + 1) * 65], p_[:, kk_ * 512:(kk_ + 1) * 512],
                                                   kt == 0 and kb == 0, kt == KTN - 1 and kb == 3, [vn_, pn_], [bn(4 + hh)])
                                for hh in range(4):
                                    h = hg * 4 + hh
                                    o_ = ost[hh % 2]
                                    on_ = "ost%d" % (hh % 2)
                                    r_ = rr[hh % 2]
                                    rn_ = "rr%d" % (hh % 2)
                                    ACT(o_[:], bank(4 + hh)[0:65, :], AF.Copy, [bn(4 + hh)], [on_])
                                    RECIP(r_[64:65, :], o_[64:65, :], [on_], [rn_])
                                    sc = hh % 4
                                    MM(bank(sc), onesf[64:65, :], r_[64:65, :], True, True, ["onesf", rn_], [bn(sc)])
                                    TT("dve", oT[:, h, :], o_[0:64, :], bank(sc)[0:64, :], ALU.mult, [on_, bn(sc)], [("oT", h)])
                    P.barrier()
                    P.checkpoint("B3")
                    if debug:
                        DMA("sp", dbg["dbg_oT"][j, :, :], oT[:].rearrange("p a b -> p (a b)"), ["oT"], [], "dbg_oT")
                    with contextlib.ExitStack() as sC:
                        alloc_wst(sC)
                        Wmo = WPA
                        Wo = sb("Wo", (128, 8, 1024), BF16, sC)
                        sg2 = [sb("sg2_%d" % i, (128, 512), F32, sC) for i in range(2)]
                        mm_ = [sb("mm_%d" % i, (128, 512), F32, sC) for i in range(2)]
                        mT = sb("mT", (128, 8, 512), BF16, sC)
                        DMA("sp", Wo[:], wb["w_o"].rearrange("(k p) c -> p k c", p=128), ["wb_w_o"], ["Wo"], "Wo")
                        for unit in range(2):
                            W_, wn = load_wst(G20 + unit * 512, 512)
                            for bi in range(4):
                                cb_ = unit * 4 + bi
                                yb = cb_ % 2
                                gb = 2 + cb_ % 2
                                for h in range(16):
                                    MM(bank(yb), Wmo[0:64, h, cb_ * 128:(cb_ + 1) * 128], oT[0:64, h, :], h == 0, h == 15, ["WPA", "oT"], [bn(yb)])
                                for k in range(8):
                                    MM(bank(gb), W_[:, k, bi * 128:(bi + 1) * 128], uT[:, k, :], k == 0, k == 7, [wn, "uT2"], [bn(gb)])
                                s_ = sg2[cb_ % 2]
                                sn = "sg2_%d" % (cb_ % 2)
                                m_ = mm_[cb_ % 2]
                                mn = "mm_%d" % (cb_ % 2)
                                ACT(s_[:], bank(gb), AF.Sigmoid, [bn(gb)], [sn])
                                TT("dve", m_[:], s_[:], bank(yb), ALU.mult, [sn, bn(yb)], [mn])
                                TT("dve", mT[:, cb_, :], m_[:], t1T[:, cb_, :], ALU.add, [mn, ("t1T", cb_)], [("mT", cb_)])
                        for c in range(4):
                            for half in range(2):
                                pb_ = 4 + half
                                for k in range(8):
                                    MM(bank(pb_), mT[:, k, c * 128:(c + 1) * 128], Wo[:, k, half * 512:(half + 1) * 512], k == 0, k == 7,
                                       ["mT", "Wo"], [bn(pb_)])
                                TT("dve", hT[:, c, half * 512:(half + 1) * 512], hT[:, c, half * 512:(half + 1) * 512], bank(pb_), ALU.add,
                                   [("hT", c), bn(pb_)], [("hT", c)])
                P.barrier()
                P.checkpoint("C")
                if debug:
                    DMA("sp", dbg["dbg_h1"][j * 512:(j + 1) * 512, :].rearrange("(c p) d -> p c d", p=128), hT[:], ["hT"], [], "dbg_h1")
                with contextlib.ExitStack() as sD:
                    hnT = sb("hnT", (128, 8, 512), BF16, sD)
                    Wxq = sb("Wxq", (128, 8, 1024), BF16, sD)
                    Wxo = sb("Wxo", (128, 8, 1024), BF16, sD)
                    qxT = sb("qxT", (128, 8, 512), BF16, sD)
                    kxT = sb("kxT", (128, 8, 256), BF16, sD)
                    vx = sb("vx", (128, 2, 1024), BF16, sD)
                    PTx = [sb("PTx%d" % i, (128, 512), BF16, sD) for i in range(2)]
                    rden = sb("rden", (128, 512), F32, sD)
                    oxT = sb("oxT", (128, 8, 512), BF16, sD)
                    DMA("sp", Wxq[:], wb["w_xq"].rearrange("(k p) c -> p k c", p=128), ["wb_w_xq"], ["Wxq"], "Wxq")
                    DMA("sp", Wxo[:], wb["w_xo"].rearrange("(k p) c -> p k c", p=128), ["wb_w_xo"], ["Wxo"], "Wxo")
                    DMA("sp", kxT[:].rearrange("p a b -> p (a b)"), KXD[:, :], ["KXD"], ["kxT"], "kxT")
                    DMA("sp", vx[:].rearrange("p a b -> p (a b)"), VXD[:, :], ["VXD"], ["vx"], "vx")
                    for c in range(4):
                        norm_transpose(hT[:, c, :], 1024, g_xa, hnT, slice(c * 128, (c + 1) * 128), [("hT", c)], ("hnT", c), scr, 0, "n2", "pv_norm_xattn")
                    for blk in range(8):
                        pb_ = 2 + blk % 2
                        for k in range(8):
                            MM(bank(pb_), Wxq[:, k, blk * 128:(blk + 1) * 128], hnT[:, k, :], k == 0, k == 7, ["Wxq", "hnT"], [bn(pb_)])
                        ACT(qxT[:, blk, :], bank(pb_), AF.Copy, [bn(pb_)], [("qxT", blk)])
                    for a in range(4):
                        for mb in range(2):
                            for dc in range(2):
                                MM(bank(mb), kxT[:, a * 2 + dc, mb * 128:(mb + 1) * 128], qxT[:, a * 2 + dc, :], dc == 0, dc == 1,
                                   ["kxT", ("qxT", a * 2 + dc)], [bn(mb)])
                            ACT(PTx[mb][:], bank(mb), AF.Exp, [bn(mb)], ["PTx%d" % mb], scale=float(256 ** -0.5))
                        for mb in range(2):
                            MM(bank(4), onesb[:], PTx[mb][:], mb == 0, mb == 1, ["onesb", "PTx%d" % mb], [bn(4)])
                        RECIP(rden[:], bank(4), [bn(4)], ["rden"])
                        for db in range(2):
                            pb_ = 5 + db
                            for mb in range(2):
                                MM(bank(pb_), vx[:, mb, a * 256 + db * 128:a * 256 + (db + 1) * 128], PTx[mb][:], mb == 0, mb == 1,
                                   ["vx", "PTx%d" % mb], [bn(pb_)])
                            TT("dve", oxT[:, a * 2 + db, :], rden[:], bank(pb_), ALU.mult, ["rden", bn(pb_)], [("oxT", a * 2 + db)])
                    for c in range(4):
                        for half in range(2):
                            pb_ = 2 + half
                            for k in range(8):
                                MM(bank(pb_), oxT[:, k, c * 128:(c + 1) * 128], Wxo[:, k, half * 512:(half + 1) * 512], k == 0, k == 7,
                                   ["oxT", "Wxo"], [bn(pb_)])
                            TT("dve", hT[:, c, half * 512:(half + 1) * 512], hT[:, c, half * 512:(half + 1) * 512], bank(pb_), ALU.add,
                               [("hT", c), bn(pb_)], [("hT", c)])
                P.barrier()
                P.checkpoint("D")
                if debug:
                    DMA("sp", dbg["dbg_h2"][j * 512:(j + 1) * 512, :].rearrange("(c p) d -> p c d", p=128), hT[:], ["hT"], [], "dbg_h2")
                with contextlib.ExitStack() as sE:
                    hnT = sb("hn2T", (128, 8, 512), BF16, sE)
                    selh = sb("selhE", (32, 32 * 128), BF16, sE)
                    DMA("sp", selh[:], c_selhb[:, :], [], ["selhE"], "selhE")
                    L = sb("L", (128, 36), F32, sE)
                    gmax = sb("gmax", (128, 1), F32, sE)
                    ngmax = sb("ngmax", (128, 1), F32, sE)
                    goh = sb("goh", (128, 4), F32, sE)
                    gj = sb("gj", (128, 4), F32, sE)
                    gsum = sb("gsum", (128, 1), F32, sE)
                    gw = sb("gw", (128, 1), F32, sE)
                    pen = sb("pen", (128, 4), F32, sE)
                    em = sb("em", (128, 32), F32, sE)
                    em2 = sb("em2", (128, 32), F32, sE)
                    m1 = sb("m1", (128, 1), F32, sE)
                    m2 = sb("m2", (128, 1), F32, sE)
                    oh1 = sb("oh1", (128, 32), F32, sE)
                    oh2 = sb("oh2", (128, 32), F32, sE)
                    dd = sb("dd", (128, 1), F32, sE)
                    ee = sb("ee", (128, 1), F32, sE)
                    w1 = sb("w1", (128, 1), F32, sE)
                    w2 = sb("w2", (128, 1), F32, sE)
                    comb = sb("comb", (128, 4, 32), F32, sE)
                    combT = sb("combT", (32, 512), BF16, sE)
                    cbs = [sb("cbs%d" % i, (128, 512), F32, sE) for i in range(2)]
                    Wg = [sb("Wg%d" % i, (128, 8, 256), BF16, sE) for i in range(2)]
                    Wu = [sb("Wu%d" % i, (128, 8, 256), BF16, sE) for i in range(2)]
                    Wd = [sb("Wd%d" % i, (128, 8, 1024), BF16, sE) for i in range(2)]
                    sgm = [sb("sgm%d" % i, (128, 512), F32, sE) for i in range(2)]
                    tg = [sb("tg%d" % i, (128, 512), F32, sE) for i in range(2)]
                    actT = [sb("actT%d" % i, (128, 8, 512), BF16, sE) for i in range(2)]
                    for c in range(4):
                        norm_transpose(hT[:, c, :], 1024, g_moe, hnT, slice(c * 128, (c + 1) * 128), [("hT", c)], ("hn2T", c), scr, 0, "n2", "pv_norm_moe")
                    for c in range(4):
                        cc = slice(c * 128, (c + 1) * 128)
                        b7 = bank(7)
                        for k in range(8):
                            MM(b7[:, 0:36], hnT[:, k, cc], Wr[:, k, :], k == 0, k == 7, [("hn2T", c), "Wr"], [bn(7)])
                        TT("dve", L[:], b7[:, 0:36], rb_bc[:], ALU.add, [bn(7), "rb_bc"], ["L"])
                        P.op("dve", lambda e: e.reduce_max(out=gmax[:], in_=L[:, 0:4], axis=mybir.AxisListType.X), reads=["L"], writes=["gmax"])
                        TS("dve", ngmax[:], gmax[:], -1.0, None, ALU.mult, None, ["gmax"], ["ngmax"])
                        TS("dve", goh[:], L[:, 0:4], gmax[:, 0:1], None, ALU.is_equal, None, ["L", "gmax"], ["goh"])
                        ACT(gj[:], L[:, 0:4], AF.Exp, ["L", "ngmax"], ["gj", "gsum"], bias=ngmax[:, 0:1], accum=gsum[:])
                        RECIP(gw[:], gsum[:], ["gsum"], ["gw"])
                        TS("dve", pen[:], goh[:], BIG, -BIG, ALU.mult, ALU.add, ["goh"], ["pen"])
                        TT("dve", em[:].rearrange("p (g e) -> p g e", g=4), L[:, 4:36].rearrange("p (g e) -> p g e", g=4),
                           pen[:, :].unsqueeze(2).to_broadcast([128, 4, 8]), ALU.add, ["L", "pen"], ["em"])
                        P.op("dve", lambda e: e.reduce_max(out=m1[:], in_=em[:], axis=mybir.AxisListType.X), reads=["em"], writes=["m1"])
                        TS("dve", oh1[:], em[:], m1[:, 0:1], None, ALU.is_equal, None, ["em", "m1"], ["oh1"])
                        STT("dve", em2[:], oh1[:], -BIG, em[:], ALU.mult, ALU.add, ["oh1", "em"], ["em2"])
                        P.op("dve", lambda e: e.reduce_max(out=m2[:], in_=em2[:], axis=mybir.AxisListType.X), reads=["em2"], writes=["m2"])
                        TS("dve", oh2[:], em2[:], m2[:, 0:1], None, ALU.is_equal, None, ["em2", "m2"], ["oh2"])
                        TT("dve", dd[:], m2[:], m1[:], ALU.subtract, ["m1", "m2"], ["dd"])
                        ACT(ee[:], dd[:], AF.Exp, ["dd"], ["ee"])
                        TS("dve", w1[:], ee[:], 1.0, None, ALU.add, None, ["ee"], ["w1"])
                        RECIP(w1[:], w1[:], ["w1"], ["w1"])
                        TT("dve", w2[:], ee[:], w1[:], ALU.mult, ["ee", "w1"], ["w2"])
                        TT("dve", w1[:], w1[:], gw[:], ALU.mult, ["w1", "gw"], ["w1"])
                        TT("dve", w2[:], w2[:], gw[:], ALU.mult, ["w2", "gw"], ["w2"])
                        TS("dve", oh1[:], oh1[:], w1[:, 0:1], None, ALU.mult, None, ["oh1", "w1"], ["oh1"])
                        STT("dve", comb[:, c, :], oh2[:], w2[:, 0:1], oh1[:], ALU.mult, ALU.add, ["oh2", "w2", "oh1"], [("comb", c)])
                        P.op("pe", lambda e, c=c: e.transpose(out=bank(6)[0:32, c * 128:(c + 1) * 128], in_=comb[:, c, :], identity=identf[:]),
                             reads=[("comb", c), "identf"], writes=[bn(6)])
                    ACT(combT[:], bank(6)[0:32, :], AF.Copy, [bn(6)], ["combT"])
                    if debug:
                        DMA("sp", dbg["dbg_comb"][j * 512:(j + 1) * 512, :].rearrange("(c p) e -> p c e", p=128), comb[:], ["comb"], [], "dbg_comb")
                    wg_v = wb["w_exp_gate"].rearrange("(e k p) f -> e p k f", e=32, p=128)
                    wu_v = wb["w_exp_up"].rearrange("(e k p) f -> e p k f", e=32, p=128)
                    wd_v = wb["w_exp_down"].rearrange("(g k p) c -> g p k c", g=8, p=128)
                    for eg in range(8):
                        wd_ = Wd[eg % 2]
                        wdn = "Wd%d" % (eg % 2)
                        DMA("sp", wd_[:], wd_v[eg], ["wb_w_exp_down"], [wdn], wdn)
                        at_ = actT[eg % 2]
                        atn = "actT%d" % (eg % 2)
                        for ei in range(4):
                            e_ = eg * 4 + ei
                            wg_ = Wg[e_ % 2]
                            wgn = "Wg%d" % (e_ % 2)
                            wu_ = Wu[e_ % 2]
                            wun = "Wu%d" % (e_ % 2)
                            DMA("sp", wg_[:], wg_v[e_], ["wb_w_exp_gate"], [wgn], wgn)
                            DMA("sp", wu_[:], wu_v[e_], ["wb_w_exp_up"], [wun], wun)
                            MM(bank(6), selh[:, e_ * 128:(e_ + 1) * 128], combT[:], True, True, ["selhE", "combT"], [bn(6)])
                            cb_ = cbs[e_ % 2]
                            cbn = "cbs%d" % (e_ % 2)
                            ACT(cb_[:], bank(6), AF.Copy, [bn(6)], [cbn])
                            for fb in range(2):
                                gbk = fb
                                ubk = 2 + fb
                                for k in range(8):
                                    MM(bank(gbk), wg_[:, k, fb * 128:(fb + 1) * 128], hnT[:, k, :], k == 0, k == 7, [wgn, "hn2T"], [bn(gbk)])
                                for k in range(8):
                                    MM(bank(ubk), wu_[:, k, fb * 128:(fb + 1) * 128], hnT[:, k, :], k == 0, k == 7, [wun, "hn2T"], [bn(ubk)])
                                s_ = sgm[fb]
                                sn = "sgm%d" % fb
                                t_ = tg[fb]
                                tn = "tg%d" % fb
                                ACT(s_[:], bank(gbk), AF.Silu, [bn(gbk)], [sn])
                                TT("dve", t_[:], s_[:], bank(ubk), ALU.mult, [sn, bn(ubk)], [tn])
                                TT("pool", at_[:, ei * 2 + fb, :], t_[:], cb_[:], ALU.mult, [tn, cbn], [(atn, ei * 2 + fb)])
                        for c in range(4):
                            for half in range(2):
                                pb_ = 4 + half
                                for fbk in range(8):
                                    MM(bank(pb_), at_[:, fbk, c * 128:(c + 1) * 128], wd_[:, fbk, half * 512:(half + 1) * 512], fbk == 0, fbk == 7,
                                       [atn, wdn], [bn(pb_)])
                                TT("dve", hT[:, c, half * 512:(half + 1) * 512], hT[:, c, half * 512:(half + 1) * 512], bank(pb_), ALU.add,
                                   [("hT", c), bn(pb_)], [("hT", c)])
                P.barrier()
                P.checkpoint("E")
                if debug:
                    DMA("sp", dbg["dbg_h3"][j * 512:(j + 1) * 512, :].rearrange("(c p) d -> p c d", p=128), hT[:], ["hT"], [], "dbg_h3")
                with contextlib.ExitStack() as sF:
                    nf_bc = sb("nf_bc", (128, 1024), F32, sF)
                    jf = sb("jf", (128, 1024), BF16, sF)
                    DMA("sp", nf_bc[:], vd["norm_final"].partition_broadcast(128), [], ["nf_bc"], "nf_bc")
                    for c in range(4):
                        ACT(jf[:], hT[:, c, :], AF.Square, [("hT", c)], ["jf", "n2ssq"], accum=ssq[:])
                        ACT(rstd[:], ssq[:], AF.Sqrt, ["n2ssq"], ["n2rstd"], bias=eps_t[:], scale=1.0 / 1024)
                        RECIP(rstd[:], rstd[:], ["n2rstd"], ["n2rstd"])
                        STT("dve", hT[:, c, :], hT[:, c, :], rstd[:, 0:1], nf_bc[:], ALU.mult, ALU.mult, [("hT", c), "n2rstd", "nf_bc"], [("hT", c)])
                    DMA("sp", out_d[j * 512:(j + 1) * 512, :].rearrange("(c p) d -> p c d", p=128), hT[:], ["hT"], [], "out")
                P.barrier()

    P.emit()
    es.close()
    return nc


def _consts():
    bf = ml_dtypes.bfloat16
    c = {}
    c["c_identb"] = np.eye(128, dtype=np.float32).astype(bf)
    c["c_identf"] = np.eye(128, dtype=np.float32)
    s = np.arange(128)
    c["c_tri"] = (s[:, None] <= s[None, :]).astype(np.float32)
    c["c_onesf"] = np.ones((128, 128), np.float32)
    c["c_maskneg"] = np.where(s[:, None] > s[None, :], NEG, 0.0).astype(np.float32).astype(bf)
    sel = np.zeros((32, 32, 128), np.float32)
    for h in range(32):
        sel[h, h, :] = 1.0
    c["c_selh"] = sel.reshape(32, 32 * 128)
    c["c_selhb"] = sel.reshape(32, 32 * 128).astype(bf)
    half = 16
    inv = (np.float32(10000.0) ** (-(np.arange(half, dtype=np.float32)) / np.float32(half))).astype(np.float32)
    invf = np.zeros((32, 2), np.float32)
    invf[:, 0] = np.concatenate([inv, inv])
    invf[:, 1] = np.concatenate([-np.ones(16), np.ones(16)])
    c["c_invf"] = invf
    return c


def make_in_maps(inputs, S):
    bf = ml_dtypes.bfloat16
    NT = S // 512
    NJ = NT // 4
    x = np.asarray(inputs["x"], np.float32)
    mem = np.asarray(inputs["mem"], np.float32)
    pos = np.asarray(inputs["positions"], np.int32)
    consts = _consts()
    shared = {}
    for n, shp in WEIGHTS:
        shared[n] = np.ascontiguousarray(np.asarray(inputs[n], np.float32).reshape(shp))
    for n, ln in VECS:
        shared[n] = np.ascontiguousarray(np.asarray(inputs[n], np.float32).reshape(1, ln))
    shared["conv_w"] = np.ascontiguousarray(np.asarray(inputs["conv_w"], np.float32).reshape(4, 3072))
    shared.update(consts)
    maps = []
    kk = np.arange(512)
    for core in range(8):
        b, q = core // 4, core % 4
        m = dict(shared)
        m["xb"] = np.ascontiguousarray(x[b])
        xo = np.zeros((NJ, 515, D), np.float32)
        po = np.zeros((1, NJ * 512), np.int32)
        for j in range(NJ):
            t0 = (4 * j + q) * 512
            xo[j, 3:] = x[b, t0:t0 + 512]
            if t0 >= 3:
                xo[j, 0:3] = x[b, t0 - 3:t0]
            po[0, j * 512:(j + 1) * 512] = pos[b, t0:t0 + 512]
        m["xo"] = xo
        m["posb"] = np.ascontiguousarray(pos[b:b + 1])
        m["poso"] = po
        m["memb"] = np.ascontiguousarray(mem[b])
        mb = np.zeros((128, 4, 4, 512), np.float32)
        for ktl in range(4):
            for kb in range(4):
                key = ktl * 512 + kb * 128 + np.arange(128)[:, None]
                qq = q * 512 + kk[None, :]
                mb[:, ktl, kb, :] = np.where(key > qq, 0.0, 1.0)
        m["maskb"] = mb.reshape(128, 16, 512).astype(bf)
        oh = np.zeros((128, 4), np.float32)
        oh[:, q] = 1.0
        m["ohq"] = oh
        maps.append(m)
    return maps


_NC_CACHE = {}


def kernel(**inputs):
    S = int(np.asarray(inputs["x"]).shape[1])
    B = int(np.asarray(inputs["x"]).shape[0])
    assert B == 2
    if S not in _NC_CACHE:
        _NC_CACHE[S] = build(S)
    nc = _NC_CACHE[S]
    maps = make_in_maps(inputs, S)
    res = run_bass_kernel_spmd(nc, maps, core_ids=list(range(8)))
    NT = S // 512
    NJ = NT // 4
    out = np.zeros((B, S, D), np.float32)
    for core in range(8):
        b, q = core // 4, core % 4
        o = np.asarray(res.results[core]["out"], np.float32)
        for j in range(NJ):
            t0 = (4 * j + q) * 512
            out[b, t0:t0 + 512] = o[j * 512:(j + 1) * 512]
    return out
```
